# Optimizing a Trainium2 kernel written in Bass

```python
import jax, jax.numpy as jnp
from jax import lax
import numpy as np

D_MODEL = 1024
BATCH = 4
SEQ = 4096
DEPTH = 2

GRID_W = 64
CTX_LEN = 256
N_EVEN = (DEPTH + 1) // 2
N_ODD = DEPTH // 2
H_A = 4
DK_A = 128
DV_A = 128
H_B = 4
DK_B = 64
DV_B = 128
CONV_W = 3
CHUNK = 32
MIX_W = H_A * DV_A + H_B * DV_B
N_HEADS_C = 8
N_KV_C = 2
GROUP_C = N_HEADS_C // N_KV_C
HEAD_DIM = 128
AXIS_DIM = HEAD_DIM // 2
Q_BLOCK = 128
ROPE_THETA = 10000.0
D_QKV_C = (N_HEADS_C + 2 * N_KV_C) * HEAD_DIM
D_FF = ((8 * D_MODEL // 3 + 255) // 256) * 256
AB_SPLIT = (H_A * DK_A, H_A * DV_A, H_A * DV_A, H_A * DK_A, H_A * DK_A,
            2 * H_B * DK_B, H_B * DV_B, H_B * DV_B, 4 * H_B)
D_AB_IN = sum(AB_SPLIT)

kernel_name = "hybrid_hgrn2_mlstm_gqa_prefix_dit"


def rmsnorm(x, g, eps=1e-6):
    xf = x.astype(jnp.float32)
    y = xf * lax.rsqrt(jnp.mean(xf * xf, axis=-1, keepdims=True) + eps)
    return (y * g.astype(jnp.float32)).astype(x.dtype)


def modulate(x, g, shift, scale):
    return rmsnorm(x, g) * (1 + scale) + shift


def ada_params(cvec, w, b):
    return jnp.split(jax.nn.silu(cvec) @ w + b, 6, axis=-1)


def swiglu(h, w_in, w_out):
    g, u = jnp.split(h @ w_in, 2, axis=-1)
    return (jax.nn.silu(g) * u) @ w_out


def conv_centred(x, w):
    return lax.conv_general_dilated(x, w[:, None, :], window_strides=(1,),
                                    padding=[(CONV_W // 2, CONV_W // 2)],
                                    dimension_numbers=('NWC', 'WIO', 'NWC'),
                                    feature_group_count=x.shape[-1])


def to_chunks(a):
    Bn, T = a.shape[:2]
    return jnp.moveaxis(a.reshape(Bn, T // CHUNK, CHUNK, *a.shape[2:]), 1, 0)


def from_chunks(a):
    nc, Bn, C = a.shape[:3]
    return jnp.moveaxis(a, 0, 1).reshape(Bn, nc * C, *a.shape[3:])


def hgrn2_chunk_scan(q, k, v, logf, state0):
    f32 = jnp.float32
    mask = jnp.tril(jnp.ones((CHUNK, CHUNK), bool))

    def step(S, xs):
        qc, kc, vc, fc = xs
        b = jnp.cumsum(fc, axis=1)
        b_mid = b[:, CHUNK // 2 - 1:CHUNK // 2]
        b_last = b[:, -1:]
        A = jnp.einsum('bthd,bshd->bhts', qc * jnp.exp(b - b_mid), kc * jnp.exp(b_mid - b))
        A = jnp.where(mask, A, 0.0)
        o = (jnp.einsum('bhts,bshe->bthe', A, vc)
             + jnp.einsum('bthd,bhde->bthe', qc * jnp.exp(b), S))
        S_new = (S * jnp.exp(b_last[:, 0])[..., None]
                 + jnp.einsum('bshd,bshe->bhde', kc * jnp.exp(b_last - b), vc))
        return S_new, o

    xs = tuple(to_chunks(a.astype(f32)) for a in (q, k, v, logf))
    S_fin, o = lax.scan(step, state0, xs)
    return from_chunks(o).astype(v.dtype), S_fin


def mlstm_chunk_scan(q, k, v, ig, logf, state0):
    f32 = jnp.float32
    mask = jnp.tril(jnp.ones((CHUNK, CHUNK), bool))

    def step(carry, xs):
        C, n, m = carry
        qc, kc, vc, igc, fc = xs
        bh = jnp.swapaxes(jnp.cumsum(fc, axis=1), 1, 2)
        igh = jnp.swapaxes(igc, 1, 2)
        D = jnp.where(mask, bh[..., :, None] - bh[..., None, :] + igh[..., None, :], -jnp.inf)
        m_inter = bh + m[..., None]
        m_t = jnp.maximum(D.max(-1), m_inter)
        S = jnp.einsum('bthd,bshd->bhts', qc, kc) * jnp.exp(D - m_t[..., None])
        w_inter = jnp.exp(m_inter - m_t)
        num = (jnp.einsum('bhts,bshe->bthe', S, vc)
               + jnp.swapaxes(w_inter, 1, 2)[..., None] * jnp.einsum('bthd,bhde->bthe', qc, C))
        den = S.sum(-1) + w_inter * jnp.einsum('bthd,bhd->bht', qc, n)
        h = num / jnp.swapaxes(jnp.maximum(jnp.abs(den), jnp.exp(-m_t)), 1, 2)[..., None]
        b_last = bh[..., -1]
        g = b_last[..., None] - bh + igh
        m_new = jnp.maximum(b_last + m, g.max(-1))
        wk = jnp.exp(g - m_new[..., None])
        decay = jnp.exp(b_last + m - m_new)
        C_new = decay[..., None, None] * C + jnp.einsum('bhs,bshd,bshe->bhde', wk, kc, vc)
        n_new = decay[..., None] * n + jnp.einsum('bhs,bshd->bhd', wk, kc)
        return (C_new, n_new, m_new), h

    xs = tuple(to_chunks(a.astype(f32)) for a in (q, k, v, ig, logf))
    state_fin, h = lax.scan(step, state0, xs)
    return from_chunks(h).astype(v.dtype), state_fin


def bidirectional_prefix(scan_fn, ctx_dirs, lat_dirs, state0):
    outs_c, outs_l = [], []
    for d, reverse in enumerate((False, True)):
        flip = (lambda a: jnp.flip(a, axis=1)) if reverse else (lambda a: a)
        o_c, st = scan_fn(*[flip(a) for a in ctx_dirs[d]], state0)
        o_l, _ = scan_fn(*[flip(a) for a in lat_dirs[d]], st)
        outs_c.append(flip(o_c))
        outs_l.append(flip(o_l))
    return outs_c[0] + outs_c[1], outs_l[0] + outs_l[1]


def mixer_ab(h_ctx, h_lat, w_in, conv_w, gate_b, lb, out_g, w_out, need_ctx):
    f32 = jnp.float32
    split_idx = [int(s) for s in np.cumsum(AB_SPLIT)[:-1]]

    def prep(h):
        Bn, T = h.shape[:2]
        aq, ai, ag, aff, afb, bqk, bv, bo, bg = jnp.split(h @ w_in, split_idx, axis=-1)
        z = jnp.stack([aff, afb], 0).astype(f32).reshape(2, Bn, T, H_A, DK_A)
        lbh = lb[:, None, None]
        logf_a = jnp.log(lbh + (1 - lbh) * jax.nn.sigmoid(z))
        k_a = (1 - lbh) * jax.nn.sigmoid(-z)
        q_a = aq.reshape(Bn, T, H_A, DK_A)
        v_a = ai.reshape(Bn, T, H_A, DV_A)
        q_b, k_b = jnp.split(jax.nn.silu(conv_centred(bqk, conv_w)), 2, axis=-1)
        q_b = q_b.reshape(Bn, T, H_B, DK_B) * (DK_B ** -0.5)
        k_b = k_b.reshape(Bn, T, H_B, DK_B)
        v_b = bv.reshape(Bn, T, H_B, DV_B)
        gates = bg.astype(f32).reshape(Bn, T, 4, H_B) + gate_b.astype(f32)
        ig_b = gates[:, :, 0::2]
        logf_b = jax.nn.log_sigmoid(gates[:, :, 1::2])
        hgrn_dirs = [(q_a, k_a[d], v_a, logf_a[d]) for d in range(2)]
        mlstm_dirs = [(q_b, k_b, v_b, ig_b[:, :, d], logf_b[:, :, d]) for d in range(2)]
        gate = jnp.concatenate([jax.nn.silu(ag), jax.nn.sigmoid(bo)], axis=-1)
        return hgrn_dirs, mlstm_dirs, gate

    ha_c, hb_c, gate_c = prep(h_ctx)
    ha_l, hb_l, gate_l = prep(h_lat)
    Bn = h_lat.shape[0]
    s0_a = jnp.zeros((Bn, H_A, DK_A, DV_A), f32)
    s0_b = (jnp.zeros((Bn, H_B, DK_B, DV_B), f32), jnp.zeros((Bn, H_B, DK_B), f32),
            jnp.zeros((Bn, H_B), f32))
    oa_c, oa_l = bidirectional_prefix(hgrn2_chunk_scan, ha_c, ha_l, s0_a)
    ob_c, ob_l = bidirectional_prefix(mlstm_chunk_scan, hb_c, hb_l, s0_b)

    def out(oa, ob, gate):
        o = jnp.concatenate([oa, ob], axis=2)
        o = rmsnorm(o, out_g.reshape(H_A + H_B, DV_A)).reshape(*o.shape[:2], MIX_W)
        return (o * gate) @ w_out

    y_lat = out(oa_l, ob_l, gate_l)
    y_ctx = out(oa_c, ob_c, gate_c) if need_ctx else None
    return y_ctx, y_lat


def axial_rope_angles(T):
    rows = T // GRID_W
    r, col = jnp.meshgrid(jnp.arange(rows, dtype=jnp.float32),
                          jnp.arange(GRID_W, dtype=jnp.float32), indexing='ij')
    inv = jnp.power(ROPE_THETA, -jnp.arange(0, AXIS_DIM, 2, dtype=jnp.float32) / AXIS_DIM)
    return jnp.stack([r.reshape(-1)[:, None] * inv, col.reshape(-1)[:, None] * inv], axis=1)


def rope2d(x, ang):
    Bn, T, H, _ = x.shape
    xf = x.astype(jnp.float32).reshape(Bn, T, H, 2, AXIS_DIM)
    x1, x2 = xf[..., :AXIS_DIM // 2], xf[..., AXIS_DIM // 2:]
    cos, sin = jnp.cos(ang)[None, :, None], jnp.sin(ang)[None, :, None]
    out = jnp.concatenate([x1 * cos - x2 * sin, x2 * cos + x1 * sin], axis=-1)
    return out.reshape(Bn, T, H, HEAD_DIM).astype(x.dtype)


def attend(q, k, v):
    s = jnp.einsum('bqhgd,bkhd->bhgqk', q, k).astype(jnp.float32)
    p = jax.nn.softmax(s, axis=-1).astype(v.dtype)
    return jnp.einsum('bhgqk,bkhd->bqhgd', p, v)


def blocked_attention(q, k, v):
    Bn, T = q.shape[:2]
    qb = jnp.moveaxis(q.reshape(Bn, T // Q_BLOCK, Q_BLOCK, *q.shape[2:]), 1, 0)
    o = lax.map(lambda qi: attend(qi, k, v), qb)
    return jnp.moveaxis(o, 0, 1).reshape(Bn, T, N_HEADS_C * HEAD_DIM)


def mixer_c(h_ctx, h_lat, w_qkv, qk_g, w_out, ang, need_ctx):
    def prep(h, use_rope):
        Bn, T = h.shape[:2]
        q, k, v = jnp.split(h @ w_qkv, [N_HEADS_C * HEAD_DIM, (N_HEADS_C + N_KV_C) * HEAD_DIM], axis=-1)
        q = rmsnorm(q.reshape(Bn, T, N_HEADS_C, HEAD_DIM), qk_g[0])
        k = rmsnorm(k.reshape(Bn, T, N_KV_C, HEAD_DIM), qk_g[1])
        v = v.reshape(Bn, T, N_KV_C, HEAD_DIM)
        if use_rope:
            q, k = rope2d(q, ang), rope2d(k, ang)
        q = q.reshape(Bn, T, N_KV_C, GROUP_C, HEAD_DIM) * (HEAD_DIM ** -0.5)
        return q, k, v

    q_c, k_c, v_c = prep(h_ctx, False)
    q_l, k_l, v_l = prep(h_lat, True)
    k_all = jnp.concatenate([k_c, k_l], axis=1)
    v_all = jnp.concatenate([v_c, v_l], axis=1)
    y_lat = blocked_attention(q_l, k_all, v_all) @ w_out
    y_ctx = None
    if need_ctx:
        Bn, Tc = h_ctx.shape[:2]
        y_ctx = attend(q_c, k_c, v_c).reshape(Bn, Tc, N_HEADS_C * HEAD_DIM) @ w_out
    return y_ctx, y_lat


def setup_inputs(seed: int = 0) -> dict:
    key = jax.random.key(seed)
    ks = jax.random.split(key, 20)
    f32 = jnp.float32

    def nrm(k, shape, s):
        return jax.random.normal(k, shape, f32) * s

    ig_b = nrm(ks[11], (N_EVEN, 2, H_B), 0.01)
    fg_b = jnp.linspace(3.0, 6.0, H_B, dtype=f32) + nrm(ks[12], (N_EVEN, 2, H_B), 0.01)
    return {
        "x": nrm(ks[0], (BATCH, SEQ, D_MODEL), 1.0),
        "c": nrm(ks[1], (BATCH, D_MODEL), 1.0),
        "ctx": nrm(ks[2], (BATCH, CTX_LEN, D_MODEL), 1.0),
        "c_ctx": nrm(ks[3], (D_MODEL,), 1.0),
        "ada_w": nrm(ks[4], (DEPTH, D_MODEL, 6 * D_MODEL), 0.5 * D_MODEL ** -0.5),
        "ada_b": nrm(ks[5], (DEPTH, 6 * D_MODEL), 0.02),
        "norm_g": 1.0 + nrm(ks[6], (DEPTH, 4, D_MODEL), 0.02),
        "ffn_w_in": nrm(ks[7], (DEPTH, D_MODEL, 2 * D_FF), D_MODEL ** -0.5),
        "ffn_w_out": nrm(ks[8], (DEPTH, D_FF, D_MODEL), D_FF ** -0.5),
        "ab_w_in": nrm(ks[9], (N_EVEN, D_MODEL, D_AB_IN), D_MODEL ** -0.5),
        "ab_conv": nrm(ks[10], (N_EVEN, CONV_W, 2 * H_B * DK_B), CONV_W ** -0.5),
        "ab_gate_b": jnp.stack([ig_b, fg_b], axis=2).reshape(N_EVEN, 4, H_B),
        "hgrn_lb": nrm(ks[13], (2, DEPTH + 1, H_A * DK_A), 0.1),
        "ab_out_g": 1.0 + nrm(ks[14], (N_EVEN, MIX_W), 0.02),
        "ab_w_out": nrm(ks[15], (N_EVEN, MIX_W, D_MODEL), MIX_W ** -0.5),
        "attn_w_qkv": nrm(ks[16], (N_ODD, D_MODEL, D_QKV_C), D_MODEL ** -0.5),
        "attn_qk_g": 1.0 + nrm(ks[17], (N_ODD, 2, HEAD_DIM), 0.02),
        "attn_w_out": nrm(ks[18], (N_ODD, N_HEADS_C * HEAD_DIM, D_MODEL), (N_HEADS_C * HEAD_DIM) ** -0.5),
    }


def reference(x, c, ctx, c_ctx, ada_w, ada_b, norm_g, ffn_w_in, ffn_w_out, ab_w_in, ab_conv,
              ab_gate_b, hgrn_lb, ab_out_g, ab_w_out, attn_w_qkv, attn_qk_g, attn_w_out):
    T = x.shape[1]
    ang = axial_rope_angles(T)
    lb_all = jnp.cumsum(jax.nn.softmax(hgrn_lb.astype(jnp.float32), axis=1), axis=1)
    for l in range(DEPTH):
        need_ctx = l < DEPTH - 1
        sh1, sc1, g1, sh2, sc2, g2 = [a[:, None, :] for a in ada_params(c, ada_w[l], ada_b[l])]
        csh1, csc1, cg1, csh2, csc2, cg2 = ada_params(c_ctx, ada_w[l], ada_b[l])
        h_lat = modulate(x, norm_g[l, 0], sh1, sc1)
        h_ctx = modulate(ctx, norm_g[l, 0], csh1, csc1)
        if l % 2 == 0:
            e = l // 2
            y_ctx, y_lat = mixer_ab(h_ctx, h_lat, ab_w_in[e], ab_conv[e], ab_gate_b[e],
                                    lb_all[:, l].reshape(2, H_A, DK_A), ab_out_g[e], ab_w_out[e],
                                    need_ctx)
        else:
            o = l // 2
            y_ctx, y_lat = mixer_c(h_ctx, h_lat, attn_w_qkv[o], attn_qk_g[o], attn_w_out[o], ang,
                                   need_ctx)
        x = x + g1 * rmsnorm(y_lat, norm_g[l, 1])
        x = x + g2 * rmsnorm(swiglu(modulate(x, norm_g[l, 2], sh2, sc2), ffn_w_in[l], ffn_w_out[l]),
                             norm_g[l, 3])
        if need_ctx:
            ctx = ctx + cg1 * rmsnorm(y_ctx, norm_g[l, 1])
            ctx = ctx + cg2 * rmsnorm(swiglu(modulate(ctx, norm_g[l, 2], csh2, csc2), ffn_w_in[l],
                                             ffn_w_out[l]), norm_g[l, 3])
    return x
```

```python
import contextlib
import math
import numpy as np
import concourse.bass as bass
import concourse.mybir as mybir
from concourse.bass_utils import run_bass_kernel_spmd

F32 = mybir.dt.float32
BF16 = mybir.dt.bfloat16
AF = mybir.ActivationFunctionType
ALU = mybir.AluOpType
AX = mybir.AxisListType

D = 1024
NCTX = 256
NLAT = 4096
TOK = NCTX + NLAT
OWN = 2048
CH = 64
NCH = TOK // CH
DFF = 2816
TOKP = TOK + 4
EPS = 1e-6
LN8 = math.log(8.0)
import os
AHEAD = int(os.environ.get('K_AHEAD', '2'))
PIPE_PREP = int(os.environ.get('K_PIPE', '0'))
ENGS = ("pe", "act", "dve", "pool", "sp")


def bpos(tok):
    return tok + 1 if tok < NCTX else tok + 3


class Op:
    __slots__ = ("eng", "fn", "deps", "is_dma", "semkey", "ndma", "tick", "signal", "idx")


class Prog:
    def __init__(self, nc):
        self.nc = nc
        self.ops = []
        self.last_writer = {}
        self.readers = {}
        self.last_dma = {}
        self.last_eng = {}
        self.barrier_deps = {}
        self.qsem = {}
        self.phase_id = 0

    def _add(self, eng, fn, reads, writes, is_dma=False, semkey=None, ndma=1):
        op = Op()
        op.eng, op.fn, op.is_dma, op.semkey, op.ndma = eng, fn, is_dma, semkey, ndma
        op.idx = len(self.ops)
        op.signal = False
        op.tick = None
        deps = set()
        for r in reads:
            w = self.last_writer.get(r)
            if w is not None:
                deps.add(w)
        for w_ in writes:
            w = self.last_writer.get(w_)
            if w is not None:
                deps.add(w)
            for rd in self.readers.get(w_, ()):
                deps.add(rd)
        if is_dma:
            prev = self.last_dma.get(semkey)
            if prev is not None:
                deps.add(prev)
            self.last_dma[semkey] = op.idx
        bd = self.barrier_deps.pop(eng, None)
        if bd:
            deps.update(bd)
        deps.discard(op.idx)
        if eng == "pe":
            deps = {d_ for d_ in deps if self.ops[d_].eng != "pe" or self.ops[d_].is_dma}
        op.deps = deps
        for r in reads:
            self.readers.setdefault(r, []).append(op.idx)
        for w_ in writes:
            self.last_writer[w_] = op.idx
            self.readers[w_] = []
        self.last_eng[eng] = op.idx
        self.ops.append(op)
        return op

    def op(self, eng, fn, reads=(), writes=()):
        return self._add(eng, fn, tuple(reads), tuple(writes))

    def dma(self, queue, fn, reads, writes, semkey, ndma=1):
        if semkey.startswith("m") and semkey[1:].isdigit():
            cnt = self.qsem.setdefault(queue, {})
            ph = self.phase_id
            k = (ph, semkey)
            if k not in cnt:
                cnt[k] = len([1 for kk in cnt if kk[0] == ph])
            semkey = "%s%d" % (queue[0], cnt[k])
        else:
            semkey = queue[0] + "_" + semkey
        return self._add(queue, fn, tuple(reads), tuple(writes), True, semkey, ndma)

    def barrier(self):
        self.phase_id += 1
        allprev = set(self.last_eng.values()) | set(self.last_dma.values())
        for e in ENGS:
            s = self.barrier_deps.setdefault(e, set())
            s.update(allprev)

    def emit(self, final_wait_keys=()):
        nc = self.nc
        ops = self.ops
        for o in ops:
            for d in o.deps:
                ops[d].signal = True
        finals = [self.last_writer[k] for k in final_wait_keys]
        for f in finals:
            ops[f].signal = True
        eng_count = {e: 0 for e in ENGS}
        dma_count = {}
        for o in ops:
            if o.is_dma:
                c = dma_count.get(o.semkey, 0) + 16 * o.ndma
                dma_count[o.semkey] = c
                o.tick = c
            elif o.signal:
                eng_count[o.eng] += 1
                o.tick = eng_count[o.eng]
        semkeys = sorted(dma_count.keys())
        with contextlib.ExitStack() as es:
            esem = {e: es.enter_context(nc.semaphore("s_" + e)) for e in ENGS}
            dsem = {k: es.enter_context(nc.semaphore("d_" + str(k))) for k in semkeys}
            block = es.enter_context(nc.Block())

            def sem_of(o):
                return dsem[o.semkey] if o.is_dma else esem[o.eng]

            def stream(engname):
                def body(e):
                    waited = {}
                    for o in ops:
                        if o.eng != engname:
                            continue
                        need = {}
                        for d in o.deps:
                            do = ops[d]
                            key = ("d", do.semkey) if do.is_dma else ("e", do.eng)
                            if need.get(key, (0, None))[0] < do.tick:
                                need[key] = (do.tick, do)
                        for key, (tick, do) in need.items():
                            if waited.get(key, 0) >= tick:
                                continue
                            e.wait_ge(sem_of(do), tick)
                            waited[key] = tick
                        res = o.fn(e)
                        if o.is_dma:
                            assert len(res) == o.ndma, (len(res), o.ndma)
                            for ins in res:
                                ins.then_inc(dsem[o.semkey], 16)
                        elif o.signal:
                            res.then_inc(esem[o.eng], 1)
                    if engname == "sp":
                        for f in finals:
                            fo = ops[f]
                            e.wait_ge(sem_of(fo), fo.tick)
                return body

            block.tensor(stream("pe"))
            block.scalar(stream("act"))
            block.vector(stream("dve"))
            block.gpsimd(stream("pool"))
            block.sync(stream("sp"))
        return eng_count, dma_count, len(ops)


class Arena:
    def __init__(self, t, nbytes):
        self.t = t
        self.n = nbytes
        self.lo = 0
        self.hi = nbytes
        self.cnt = 0
        self.semmap = {}
        self.pidx = 0

    def _view(self, off, shape, dt, parts):
        nel = int(np.prod(shape))
        if dt == F32:
            v = self.t[0:parts, off // 2: off // 2 + nel * 2].bitcast(F32)
        else:
            v = self.t[0:parts, off // 2: off // 2 + nel]
        if len(shape) == 2:
            v = v.rearrange("p (a b) -> p a b", a=shape[0])
        elif len(shape) == 3:
            v = v.rearrange("p (a b c) -> p a b c", a=shape[0], b=shape[1])
        elif len(shape) == 4:
            v = v.rearrange("p (a b c d) -> p a b c d", a=shape[0], b=shape[1], c=shape[2])
        return v

    def alloc(self, shape, dt, parts=128, persist=False):
        nb = int(np.prod(shape)) * (4 if dt == F32 else 2)
        nb = (nb + 63) // 64 * 64
        if persist:
            self.hi -= nb
            off = self.hi
        else:
            off = self.lo
            self.lo += nb
        assert self.lo <= self.hi, ("SBUF arena overflow", self.lo, self.hi)
        self.cnt += 1
        key = "sb%d" % self.cnt
        self.semmap[key] = "m%d" % self.pidx
        self.pidx += 1
        return self._view(off, shape, dt, parts), key

    def reset(self):
        self.lo = 0
        self.pidx = 0


class Buf:
    def __init__(self, ar, n, shape, dt, parts=128):
        self.slots = [ar.alloc(shape, dt, parts) for _ in range(n)]
        self.i = -1

    def next(self):
        self.i = (self.i + 1) % len(self.slots)
        return self.slots[self.i]

    def cur(self):
        return self.slots[self.i]


def build_nc(debug=False, stop_after=None):
    nc = bass.Bass("TRN2", target_bir_lowering=False)
    es = contextlib.ExitStack()

    def din(name, shape, dt=F32):
        return nc.dram_tensor(name, list(shape), dt, kind="ExternalInput").ap()

    dbg_names = []

    def dscr(name, shape, dt=F32):
        if debug:
            dbg_names.append(name)
            return nc.dram_tensor(name, list(shape), dt, kind="ExternalOutput").ap()
        return nc.dram_tensor(name, list(shape), dt, kind="Internal").ap()

    xs = din("xs", [TOK, D])
    cvec = din("cvec", [2, D])
    ada_w = din("ada_w", [2, D, 6 * D])
    ada_b = din("ada_b", [2, 6 * D])
    norm_g = din("norm_g", [2, 4, D])
    ffn_w_in = din("ffn_w_in", [2, D, 2 * DFF])
    ffn_w_out = din("ffn_w_out", [2, DFF, D])
    ab_w_in = din("ab_w_in", [D, 4112])
    ab_conv = din("ab_conv", [3, 512])
    ab_gate_b = din("ab_gate_b", [16])
    hgrn_lb = din("hgrn_lb", [2, 3, 512])
    ab_out_g = din("ab_out_g", [D])
    ab_w_out = din("ab_w_out", [D, D])
    attn_w_qkv = din("attn_w_qkv", [D, 1536])
    attn_qk_g = din("attn_qk_g", [2, 128])
    attn_w_out = din("attn_w_out", [D, D])
    c_ident = din("c_ident", [128, 128])
    c_cm = din("c_cm", [64, 2, 4, 64])
    c_sel = din("c_sel", [2, 2, 128])
    c_rope = din("c_rope", [NLAT, 2, 128])
    y_out = nc.dram_tensor("y", [OWN, D], F32, kind="ExternalOutput").ap()

    QA_c = dscr("QA_c", [NCH, 128, 4, CH], BF16)
    KA_c = dscr("KA_c", [2, NCH, 128, 4, CH], BF16)
    KA_tm = dscr("KA_tm", [2, TOK, 512], BF16)
    LF_tm = dscr("LF_tm", [2, TOK, 512], F32)
    VA_tm = dscr("VA_tm", [TOK, 512], BF16)
    BQK_d = dscr("BQK_d", [64, 8, TOKP], F32)
    QK_c = dscr("QK_c", [NCH, 2, 64, 4, CH], BF16)
    VB_tm = dscr("VB_tm", [TOK, 512], BF16)
    GT_d = dscr("GT_d", [TOK, 16], F32)
    G_tm = dscr("G_tm", [TOK, D], BF16)
    O_d = [dscr("O_f", [TOK, D], F32), dscr("O_b", [TOK, D], F32)]
    XM = dscr("XM", [TOK, D], F32)
    X1 = dscr("X1", [TOK, D], F32)
    X2 = dscr("X2", [OWN, D], F32)
    QT_d = dscr("QT_d", [OWN // 128, 128, 8, 128], BF16)
    ADA_d = dscr("ADA_d", [2, 2, 6 * D], F32)

    ARB = 206 * 1024
    arena_t = es.enter_context(nc.sbuf_tensor("arena", [128, ARB // 2], BF16))
    ar = Arena(arena_t, ARB)
    banks = [es.enter_context(nc.psum_tensor("psb%d" % i, [128, 512], F32)) for i in range(8)]
    BK = ["bank%d" % i for i in range(8)]

    p = Prog(nc)

    def pv(i, shape, dt=F32, parts=128, off=0):
        nel = int(np.prod(shape))
        if dt == F32:
            v = banks[i][0:parts, off:off + nel]
        else:
            v = banks[i][0:parts, off:off + (nel + 1) // 2].bitcast(BF16)
        if len(shape) == 2:
            v = v.rearrange("p (a b) -> p a b", a=shape[0])
        elif len(shape) == 3:
            v = v.rearrange("p (a b c) -> p a b c", a=shape[0], b=shape[1])
        return v

    def sk(key):
        return ar.semmap[key]

    def load(dst, dkey, src, semkey, rkeys=(), q="sp"):
        p.dma(q, lambda e: [e.dma_start(out=dst, in_=src, allow_slow_non_contiguous=True)], rkeys, [dkey], semkey)

    def store(dst, dkeys, src, skey, semkey, q="pool"):
        p.dma(q, lambda e: [e.dma_start(out=dst, in_=src, allow_slow_non_contiguous=True)], [skey], dkeys, semkey)

    identf, k_identf = ar.alloc([128], F32, persist=True)
    identb, k_identb = ar.alloc([128], BF16, persist=True)
    cm, k_cm = ar.alloc([2, 4, 64], F32, parts=64, persist=True)
    epsc, k_eps = ar.alloc([1], F32, persist=True)
    onec, k_one = ar.alloc([1], F32, persist=True)
    ones64, k_ones64 = ar.alloc([64], F32, parts=64, persist=True)
    sel, k_sel = ar.alloc([2, 128], F32, parts=2, persist=True)
    modc, k_modc = ar.alloc([6, 8, 2], F32, persist=True)
    gm1, k_gm1 = ar.alloc([8, 2], F32, persist=True)
    gm2, k_gm2 = ar.alloc([8, 2], F32, persist=True)
    GG1, k_GG1 = ar.alloc([2, D], F32, persist=True)
    GG2, k_GG2 = ar.alloc([2, D], F32, persist=True)

    load(identf, k_identf, c_ident, "c0")
    p.op("dve", lambda e: e.tensor_copy(out=identb, in_=identf), [k_identf], [k_identb])
    load(cm, k_cm, c_cm, "c1")
    load(sel, k_sel, c_sel.rearrange("r k m -> k r m"), "c2")
    p.op("pool", lambda e: e.memset(epsc, EPS), [], [k_eps])
    p.op("pool", lambda e: e.memset(onec, 1.0), [], [k_one])
    p.op("pool", lambda e: e.memset(ones64, 1.0), [], [k_ones64])

    def rstd_from_ssq(ssq, k_ssq, out, k_out, n, parts=128):
        p.op("act", lambda e: e.activation(out=out, in_=ssq, func=AF.Sqrt, bias=epsc[0:parts, :],
                                           scale=1.0 / n), [k_ssq, k_eps], [k_out])
        p.op("dve", lambda e: e.reciprocal(out=out, in_=out), [k_out], [k_out])

    def ada_layer(l):
        ar.reset()
        scT, k_scT = ar.alloc([8, 2], F32)
        adas, k_adas = ar.alloc([6 * D], F32, parts=2)
        adab, k_adab = ar.alloc([6 * D], F32, parts=2)
        ngc, k_ngc = ar.alloc([4, 8], F32)
        ngb, k_ngb = ar.alloc([2, D], F32)
        wbuf = Buf(ar, 2, [8, 512], F32)
        p.dma("sp", lambda e: [e.dma_start(out=scT[:, :, r_], in_=cvec[r_].rearrange("(k q) -> q k", q=128),
                                           allow_slow_non_contiguous=True) for r_ in range(2)],
              [], [k_scT], "a0", ndma=2)
        p.op("act", lambda e: e.activation(out=scT, in_=scT, func=AF.Silu), [k_scT], [k_scT])
        load(adab, k_adab, ada_b[l:l + 1, :].to_broadcast([2, 6 * D]), "a1")
        p.dma("sp", lambda e: [e.dma_start(out=ngc, in_=norm_g[l].rearrange("v (k q) -> q v k", q=128),
                                           allow_slow_non_contiguous=True)], [], [k_ngc], "a2")
        for n in range(12):
            (wt, k_wt) = wbuf.next()
            load(wt, k_wt, ada_w[l, :, n * 512:(n + 1) * 512].rearrange("(k q) n -> q k n", q=128),
                 "aw%d" % (n % 2))
            bk = n % 2

            def mm(e, wt=wt, bk=bk):
                for k in range(8):
                    r = e.matmul(banks[bk][0:2, :], lhsT=scT[:, k, :], rhs=wt[:, k, :],
                                 start=(k == 0), stop=(k == 7))
                return r
            p.op("pe", mm, [k_scT, k_wt], [BK[bk]])
            p.op("dve", lambda e, bk=bk, n=n: e.tensor_tensor(
                out=adas[:, n * 512:(n + 1) * 512], in0=banks[bk][0:2, :],
                in1=adab[:, n * 512:(n + 1) * 512], op=ALU.add), [BK[bk], k_adab], [k_adas])
        if debug:
            store(ADA_d[l], ["ADA_d"], adas, k_adas, "dbg")
        def colmm(e):
            for v in range(6):
                for k in range(8):
                    c0 = v * D + k * 128
                    r = e.matmul(pv(2, [6, 8, 2])[:, v, k, :], lhsT=adas[:, c0:c0 + 128],
                                 rhs=identf[0:2, 0:2], start=True, stop=True)
            return r
        p.op("pe", colmm, [k_adas, k_identf], [BK[2]])
        p.op("dve", lambda e: e.tensor_copy(out=modc, in_=pv(2, [6, 8, 2])), [BK[2]], [k_modc])
        for (gm, k_gm, vsc, vng) in ((gm1, k_gm1, 1, 0), (gm2, k_gm2, 4, 2)):
            p.op("dve", lambda e, gm=gm, vsc=vsc: e.tensor_scalar(
                out=gm, in0=modc[:, vsc, :, :], scalar1=1.0, scalar2=None, op0=ALU.add),
                [k_modc], [k_gm])
            p.op("dve", lambda e, gm=gm, vng=vng: e.tensor_tensor(
                out=gm, in0=gm, in1=ngc[:, vng, :].unsqueeze(2).to_broadcast([128, 8, 2]), op=ALU.mult),
                [k_gm, k_ngc], [k_gm])
        for (GG, k_GG, vg, vng) in ((GG1, k_GG1, 2, 1), (GG2, k_GG2, 5, 3)):
            load(ngb[:, 0, :], k_ngb, norm_g[l, vng:vng + 1, :].to_broadcast([128, D]), "a3")
            load(ngb[:, 1, :], k_ngb, norm_g[l, vng:vng + 1, :].to_broadcast([128, D]), "a3")
            for r in range(2):
                for hf in range(2):
                    bk = 3 + hf
                    c0 = vg * D + hf * 512
                    p.op("pe", lambda e, bk=bk, c0=c0, r=r: e.matmul(
                        banks[bk][:, :], lhsT=sel[:, r, :], rhs=adas[:, c0:c0 + 512],
                        start=True, stop=True), [k_adas, k_sel], [BK[bk]])
                    p.op("dve", lambda e, bk=bk, GG=GG, r=r, hf=hf: e.tensor_tensor(
                        out=GG[:, r, hf * 512:(hf + 1) * 512], in0=banks[bk][:, :],
                        in1=ngb[:, r, hf * 512:(hf + 1) * 512], op=ALU.mult),
                        [BK[bk], k_ngb], [k_GG])
        p.barrier()

    def load_weight_bf16(dst, dkey, src_rows, kchunks, semkey):
        for k in range(kchunks):
            p.dma("pool", lambda e, k=k: [e.dma_start(out=dst[:, k, :], in_=src_rows[k * 128:(k + 1) * 128, :])],
                  [], [dkey], "W" + str(k % 4))

    def prep_tile(src_ap, hT, k_hT, col0, gm, k_gm, shv, r, bufs, xkeep=None):
        xb, sqb, ssb, xnb = bufs
        if xkeep is None:
            (xt, k_xt) = xb.next()
        else:
            (xt, k_xt) = xkeep
        (sq, k_sq) = sqb.next()
        (ss, k_ss) = ssb.next()
        (xn, k_xn) = xnb.next()
        load(xt, k_xt, src_ap, sk(k_xt))
        p.op("act", lambda e: e.activation(out=sq, in_=xt, func=AF.Square, accum_out=ss[:, 0:1]),
             [k_xt], [k_sq, k_ss])
        rstd_from_ssq(ss[:, 0:1], k_ss, ss[:, 1:2], k_ss, D)
        p.op("act", lambda e: e.activation(out=xn, in_=xt, func=AF.Copy, scale=ss[:, 1:2]),
             [k_xt, k_ss], [k_xn])

        def tr(e):
            for k in range(8):
                r_ = e.transpose(out=pv(k // 4, [4, 128])[:, k % 4, :], in_=xn[:, k * 128:(k + 1) * 128],
                                 identity=identf)
            return r_
        p.op("pe", tr, [k_xn, k_identf], [BK[0], BK[1]])
        for k in range(8):
            p.op("dve", lambda e, k=k: e.tensor_scalar(
                out=hT[:, k, col0:col0 + 128], in0=pv(k // 4, [4, 128])[:, k % 4, :],
                scalar1=gm[:, k, r:r + 1], scalar2=modc[:, shv, k, r:r + 1], op0=ALU.mult, op1=ALU.add),
                [BK[k // 4], k_gm, k_modc], [k_hT])
        return xt, k_xt

    def residual_out(py_banks, xt, k_xt, GG, k_GG, r, dst_ap, dkey, bufs, semkey):
        sqb, ssb, outb = bufs
        (sq, k_sq) = sqb.next()
        (ss, k_ss) = ssb.next()
        (xo, k_xo) = outb.next()
        for hf in range(2):
            bk = py_banks[hf]
            p.op("act", lambda e, bk=bk, hf=hf: e.activation(
                out=sq[:, 0:512], in_=banks[bk][:, :], func=AF.Square, accum_out=ss[:, 2 + hf:3 + hf]),
                [BK[bk]], [k_sq, k_ss])
        p.op("dve", lambda e: e.tensor_tensor(out=ss[:, 0:1], in0=ss[:, 2:3], in1=ss[:, 3:4], op=ALU.add),
             [k_ss], [k_ss])
        rstd_from_ssq(ss[:, 0:1], k_ss, ss[:, 1:2], k_ss, D)
        for hf in range(2):
            bk = py_banks[hf]
            p.op("dve", lambda e, bk=bk, hf=hf: e.scalar_tensor_tensor(
                out=xo[:, hf * 512:(hf + 1) * 512], in0=banks[bk][:, :], scalar=ss[:, 1:2],
                in1=GG[:, r, hf * 512:(hf + 1) * 512], op0=ALU.mult, op1=ALU.mult),
                [BK[bk], k_ss, k_GG], [k_xo])
        p.op("dve", lambda e: e.tensor_tensor(out=xo, in0=xo, in1=xt, op=ALU.add), [k_xo, k_xt], [k_xo])
        store(dst_ap, [dkey], xo, k_xo, sk(k_xo))

    def phase_A():
        ar.reset()
        WA, k_WA = ar.alloc([8, 4112], BF16)
        load_weight_bf16(WA, k_WA, ab_w_in, 8, "wA")
        lbf, k_lbf = ar.alloc([2, 3, 4], F32)
        lbb, k_lbb = ar.alloc([2, 3, 512], F32)
        omlb_c, k_omlbc = ar.alloc([2, 4], F32)
        lb_b, k_lb_b = ar.alloc([2, 512], F32)
        omlb_b, k_omlb_b = ar.alloc([2, 512], F32)
        gbb, k_gbb = ar.alloc([16], F32)
        zt, k_zt = ar.alloc([8, 4], F32, parts=64)
        p.dma("sp", lambda e: [e.dma_start(out=lbf, in_=hgrn_lb.rearrange("r l (h q) -> q r l h", q=128),
                                           allow_slow_non_contiguous=True)], [], [k_lbf], "l0")
        load(lbb, k_lbb, hgrn_lb.rearrange("r l c -> (r l c)").unsqueeze(0).to_broadcast([128, 3072])
             .rearrange("p (r l c) -> p r l c", r=2, l=3), "l1")
        load(gbb, k_gbb, ab_gate_b.unsqueeze(0).to_broadcast([128, 16]), "l2")
        for (t, kt_, o1, ko1, o2, ko2) in ((lbf, k_lbf, omlb_c, k_omlbc, None, None),
                                           (lbb, k_lbb, omlb_b, k_omlb_b, lb_b, k_lb_b)):
            p.op("act", lambda e, t=t: e.activation(out=t, in_=t, func=AF.Exp), [kt_], [kt_])
            p.op("dve", lambda e, t=t, o1=o1: e.tensor_tensor(out=o1, in0=t[:, :, 0, :], in1=t[:, :, 1, :], op=ALU.add),
                 [kt_], [ko1])
            p.op("dve", lambda e, t=t, o1=o1: e.tensor_tensor(out=o1, in0=o1, in1=t[:, :, 2, :], op=ALU.add),
                 [kt_, ko1], [ko1])
            p.op("dve", lambda e, o1=o1: e.reciprocal(out=o1, in_=o1), [ko1], [ko1])
            p.op("dve", lambda e, t=t, o1=o1: e.tensor_tensor(out=o1, in0=o1, in1=t[:, :, 0, :], op=ALU.mult),
                 [kt_, ko1], [ko1])
            if o2 is not None:
                p.op("dve", lambda e, o1=o1, o2=o2: e.tensor_copy(out=o2, in_=o1), [ko1], [ko2])
            p.op("dve", lambda e, o1=o1: e.tensor_scalar(out=o1, in0=o1, scalar1=-1.0, scalar2=1.0,
                                                         op0=ALU.mult, op1=ALU.add), [ko1], [ko1])
        p.op("pool", lambda e: e.memset(zt, 0.0), [], [k_zt])
        for i, pos in enumerate((0, NCTX + 1, NCTX + 2, TOKP - 1)):
            store(BQK_d[:, :, pos:pos + 1], ["BQK_d"], zt[:, :, 0:1], k_zt, "z%d" % i, q="sp")

        NT = 256
        xb = Buf(ar, 2, [D], F32)
        sqb = Buf(ar, 1, [D], BF16)
        ssb = Buf(ar, 2, [4], F32)
        xnb = Buf(ar, 2, [D], F32)
        hTb = Buf(ar, 2, [8, NT], BF16)
        qst = Buf(ar, 2, [NT // CH, 4, CH], BF16)
        kst = Buf(ar, 2, [NT // CH, 4, CH], BF16)
        sgt = Buf(ar, 2, [NT], F32)
        bst = Buf(ar, 2, [8, NT], F32, parts=64)
        vst = Buf(ar, 2, [512], BF16)
        gst = Buf(ar, 2, [D], BF16)
        vbst = Buf(ar, 2, [512], BF16)
        sst = Buf(ar, 2, [512], F32)
        ust = Buf(ar, 2, [512], F32)
        lfst = Buf(ar, 2, [512], F32)
        ktst = Buf(ar, 2, [512], BF16)
        gtst = Buf(ar, 2, [16], F32)
        gt2 = Buf(ar, 2, [16], F32)
        fmb = [2, 3]
        tmb = [4, 5, 6]
        fm_i = [0]
        tm_i = [0]

        def fm_slot():
            i = fm_i[0]
            fm_i[0] = (i + 1) % 4
            return fmb[i // 2], (i % 2) * 256

        def tm_bank():
            i = tm_i[0]
            tm_i[0] = (i + 1) % 3
            return tmb[i]

        def prep_st(st):
            tok0 = st * NT
            r = 1 if tok0 < NCTX else 0
            (hT, k_hT) = hTb.next()
            for j in range(NT // 128):
                t0 = tok0 + j * 128
                prep_tile(xs[t0:t0 + 128, :], hT, k_hT, j * 128, gm1, k_gm1, 0, r, (xb, sqb, ssb, xnb))
            return hT, k_hT

        nxt = prep_st(0)
        for st in range(TOK // NT):
            tok0 = st * NT
            (hT, k_hT) = nxt if (PIPE_PREP or st == 0) else prep_st(st)
            if PIPE_PREP and st + 1 < TOK // NT:
                nxt = prep_st(st + 1)
            c0 = tok0 // CH
            nchk = NT // CH

            def fm_mm(bk, off, col0, hT=hT):
                def f(e):
                    for k in range(8):
                        r_ = e.matmul(banks[bk][:, off:off + NT], lhsT=WA[:, k, col0:col0 + 128], rhs=hT[:, k, :],
                                      start=(k == 0), stop=(k == 7))
                    return r_
                return f
            (qs, k_qs) = qst.next()
            for h in range(4):
                bk, off = fm_slot()
                p.op("pe", fm_mm(bk, off, h * 128), [k_hT, k_WA], [BK[bk]])
                p.op("act", lambda e, bk=bk, off=off, h=h, qs=qs: e.activation(
                    out=qs[:, :, h, :], in_=banks[bk][:, off:off + NT].rearrange("q (c t) -> q c t", t=CH),
                    func=AF.Copy), [BK[bk]], [k_qs])
            store(QA_c[c0:c0 + nchk].rearrange("c q h t -> q c (h t)"), ["QA_c"],
                  qs.rearrange("q c h t -> q c (h t)"), k_qs, sk(k_qs))
            for d in range(2):
                (ks, k_ks) = kst.next()
                for h in range(4):
                    bk, off = fm_slot()
                    (sg, k_sg) = sgt.next()
                    p.op("pe", fm_mm(bk, off, 1536 + d * 512 + h * 128), [k_hT, k_WA], [BK[bk]])
                    p.op("act", lambda e, bk=bk, off=off, sg=sg: e.activation(
                        out=sg, in_=banks[bk][:, off:off + NT], func=AF.Sigmoid, scale=-1.0), [BK[bk]], [k_sg])
                    p.op("dve", lambda e, sg=sg, ks=ks, d=d, h=h: e.tensor_scalar(
                        out=ks[:, :, h, :], in0=sg.rearrange("q (c t) -> q c t", t=CH),
                        scalar1=omlb_c[:, d, h:h + 1], scalar2=None, op0=ALU.mult),
                        [k_sg, k_omlbc], [k_ks])
                store(KA_c[d, c0:c0 + nchk].rearrange("c q h t -> q c (h t)"), ["KA_c"],
                      ks.rearrange("q c h t -> q c (h t)"), k_ks, sk(k_ks))
            (bs, k_bs) = bst.next()
            for g in range(8):
                bk, off = fm_slot()

                def f(e, bk=bk, off=off, g=g, hT=hT):
                    for k in range(8):
                        r_ = e.matmul(banks[bk][0:64, off:off + NT], lhsT=WA[:, k, 2560 + g * 64:2560 + (g + 1) * 64],
                                      rhs=hT[:, k, :], start=(k == 0), stop=(k == 7))
                    return r_
                p.op("pe", f, [k_hT, k_WA], [BK[bk]])
                p.op("act", lambda e, bk=bk, off=off, g=g, bs=bs: e.activation(
                    out=bs[:, g, :], in_=banks[bk][0:64, off:off + NT], func=AF.Copy), [BK[bk]], [k_bs])
            store(BQK_d[:, :, bpos(tok0):bpos(tok0) + NT], ["BQK_d"], bs, k_bs, sk(k_bs))
            for j in range(NT // 128):
                t0 = tok0 + j * 128

                def tm_mm(bk, col0, ncol=512, j=j, hT=hT):
                    def f(e):
                        for k in range(8):
                            r_ = e.matmul(banks[bk][:, 0:ncol], lhsT=hT[:, k, j * 128:(j + 1) * 128],
                                          rhs=WA[:, k, col0:col0 + ncol], start=(k == 0), stop=(k == 7))
                        return r_
                    return f
                bk = tm_bank()
                (vs, k_vs) = vst.next()
                p.op("pe", tm_mm(bk, 512), [k_hT, k_WA], [BK[bk]])
                p.op("act", lambda e, bk=bk, vs=vs: e.activation(out=vs, in_=banks[bk][:, :], func=AF.Copy),
                     [BK[bk]], [k_vs])
                store(VA_tm[t0:t0 + 128, :], ["VA_tm"], vs, k_vs, sk(k_vs))
                (gs, k_gs) = gst.next()
                bk = tm_bank()
                p.op("pe", tm_mm(bk, 1024), [k_hT, k_WA], [BK[bk]])
                p.op("act", lambda e, bk=bk, gs=gs: e.activation(out=gs[:, 0:512], in_=banks[bk][:, :], func=AF.Silu),
                     [BK[bk]], [k_gs])
                bk = tm_bank()
                p.op("pe", tm_mm(bk, 3584), [k_hT, k_WA], [BK[bk]])
                p.op("act", lambda e, bk=bk, gs=gs: e.activation(out=gs[:, 512:1024], in_=banks[bk][:, :],
                                                                 func=AF.Sigmoid), [BK[bk]], [k_gs])
                store(G_tm[t0:t0 + 128, :], ["G_tm"], gs, k_gs, sk(k_gs))
                bk = tm_bank()
                (vb, k_vb) = vbst.next()
                p.op("pe", tm_mm(bk, 3072), [k_hT, k_WA], [BK[bk]])
                p.op("act", lambda e, bk=bk, vb=vb: e.activation(out=vb, in_=banks[bk][:, :], func=AF.Copy),
                     [BK[bk]], [k_vb])
                store(VB_tm[t0:t0 + 128, :], ["VB_tm"], vb, k_vb, sk(k_vb))
                for d in range(2):
                    bk = tm_bank()
                    (s_, k_s) = sst.next()
                    (u_, k_u) = ust.next()
                    (lf, k_lf) = lfst.next()
                    (kt, k_kt) = ktst.next()
                    p.op("pe", tm_mm(bk, 1536 + d * 512), [k_hT, k_WA], [BK[bk]])
                    p.op("act", lambda e, bk=bk, s_=s_: e.activation(out=s_, in_=banks[bk][:, :], func=AF.Sigmoid),
                         [BK[bk]], [k_s])
                    p.op("dve", lambda e, s_=s_, u_=u_, d=d: e.tensor_tensor(out=u_, in0=s_, in1=omlb_b[:, d, :],
                                                                             op=ALU.mult), [k_s, k_omlb_b], [k_u])
                    p.op("dve", lambda e, s_=s_, u_=u_, d=d: e.tensor_tensor(out=s_, in0=u_, in1=lb_b[:, d, :],
                                                                             op=ALU.add), [k_u, k_lb_b], [k_s])
                    p.op("act", lambda e, s_=s_, lf=lf: e.activation(out=lf, in_=s_, func=AF.Ln), [k_s], [k_lf])
                    store(LF_tm[d, t0:t0 + 128, :], ["LF_tm"], lf, k_lf, sk(k_lf))
                    p.op("dve", lambda e, u_=u_, kt=kt, d=d: e.tensor_tensor(out=kt, in0=omlb_b[:, d, :], in1=u_,
                                                                             op=ALU.subtract), [k_u, k_omlb_b], [k_kt])
                    store(KA_tm[d, t0:t0 + 128, :], ["KA_tm"], kt, k_kt, sk(k_kt))
                bk = tm_bank()
                (g1_, k_g1) = gtst.next()
                (g2_, k_g2) = gt2.next()
                p.op("pe", tm_mm(bk, 4096, 16), [k_hT, k_WA], [BK[bk]])
                p.op("dve", lambda e, bk=bk, g1_=g1_: e.tensor_tensor(out=g1_, in0=banks[bk][:, 0:16], in1=gbb,
                                                                       op=ALU.add), [BK[bk], k_gbb], [k_g1])
                p.op("act", lambda e, g1_=g1_, g2_=g2_: e.activation(out=g2_, in_=g1_, func=AF.Exp, scale=-1.0),
                     [k_g1], [k_g2])
                p.op("act", lambda e, g2_=g2_: e.activation(out=g2_, in_=g2_, func=AF.Ln, bias=onec, scale=1.0),
                     [k_g2, k_one], [k_g2])
                p.op("dve", lambda e, g1_=g1_, g2_=g2_: e.tensor_scalar(
                    out=g1_.rearrange("q (a b) -> q a b", a=2)[:, :, 4:8],
                    in0=g2_.rearrange("q (a b) -> q a b", a=2)[:, :, 4:8],
                    scalar1=-1.0, scalar2=None, op0=ALU.mult), [k_g1, k_g2], [k_g1])
                store(GT_d[t0:t0 + 128, :], ["GT_d"], g1_, k_g1, sk(k_g1))
        p.barrier()


    def phase_A2():
        ar.reset()
        NT = 256
        cw2, k_cw2 = ar.alloc([4, 3], F32)
        p.dma("sp", lambda e: [e.dma_start(
            out=cw2[gg * 64:(gg + 1) * 64, :, w_],
            in_=ab_conv[w_].rearrange("(j gg q) -> gg q j", gg=2, q=64)[gg],
            allow_slow_non_contiguous=True) for w_ in range(3) for gg in range(2)],
            [], [k_cw2], "b0", ndma=6)
        xb2 = Buf(ar, 2, [4, NT + 2], F32)
        acb = Buf(ar, 2, [4, NT], F32)
        qkb2 = Buf(ar, 2, [NT // CH, 4, CH], BF16)
        for st in range(TOK // NT):
            tok0 = st * NT
            c0 = tok0 // CH
            pos0 = bpos(tok0)
            (X, k_X) = xb2.next()
            (acc, k_acc) = acb.next()
            (qo, k_qo) = qkb2.next()
            p.dma("sp", lambda e, X=X, pos0=pos0: [e.dma_start(
                out=X[gg * 64:(gg + 1) * 64, :, :],
                in_=BQK_d.rearrange("q (j gg) t -> gg q j t", gg=2)[gg, :, :, pos0 - 1:pos0 + NT + 1],
                allow_slow_non_contiguous=True) for gg in range(2)], ["BQK_d"], [k_X], sk(k_X), ndma=2)
            for j in range(4):
                p.op("dve", lambda e, X=X, acc=acc, j=j: e.tensor_scalar(
                    out=acc[:, j, :], in0=X[:, j, 0:NT], scalar1=cw2[:, j, 0:1], scalar2=None, op0=ALU.mult),
                    [k_X, k_cw2], [k_acc])
                for w in (1, 2):
                    p.op("dve", lambda e, X=X, acc=acc, j=j, w=w: e.scalar_tensor_tensor(
                        out=acc[:, j, :], in0=X[:, j, w:w + NT], scalar=cw2[:, j, w:w + 1], in1=acc[:, j, :],
                        op0=ALU.mult, op1=ALU.add), [k_X, k_cw2, k_acc], [k_acc])
            p.op("act", lambda e, acc=acc, qo=qo: e.activation(
                out=qo, in_=acc.rearrange("q j (c t) -> q c j t", t=CH), func=AF.Silu), [k_acc], [k_qo])
            p.dma("pool", lambda e, qo=qo, c0=c0: [e.dma_start(
                out=QK_c[c0:c0 + NT // CH, gg].rearrange("c q j t -> q c (j t)"),
                in_=qo[gg * 64:(gg + 1) * 64].rearrange("q c j t -> q c (j t)"),
                allow_slow_non_contiguous=True) for gg in range(2)], [k_qo], ["QK_c"], sk(k_qo), ndma=2)
        p.barrier()

    def phase_B():
        ar.reset()
        Sf = [ar.alloc([4, 128], F32) for _ in range(2)]
        Sb = [ar.alloc([4, 128], BF16) for _ in range(2)]
        Cf = [ar.alloc([4, 132], F32, parts=64) for _ in range(2)]
        Cb = [ar.alloc([4, 132], BF16, parts=64) for _ in range(2)]
        for (t, k) in Sf + Sb + Cf + Cb:
            p.op("pool", lambda e, t=t: e.memset(t, 0.0), [], [k])
        lfb = Buf(ar, 2, [512], F32, parts=64)
        qfb = Buf(ar, 2, [4, CH], BF16)
        kfb = Buf(ar, 2, [4, CH], BF16)
        ktb = Buf(ar, 2, [512], BF16, parts=64)
        vtb = Buf(ar, 2, [512], BF16, parts=64)
        E1b = Buf(ar, 2, [4, 128], F32)
        E2b = Buf(ar, 2, [4, CH], F32)
        EUb = Buf(ar, 2, [512], F32, parts=64)
        qqb = Buf(ar, 2, [4, 128], BF16)
        kkb = Buf(ar, 2, [4, CH], BF16)
        kSb = Buf(ar, 2, [512], BF16, parts=64)
        ATb = Buf(ar, 2, [4, CH], BF16, parts=64)
        osb = Buf(ar, 2, [512], F32, parts=64)
        Vxb = Buf(ar, 2, [4, 132], BF16, parts=64)
        gtb = Buf(ar, 2, [16], F32, parts=64)
        qkb = Buf(ar, 2, [8, CH], BF16, parts=64)
        argb = Buf(ar, 2, [16], F32, parts=64)
        EXb = Buf(ar, 2, [16], F32, parts=64)
        ATmb = Buf(ar, 2, [4, CH], BF16, parts=64)
        kSmb = Buf(ar, 2, [4, CH], BF16, parts=64)
        adb = Buf(ar, 2, [8], F32, parts=64)
        omb = Buf(ar, 2, [512], F32, parts=64)
        for (t, k) in Vxb.slots:
            p.op("pool", lambda e, t=t: e.memset(t, 1.0), [], [k])

        def qpos(h):
            return (h % 2) * 4 + h // 2

        def kpos(h):
            return (h % 2) * 4 + 2 + h // 2

        for it in range(NCH):
            for d in range(2):
                if d == 0:
                    c = it
                else:
                    c = (3 - it) if it < 4 else (NCH + 3 - it)
                tok0 = c * CH
                tl = CH - 1 if d == 0 else 0
                (lf, k_lf) = lfb.next()
                (qf, k_qf) = qfb.next()
                (kf, k_kf) = kfb.next()
                (kt, k_kt) = ktb.next()
                (vt, k_vt) = vtb.next()
                load(lf, k_lf, LF_tm[d, tok0:tok0 + CH, :], sk(k_lf), ["LF_tm"])
                load(qf, k_qf, QA_c[c], sk(k_qf), ["QA_c"])
                load(kf, k_kf, KA_c[d, c], sk(k_kf), ["KA_c"])
                load(kt, k_kt, KA_tm[d, tok0:tok0 + CH, :], sk(k_kt), ["KA_tm"])
                load(vt, k_vt, VA_tm[tok0:tok0 + CH, :], sk(k_vt), ["VA_tm"])
                (E1, k_E1) = E1b.next()
                (E2, k_E2) = E2b.next()
                (EU, k_EU) = EUb.next()
                (qq, k_qq) = qqb.next()
                (kk, k_kk) = kkb.next()
                (kS, k_kS) = kSb.next()
                (AT, k_AT) = ATb.next()
                (os_, k_os) = osb.next()

                def p1(e, lf=lf, d=d):
                    for h in range(4):
                        r_ = e.matmul(pv(0, [4, 128])[:, h, :], lhsT=lf[:, h * 128:(h + 1) * 128],
                                      rhs=cm[:, d, 0:2, :], start=True, stop=True)
                    return r_
                p.op("pe", p1, [k_lf, k_cm], [BK[0]])
                p.op("pe", lambda e, lf=lf, d=d: e.matmul(banks[1][0:64, :], lhsT=cm[:, d, 2, :], rhs=lf,
                                                          start=True, stop=True), [k_lf, k_cm], [BK[1]])
                p.op("act", lambda e, E1=E1: e.activation(out=E1, in_=pv(0, [4, 128]), func=AF.Exp), [BK[0]], [k_E1])
                p.op("act", lambda e, E2=E2: e.activation(out=E2, in_=pv(0, [4, 128])[:, :, 0:CH], func=AF.Exp,
                                                          scale=-1.0), [BK[0]], [k_E2])
                p.op("act", lambda e, EU=EU: e.activation(out=EU, in_=banks[1][0:64, :], func=AF.Exp), [BK[1]], [k_EU])
                p.op("dve", lambda e, qq=qq, E1=E1, qf=qf: e.tensor_tensor(
                    out=qq.rearrange("q h (a t) -> q h a t", a=2), in0=E1.rearrange("q h (a t) -> q h a t", a=2),
                    in1=qf.unsqueeze(2).to_broadcast([128, 4, 2, CH]), op=ALU.mult), [k_E1, k_qf], [k_qq])
                p.op("dve", lambda e, kk=kk, E2=E2, kf=kf: e.tensor_tensor(out=kk, in0=E2, in1=kf, op=ALU.mult),
                     [k_E2, k_kf], [k_kk])
                p.op("dve", lambda e, kS=kS, EU=EU, kt=kt: e.tensor_tensor(out=kS, in0=EU, in1=kt, op=ALU.mult),
                     [k_EU, k_kt], [k_kS])

                def p3(e, kk=kk, qq=qq):
                    for h in range(4):
                        r_ = e.matmul(pv(2, [4, CH], parts=64)[:, h, :], lhsT=kk[:, h, :], rhs=qq[:, h, 0:CH],
                                      start=True, stop=True)
                    return r_
                p.op("pe", p3, [k_kk, k_qq], [BK[2]])
                p.op("dve", lambda e, AT=AT, d=d: e.tensor_tensor(
                    out=AT, in0=pv(2, [4, CH], parts=64),
                    in1=cm[:, d, 3, :].unsqueeze(1).to_broadcast([64, 4, CH]), op=ALU.mult), [BK[2], k_cm], [k_AT])

                def p4(e, AT=AT, vt=vt, qq=qq, d=d):
                    for h in range(4):
                        e.matmul(banks[3][0:64, h * 128:(h + 1) * 128], lhsT=AT[:, h, :],
                                 rhs=vt[:, h * 128:(h + 1) * 128], start=True, stop=False)
                        r_ = e.matmul(banks[3][0:64, h * 128:(h + 1) * 128], lhsT=qq[:, h, CH:2 * CH],
                                      rhs=Sb[d][0][:, h, :], start=False, stop=True)
                    return r_
                p.op("pe", p4, [k_AT, k_vt, k_qq, Sb[d][1]], [BK[3]])

                def p5(e, kS=kS, vt=vt):
                    for h in range(4):
                        r_ = e.matmul(pv(4, [4, 128])[:, h, :], lhsT=kS[:, h * 128:(h + 1) * 128],
                                      rhs=vt[:, h * 128:(h + 1) * 128], start=True, stop=True)
                    return r_
                p.op("pe", p5, [k_kS, k_vt], [BK[4]])
                p.op("act", lambda e, os_=os_: e.activation(out=os_, in_=banks[3][0:64, :], func=AF.Copy),
                     [BK[3]], [k_os])
                store(O_d[d][tok0:tok0 + CH, 0:512], ["O%d" % d], os_, k_os, sk(k_os))
                for h in range(4):
                    p.op("dve", lambda e, h=h, d=d, E1=E1, tl=tl: e.scalar_tensor_tensor(
                        out=Sf[d][0][:, h, :], in0=Sf[d][0][:, h, :], scalar=E1[:, h, CH + tl:CH + tl + 1],
                        in1=pv(4, [4, 128])[:, h, :], op0=ALU.mult, op1=ALU.add),
                        [Sf[d][1], k_E1, BK[4]], [Sf[d][1]])
                p.op("act", lambda e, d=d: e.activation(out=Sb[d][0], in_=Sf[d][0], func=AF.Copy),
                     [Sf[d][1]], [Sb[d][1]])

                (Vx, k_Vx) = Vxb.next()
                (gt, k_gt) = gtb.next()
                (qk, k_qk) = qkb.next()
                (arg, k_arg) = argb.next()
                (EX, k_EX) = EXb.next()
                (ATm, k_ATm) = ATmb.next()
                (kSm, k_kSm) = kSmb.next()
                (ad, k_ad) = adb.next()
                (om, k_om) = omb.next()
                load(qk.rearrange("q (gg j) t -> q gg (j t)", gg=2), k_qk,
                     QK_c[c].rearrange("gg q j t -> q gg (j t)"), sk(k_qk), ["QK_c"])
                load(Vx[:, :, 0:128], k_Vx, VB_tm[tok0:tok0 + CH, :].rearrange("t (h e) -> t h e", h=4),
                     sk(k_Vx), ["VB_tm"])
                load(gt, k_gt, GT_d[tok0:tok0 + CH, :], sk(k_gt), ["GT_d"])

                lfc = gt[:, 8 * d + 4:8 * d + 8]
                igc = gt[:, 8 * d:8 * d + 4]

                def pg(e, lfc=lfc, d=d):
                    e.matmul(banks[6][0:64, 0:4], lhsT=cm[:, d, 1, :], rhs=lfc, start=True, stop=True)
                    e.matmul(banks[6][0:64, 4:8], lhsT=cm[:, d, 2, :], rhs=lfc, start=True, stop=True)
                    return e.matmul(banks[6][0:64, 8:12], lhsT=ones64, rhs=lfc, start=True, stop=True)
                p.op("pe", pg, [k_gt, k_cm, k_ones64], [BK[6]])
                p.op("dve", lambda e, arg=arg, igc=igc: e.tensor_tensor(out=arg[:, 0:4], in0=igc,
                                                                        in1=banks[6][0:64, 0:4], op=ALU.subtract),
                     [k_gt, BK[6]], [k_arg])
                p.op("dve", lambda e, arg=arg, igc=igc: e.tensor_tensor(out=arg[:, 4:8], in0=banks[6][0:64, 4:8],
                                                                        in1=igc, op=ALU.add), [k_gt, BK[6]], [k_arg])
                p.op("dve", lambda e, arg=arg: e.tensor_scalar(out=arg[:, 8:12], in0=banks[6][0:64, 0:4],
                                                               scalar1=-1.0, scalar2=LN8, op0=ALU.mult, op1=ALU.add),
                     [BK[6]], [k_arg])
                p.op("dve", lambda e, arg=arg: e.tensor_copy(out=arg[:, 12:16], in_=banks[6][0:64, 8:12]),
                     [BK[6]], [k_arg])
                p.op("act", lambda e, arg=arg, EX=EX: e.activation(out=EX, in_=arg, func=AF.Exp), [k_arg], [k_EX])

                def pst(e, qk=qk):
                    for h in range(4):
                        e.matmul(pv(7, [4, CH], parts=64)[:, h, :], lhsT=qk[:, kpos(h), :], rhs=qk[:, qpos(h), :],
                                 start=True, stop=True)
                    for h in range(4):
                        r_ = e.transpose(out=pv(7, [4, CH], BF16, parts=64, off=256)[:, h, :], in_=qk[:, kpos(h), :],
                                         identity=identb[0:64, 0:64])
                    return r_
                p.op("pe", pst, [k_qk, k_identb], [BK[7]])
                for h in range(4):
                    p.op("dve", lambda e, h=h, ATm=ATm, EX=EX, d=d: e.scalar_tensor_tensor(
                        out=ATm[:, h, :], in0=pv(7, [4, CH], parts=64)[:, h, :], scalar=EX[:, h:h + 1],
                        in1=cm[:, d, 3, :], op0=ALU.mult, op1=ALU.mult), [BK[7], k_EX, k_cm], [k_ATm])
                p.op("dve", lambda e, kSm=kSm, EX=EX: e.tensor_tensor(
                    out=kSm, in0=pv(7, [4, CH], BF16, parts=64, off=256),
                    in1=EX[:, 4:8].unsqueeze(2).to_broadcast([64, 4, CH]), op=ALU.mult), [BK[7], k_EX], [k_kSm])

                def pn(e, ATm=ATm, Vx=Vx, qk=qk, d=d):
                    for h in range(4):
                        bk = 0 if h < 2 else 1
                        o = pv(bk, [2, 132], parts=64)[:, h % 2, 0:129]
                        e.matmul(o, lhsT=ATm[:, h, :], rhs=Vx[:, h, 0:129], start=True, stop=False)
                        r_ = e.matmul(o, lhsT=qk[:, qpos(h), :], rhs=Cb[d][0][:, h, 0:129], start=False, stop=True)
                    return r_
                p.op("pe", pn, [k_ATm, k_Vx, k_qk, Cb[d][1]], [BK[0], BK[1]])

                def pc(e, kSm=kSm, Vx=Vx):
                    for h in range(4):
                        bk = 2 if h < 2 else 4
                        r_ = e.matmul(pv(bk, [2, 132], parts=64)[:, h % 2, 0:129], lhsT=kSm[:, h, :],
                                      rhs=Vx[:, h, 0:129], start=True, stop=True)
                    return r_
                p.op("pe", pc, [k_kSm, k_Vx], [BK[2], BK[4]])
                for hb in range(2):
                    p.op("act", lambda e, hb=hb, ad=ad: e.activation(
                        out=ad[:, 2 * hb:2 * hb + 2], in_=pv(hb, [2, 132], parts=64)[:, :, 128], func=AF.Abs),
                        [BK[hb]], [k_ad])
                p.op("dve", lambda e, ad=ad, EX=EX: e.tensor_tensor(out=ad[:, 0:4], in0=ad[:, 0:4], in1=EX[:, 8:12],
                                                                    op=ALU.max), [k_ad, k_EX], [k_ad])
                p.op("dve", lambda e, ad=ad: e.reciprocal(out=ad[:, 4:8], in_=ad[:, 0:4]), [k_ad], [k_ad])
                for h in range(4):
                    p.op("act", lambda e, h=h, om=om, ad=ad: e.activation(
                        out=om[:, h * 128:(h + 1) * 128], in_=pv(h // 2, [2, 132], parts=64)[:, h % 2, 0:128],
                        func=AF.Copy, scale=ad[:, 4 + h:5 + h]), [BK[h // 2], k_ad], [k_om])
                store(O_d[d][tok0:tok0 + CH, 512:1024], ["O%d" % d], om, k_om, sk(k_om))
                for h in range(4):
                    bk = 2 if h < 2 else 4
                    p.op("dve", lambda e, h=h, d=d, EX=EX, bk=bk: e.scalar_tensor_tensor(
                        out=Cf[d][0][:, h, 0:129], in0=Cf[d][0][:, h, 0:129], scalar=EX[:, 12 + h:13 + h],
                        in1=pv(bk, [2, 132], parts=64)[:, h % 2, 0:129], op0=ALU.mult, op1=ALU.add),
                        [Cf[d][1], k_EX, BK[bk]], [Cf[d][1]])
                p.op("act", lambda e, d=d: e.activation(out=Cb[d][0], in_=Cf[d][0], func=AF.Copy),
                     [Cf[d][1]], [Cb[d][1]])
        p.barrier()

    def phase_C1():
        ar.reset()
        WO, k_WO = ar.alloc([8, D], BF16)
        load_weight_bf16(WO, k_WO, ab_w_out, 8, "wO")
        OG, k_OG = ar.alloc([D], F32)
        load(OG, k_OG, ab_out_g.unsqueeze(0).to_broadcast([128, D]), "c1og")
        ofb = Buf(ar, 2, [D], F32)
        obb = Buf(ar, 2, [D], F32)
        gb = Buf(ar, 2, [D], BF16)
        xb = Buf(ar, 2, [D], F32)
        sqb = Buf(ar, 2, [D], F32)
        s8b = Buf(ar, 2, [16], F32)
        omb = Buf(ar, 2, [D], BF16)
        oTb = Buf(ar, 2, [8, 128], BF16)
        ssb = Buf(ar, 2, [4], F32)
        xob = Buf(ar, 2, [D], F32)
        for t in range(TOK // 128):
            t0 = t * 128
            r = 1 if t0 < NCTX else 0
            (of, k_of) = ofb.next()
            (ob, k_ob) = obb.next()
            (g, k_g) = gb.next()
            (xt, k_xt) = xb.next()
            (sq, k_sq) = sqb.next()
            (s8, k_s8) = s8b.next()
            (om, k_om) = omb.next()
            (oT, k_oT) = oTb.next()
            load(of, k_of, O_d[0][t0:t0 + 128, :], sk(k_of), ["O0"])
            load(ob, k_ob, O_d[1][t0:t0 + 128, :], sk(k_ob), ["O1"])
            load(g, k_g, G_tm[t0:t0 + 128, :], sk(k_g), ["G_tm"])
            load(xt, k_xt, xs[t0:t0 + 128, :], sk(k_xt))
            p.op("dve", lambda e, of=of, ob=ob: e.tensor_tensor(out=of, in0=of, in1=ob, op=ALU.add),
                 [k_of, k_ob], [k_of])
            p.op("act", lambda e, of=of, sq=sq: e.activation(out=sq, in_=of, func=AF.Square), [k_of], [k_sq])
            p.op("dve", lambda e, sq=sq, s8=s8: e.tensor_reduce(
                out=s8[:, 0:8], in_=sq.rearrange("q (h e) -> q h e", h=8), axis=AX.X, op=ALU.add), [k_sq], [k_s8])
            rstd_from_ssq(s8[:, 0:8], k_s8, s8[:, 8:16], k_s8, 128)
            p.op("dve", lambda e, of=of, s8=s8: e.tensor_tensor(
                out=of.rearrange("q (h e) -> q h e", h=8), in0=of.rearrange("q (h e) -> q h e", h=8),
                in1=s8[:, 8:16].unsqueeze(2).to_broadcast([128, 8, 128]), op=ALU.mult), [k_of, k_s8], [k_of])
            p.op("dve", lambda e, of=of: e.tensor_tensor(out=of, in0=of, in1=OG, op=ALU.mult), [k_of, k_OG], [k_of])
            p.op("dve", lambda e, of=of, g=g, om=om: e.tensor_tensor(out=om, in0=of, in1=g, op=ALU.mult),
                 [k_of, k_g], [k_om])

            def tr(e, om=om):
                for k in range(8):
                    r_ = e.transpose(out=pv(0, [8, 128], BF16)[:, k, :], in_=om[:, k * 128:(k + 1) * 128],
                                     identity=identb)
                return r_
            p.op("pe", tr, [k_om, k_identb], [BK[0]])
            p.op("act", lambda e, oT=oT: e.activation(out=oT, in_=pv(0, [8, 128], BF16), func=AF.Copy),
                 [BK[0]], [k_oT])
            pyb = (1 + 2 * (t % 2), 2 + 2 * (t % 2))
            for hf in range(2):
                def mm(e, hf=hf, oT=oT, bk=pyb[hf]):
                    for k in range(8):
                        r_ = e.matmul(banks[bk][:, :], lhsT=oT[:, k, :], rhs=WO[:, k, hf * 512:(hf + 1) * 512],
                                      start=(k == 0), stop=(k == 7))
                    return r_
                p.op("pe", mm, [k_oT, k_WO], [BK[pyb[hf]]])
            residual_out(pyb, xt, k_xt, GG1, k_GG1, r, XM[t0:t0 + 128, :], "XM", (sqb, ssb, xob), "sxm")
        p.barrier()

    def phase_FFN(l, src, skey, dst, dkey, ntok, ctx_tokens):
        ar.reset()
        W1, k_W1 = ar.alloc([8, 2 * DFF], BF16)
        W2, k_W2 = ar.alloc([22, D], BF16)
        load_weight_bf16(W1, k_W1, ffn_w_in[l], 8, "w1")
        load_weight_bf16(W2, k_W2, ffn_w_out[l], 22, "w2")
        NT = 256
        xb = Buf(ar, 2, [D], F32)
        sqb = Buf(ar, 1, [D], BF16)
        ssb = Buf(ar, 4, [4], F32)
        xnb = Buf(ar, 1, [D], F32)
        hTb = Buf(ar, 2, [8, NT], BF16)
        aTb = Buf(ar, 1, [22, NT], BF16)
        sgb = Buf(ar, 2, [512], F32)
        amb = Buf(ar, 2, [1536], BF16)
        xob = Buf(ar, 1, [D], F32)
        fmb = [2, 3, 4, 5]
        fm_i = [0]

        def fm_slot():
            i = fm_i[0]
            fm_i[0] = (i + 1) % 8
            return fmb[i // 2], (i % 2) * 256

        def prep_st(st):
            tok0 = st * NT
            r = 1 if tok0 < ctx_tokens else 0
            (hT, k_hT) = hTb.next()
            xts = []
            for j in range(NT // 128):
                t0 = tok0 + j * 128
                xts.append(prep_tile(src[t0:t0 + 128, :], hT, k_hT, j * 128, gm2, k_gm2, 3, r,
                                     (xb, sqb, ssb, xnb)))
            return hT, k_hT, xts

        nxt = prep_st(0)
        for st in range(ntok // NT):
            tok0 = st * NT
            r = 1 if tok0 < ctx_tokens else 0
            (hT, k_hT, xts) = nxt if (PIPE_PREP or st == 0) else prep_st(st)
            if PIPE_PREP and st + 1 < ntok // NT:
                nxt = prep_st(st + 1)
            (aT, k_aT) = aTb.next()
            for j in range(NT // 128):
                t0 = tok0 + j * 128
                for ps in range(2):
                    cbase = ps * 1536
                    ncols = [512, 512, 512] if ps == 0 else [512, 512, 256]
                    blocks = []
                    for i in range(3):
                        blocks.append((i, cbase + i * 512, ncols[i]))
                    for i in range(3):
                        blocks.append((3 + i, DFF + cbase + i * 512, ncols[i]))

                    def mm(e, blocks=blocks, hT=hT, j=j):
                        for k in range(8):
                            for (bk, col0, nc_) in blocks:
                                r_ = e.matmul(banks[bk][:, 0:nc_], lhsT=hT[:, k, j * 128:(j + 1) * 128],
                                              rhs=W1[:, k, col0:col0 + nc_], start=(k == 0), stop=(k == 7))
                        return r_
                    p.op("pe", mm, [k_hT, k_W1], [BK[b] for b in range(6)])
                    (am, k_am) = amb.next()
                    for i in range(3):
                        (sg, k_sg) = sgb.next()
                        nc_ = ncols[i]
                        p.op("act", lambda e, i=i, sg=sg, nc_=nc_: e.activation(
                            out=sg[:, 0:nc_], in_=banks[i][:, 0:nc_], func=AF.Silu), [BK[i]], [k_sg])
                        p.op("dve", lambda e, i=i, sg=sg, am=am, nc_=nc_: e.tensor_tensor(
                            out=am[:, i * 512:i * 512 + nc_], in0=sg[:, 0:nc_], in1=banks[3 + i][:, 0:nc_],
                            op=ALU.mult), [k_sg, BK[3 + i]], [k_am])
                    nchunk = sum(ncols) // 128
                    cb0 = cbase // 128
                    groups = [(6, 0, min(8, nchunk))]
                    if nchunk > 8:
                        groups.append((7, 8, nchunk - 8))
                    for (tb, c_lo, n_) in groups:
                        def tr(e, am=am, tb=tb, c_lo=c_lo, n_=n_):
                            for c_ in range(n_):
                                r_ = e.transpose(out=pv(tb, [8, 128], BF16)[:, c_, :],
                                                 in_=am[:, (c_lo + c_) * 128:(c_lo + c_ + 1) * 128], identity=identb)
                            return r_
                        p.op("pe", tr, [k_am, k_identb], [BK[tb]])
                        eng_ = "act" if tb == 6 else "dve"
                        if eng_ == "act":
                            p.op("act", lambda e, aT=aT, tb=tb, c_lo=c_lo, n_=n_, cb0=cb0, j=j: e.activation(
                                out=aT[:, cb0 + c_lo:cb0 + c_lo + n_, j * 128:(j + 1) * 128],
                                in_=pv(tb, [8, 128], BF16)[:, 0:n_, :], func=AF.Copy), [BK[tb]], [k_aT])
                        else:
                            p.op("dve", lambda e, aT=aT, tb=tb, c_lo=c_lo, n_=n_, cb0=cb0, j=j: e.tensor_copy(
                                out=aT[:, cb0 + c_lo:cb0 + c_lo + n_, j * 128:(j + 1) * 128],
                                in_=pv(tb, [8, 128], BF16)[:, 0:n_, :]), [BK[tb]], [k_aT])
                pyb = (6, 7)

                def mm2(e, aT=aT, j=j):
                    for cb in range(22):
                        for hf in range(2):
                            r_ = e.matmul(banks[pyb[hf]][:, :], lhsT=aT[:, cb, j * 128:(j + 1) * 128],
                                          rhs=W2[:, cb, hf * 512:(hf + 1) * 512], start=(cb == 0), stop=(cb == 21))
                    return r_
                p.op("pe", mm2, [k_aT, k_W2], [BK[6], BK[7]])
                (xt, k_xt) = xts[j]
                residual_out(pyb, xt, k_xt, GG2, k_GG2, r, dst[t0:t0 + 128, :], dkey, (sqb, ssb, xob), "sff")
        p.barrier()

    state = {}

    def phase_D():
        ar.reset()
        state["hi0"] = ar.hi
        KT, k_KT = ar.alloc([2, TOK], BF16, persist=True)
        Vs, k_Vs = ar.alloc([TOK // 128, 2, 132], BF16, persist=True)
        state["KT"] = (KT, k_KT)
        state["Vs"] = (Vs, k_Vs)
        p.op("pool", lambda e: e.memset(Vs, 1.0), [], [k_Vs])
        WQ, k_WQ = ar.alloc([8, 1536], BF16)
        load_weight_bf16(WQ, k_WQ, attn_w_qkv, 8, "wq")
        QKG, k_QKG = ar.alloc([10, 128], F32)
        load(QKG[:, 0:8, :], k_QKG, attn_qk_g[0:1, :].unsqueeze(1).to_broadcast([128, 8, 128]), "d0")
        load(QKG[:, 8:10, :], k_QKG, attn_qk_g[1:2, :].unsqueeze(1).to_broadcast([128, 2, 128]), "d1")
        p.op("dve", lambda e: e.tensor_scalar(out=QKG[:, 0:8, :], in0=QKG[:, 0:8, :], scalar1=128.0 ** -0.5,
                                              scalar2=None, op0=ALU.mult), [k_QKG], [k_QKG])
        NT = 256
        xb = Buf(ar, 2, [D], F32)
        sqb = Buf(ar, 1, [D], BF16)
        ssb = Buf(ar, 2, [4], F32)
        xnb = Buf(ar, 2, [D], F32)
        hTb = Buf(ar, 2, [8, NT], BF16)
        rpb = Buf(ar, 2, [2, 128], F32)
        sq2 = Buf(ar, 2, [10, 128], F32)
        s10 = Buf(ar, 2, [20], F32)
        t1b = Buf(ar, 2, [10, 128], F32)
        t2b = Buf(ar, 2, [10, 128], F32)
        qrb = Buf(ar, 2, [10, 128], BF16)
        qsb = Buf(ar, 2, [8, 128], BF16)
        def prep_st(st):
            tok0 = st * NT
            r = 1 if tok0 < NCTX else 0
            (hT, k_hT) = hTb.next()
            for j in range(NT // 128):
                t0 = tok0 + j * 128
                prep_tile(X1[t0:t0 + 128, :], hT, k_hT, j * 128, gm1, k_gm1, 0, r, (xb, sqb, ssb, xnb))
            return hT, k_hT

        nxt = prep_st(0)
        for st in range(TOK // NT):
            tok0 = st * NT
            is_ctx = tok0 < NCTX
            r = 1 if is_ctx else 0
            (hT, k_hT) = nxt if (PIPE_PREP or st == 0) else prep_st(st)
            if PIPE_PREP and st + 1 < TOK // NT:
                nxt = prep_st(st + 1)
            for j in range(NT // 128):
                t0 = tok0 + j * 128
                tile = t0 // 128
                own = (not is_ctx) and (t0 - NCTX) < OWN
                nh = 10 if own else 2
                h0 = 0 if own else 8
                def mm(e, bk, col0, j=j, hT=hT):
                    for k in range(8):
                        r_ = e.matmul(banks[bk][:, :], lhsT=hT[:, k, j * 128:(j + 1) * 128],
                                      rhs=WQ[:, k, col0:col0 + 512], start=(k == 0), stop=(k == 7))
                    return r_
                p.op("pe", lambda e, mm=mm: mm(e, 4, 1024), [k_hT, k_WQ], [BK[4]])
                if own:
                    p.op("pe", lambda e, mm=mm: mm(e, 2, 0), [k_hT, k_WQ], [BK[2]])
                    p.op("pe", lambda e, mm=mm: mm(e, 3, 512), [k_hT, k_WQ], [BK[3]])
                p.op("act", lambda e, tile=tile: e.activation(
                    out=Vs[:, tile, :, 0:128], in_=pv(4, [4, 128])[:, 2:4, :], func=AF.Copy), [BK[4]], [k_Vs])
                (sq, k_sq) = sq2.next()
                (s1, k_s1) = s10.next()
                (t1, k_t1) = t1b.next()
                (t2, k_t2) = t2b.next()
                (qr, k_qr) = qrb.next()
                srcs = []
                if own:
                    srcs += [(2, 0, 4), (3, 4, 4)]
                srcs += [(4, 8, 2)]
                for (bk, hh, n) in srcs:
                    p.op("act", lambda e, bk=bk, hh=hh, n=n, sq=sq: e.activation(
                        out=sq[:, hh:hh + n, :], in_=pv(bk, [4, 128])[:, 0:n, :], func=AF.Square), [BK[bk]], [k_sq])
                p.op("dve", lambda e, sq=sq, s1=s1, h0=h0, nh=nh: e.tensor_reduce(
                    out=s1[:, h0:h0 + nh], in_=sq[:, h0:h0 + nh, :], axis=AX.X, op=ALU.add), [k_sq], [k_s1])
                rstd_from_ssq(s1[:, h0:h0 + nh], k_s1, s1[:, 10 + h0:10 + h0 + nh], k_s1, 128)
                for (bk, hh, n) in srcs:
                    p.op("dve", lambda e, bk=bk, hh=hh, n=n, t1=t1, s1=s1: e.tensor_tensor(
                        out=t1[:, hh:hh + n, :], in0=pv(bk, [4, 128])[:, 0:n, :],
                        in1=s1[:, 10 + hh:10 + hh + n].unsqueeze(2).to_broadcast([128, n, 128]), op=ALU.mult),
                        [BK[bk], k_s1], [k_t1])
                if is_ctx:
                    p.op("dve", lambda e, t1=t1, qr=qr: e.tensor_tensor(
                        out=qr[:, 8:10, :], in0=t1[:, 8:10, :], in1=QKG[:, 8:10, :], op=ALU.mult),
                        [k_t1, k_QKG], [k_qr])
                else:
                    (rp, k_rp) = rpb.next()
                    lt0 = t0 - NCTX
                    load(rp, k_rp, c_rope[lt0:lt0 + 128], sk(k_rp))
                    sl = slice(h0, h0 + nh)
                    p.op("dve", lambda e, t1=t1, sl=sl: e.tensor_tensor(
                        out=t1[:, sl, :], in0=t1[:, sl, :], in1=QKG[:, sl, :], op=ALU.mult), [k_t1, k_QKG], [k_t1])
                    p.op("dve", lambda e, t1=t1, t2=t2, rp=rp, sl=sl, nh=nh: e.tensor_tensor(
                        out=t2[:, sl, :], in0=t1[:, sl, :],
                        in1=rp[:, 0, :].unsqueeze(1).to_broadcast([128, nh, 128]), op=ALU.mult),
                        [k_t1, k_rp], [k_t2])

                    def v4(a, sl=sl):
                        return a[:, sl, :].rearrange("q h (a b) -> q h a b", a=2)
                    for (ho, hi) in ((0, 32), (32, 0)):
                        p.op("dve", lambda e, t1=t1, sq=sq, rp=rp, ho=ho, hi=hi, nh=nh, v4=v4: e.tensor_tensor(
                            out=v4(sq)[:, :, :, ho:ho + 32], in0=v4(t1)[:, :, :, hi:hi + 32],
                            in1=rp[:, 1, :].rearrange("q (a b) -> q a b", a=2)[:, :, ho:ho + 32]
                            .unsqueeze(1).to_broadcast([128, nh, 2, 32]), op=ALU.mult),
                            [k_t1, k_rp], [k_sq])
                    p.op("dve", lambda e, t2=t2, sq=sq, qr=qr, sl=sl: e.tensor_tensor(
                        out=qr[:, sl, :], in0=t2[:, sl, :], in1=sq[:, sl, :], op=ALU.add), [k_t2, k_sq], [k_qr])
                def trk(e, qr=qr):
                    for g in range(2):
                        r_ = e.transpose(out=pv(5, [8, 128], BF16)[:, g, :], in_=qr[:, 8 + g, :], identity=identb)
                    return r_
                p.op("pe", trk, [k_qr, k_identb], [BK[5]])
                p.op("act", lambda e, t0=t0: e.activation(out=KT[:, :, t0:t0 + 128],
                                                          in_=pv(5, [8, 128], BF16)[:, 0:2, :], func=AF.Copy),
                     [BK[5]], [k_KT])
                if own:
                    (qs, k_qs) = qsb.next()

                    def trq(e, qr=qr):
                        for h in range(8):
                            r_ = e.transpose(out=pv(6, [8, 128], BF16)[:, h, :], in_=qr[:, h, :], identity=identb)
                        return r_
                    p.op("pe", trq, [k_qr, k_identb], [BK[6]])
                    p.op("act", lambda e, qs=qs: e.activation(out=qs, in_=pv(6, [8, 128], BF16), func=AF.Copy),
                         [BK[6]], [k_qs])
                    store(QT_d[(t0 - NCTX) // 128], ["QT_d"], qs, k_qs, sk(k_qs))
        p.barrier()

    def phase_E():
        ar.reset()
        (KT, k_KT) = state["KT"]
        (Vs, k_Vs) = state["Vs"]
        WO, k_WO = ar.alloc([8, D], BF16)
        load_weight_bf16(WO, k_WO, attn_w_out, 8, "wo1")
        qTb = Buf(ar, 2, [8, 128], BF16)
        xb = Buf(ar, 2, [D], F32)
        PTb = Buf(ar, 4, [512], BF16)
        atb = Buf(ar, 2, [8, 128], BF16)
        aTb = Buf(ar, 2, [8, 128], BF16)
        rdb = Buf(ar, 2, [4], F32)
        sqb = Buf(ar, 1, [D], BF16)
        ssb = Buf(ar, 2, [4], F32)
        xob = Buf(ar, 2, [D], F32)
        NKT = TOK // 128
        NQ = OWN // 128
        units = [(qi, g, kt) for qi in range(NQ) for g in range(2) for kt in range(NKT)]
        qbuf = {}

        def get_q(qi):
            if qi not in qbuf:
                (qT, k_qT) = qTb.next()
                load(qT, k_qT, QT_d[qi], sk(k_qT), ["QT_d"])
                qbuf[qi] = (qT, k_qT)
            return qbuf[qi]

        pts = {}

        def issue_score(u):
            (qi, g, kt) = units[u]
            (qT, k_qT) = get_q(qi)
            sb_ = u % 2
            (PT, k_PT) = PTb.next()
            pts[u] = (PT, k_PT)
            p.op("pe", lambda e, sb_=sb_, g=g, kt=kt, qT=qT: e.matmul(
                banks[sb_][:, :], lhsT=KT[:, g, kt * 128:(kt + 1) * 128],
                rhs=qT[:, 4 * g:4 * g + 4, :].rearrange("q h t -> q (h t)"), start=True, stop=True),
                [k_KT, k_qT], [BK[sb_]])
            p.op("act", lambda e, sb_=sb_, PT=PT: e.activation(out=PT, in_=banks[sb_][:, :], func=AF.Exp),
                 [BK[sb_]], [k_PT])

        for u_ in range(AHEAD):
            issue_score(u_)
        cur = {}
        for u, (qi, g, kt) in enumerate(units):
            if g == 0 and kt == 0:
                (xt, k_xt) = xb.next()
                (at, k_at) = atb.next()
                (aT, k_aT) = aTb.next()
                load(xt, k_xt, X1[NCTX + qi * 128:NCTX + (qi + 1) * 128, :], sk(k_xt), ["X1"])
                cur = dict(xt=xt, k_xt=k_xt, at=at, k_at=k_at, aT=aT, k_aT=k_aT)
            if kt == 0:
                (rd, k_rd) = rdb.next()
                cur["rd"], cur["k_rd"] = rd, k_rd
            pob = (3 + 2 * g, 4 + 2 * g)
            if AHEAD == 0:
                issue_score(u)
            (PT, k_PT) = pts.pop(u)

            def pvm(e, PT=PT, kt=kt, g=g, pob=pob):
                for hh in range(4):
                    r_ = e.matmul(pv(pob[hh // 2], [2, 132])[:, hh % 2, 0:129],
                                  lhsT=PT[:, hh * 128:(hh + 1) * 128], rhs=Vs[:, kt, g, 0:129],
                                  start=(kt == 0), stop=(kt == NKT - 1))
                return r_
            if AHEAD > 0 and u + AHEAD < len(units):
                issue_score(u + AHEAD)
            p.op("pe", pvm, [k_PT, k_Vs], [BK[pob[0]], BK[pob[1]]])
            if kt == NKT - 1:
                rd, k_rd, at, k_at = cur["rd"], cur["k_rd"], cur["at"], cur["k_at"]
                for hb in range(2):
                    p.op("dve", lambda e, hb=hb, rd=rd, pob=pob: e.reciprocal(
                        out=rd[:, 2 * hb:2 * hb + 2], in_=pv(pob[hb], [2, 132])[:, :, 128]), [BK[pob[hb]]], [k_rd])
                for hh in range(4):
                    p.op("act", lambda e, hh=hh, g=g, at=at, rd=rd, pob=pob: e.activation(
                        out=at[:, 4 * g + hh, :], in_=pv(pob[hh // 2], [2, 132])[:, hh % 2, 0:128], func=AF.Copy,
                        scale=rd[:, hh:hh + 1]), [BK[pob[hh // 2]], k_rd], [k_at])
                if g == 1:
                    aT, k_aT, xt, k_xt = cur["aT"], cur["k_aT"], cur["xt"], cur["k_xt"]

                    def tr(e, at=at):
                        for h in range(8):
                            r_ = e.transpose(out=pv(7, [8, 128], BF16)[:, h, :], in_=at[:, h, :], identity=identb)
                        return r_
                    p.op("pe", tr, [k_at, k_identb], [BK[7]])
                    p.op("dve", lambda e, aT=aT: e.tensor_copy(out=aT, in_=pv(7, [8, 128], BF16)), [BK[7]], [k_aT])
                    pyb = (2, 7)
                    for hf in range(2):
                        def mm(e, hf=hf, aT=aT, bk=pyb[hf]):
                            for k in range(8):
                                r_ = e.matmul(banks[bk][:, :], lhsT=aT[:, k, :], rhs=WO[:, k, hf * 512:(hf + 1) * 512],
                                              start=(k == 0), stop=(k == 7))
                            return r_
                        p.op("pe", mm, [k_aT, k_WO], [BK[pyb[hf]]])
                    residual_out(pyb, xt, k_xt, GG1, k_GG1, 0, X2[qi * 128:(qi + 1) * 128, :], "X2",
                                 (sqb, ssb, xob), "sx2")
        p.barrier()
        ar.hi = state["hi0"]

    phases = [
        ("ada0", lambda: ada_layer(0)),
        ("A", phase_A),
        ("A2", phase_A2),
        ("B", phase_B),
        ("C1", phase_C1),
        ("C2", lambda: phase_FFN(0, XM, "XM", X1, "X1", TOK, NCTX)),
        ("ada1", lambda: ada_layer(1)),
        ("D", phase_D),
        ("E", phase_E),
        ("F", lambda: phase_FFN(1, X2, "X2", y_out, "y", OWN, 0)),
    ]
    finals = []
    for name, fn in phases:
        fn()
        if stop_after == name:
            break
    else:
        finals = ["y"]
    if debug:
        finals = finals + [k for k in ("ADA_d", "QA_c", "KA_c", "QK_c", "KA_tm", "LF_tm", "VA_tm", "BQK_d", "VB_tm", "GT_d",
                                       "G_tm", "O0", "O1", "XM", "X1", "X2", "QT_d") if k in p.last_writer]
    stats = p.emit(final_wait_keys=finals)
    es.close()
    return nc, stats


def _consts():
    s = np.arange(64)[:, None]
    t = np.arange(64)[None, :]
    cm = np.zeros((64, 2, 4, 64), np.float32)
    tri_f = (s <= t).astype(np.float32)
    cm[:, 0, 0] = tri_f - (s <= 31).astype(np.float32)
    cm[:, 0, 1] = tri_f
    cm[:, 0, 2] = (s > t).astype(np.float32)
    cm[:, 0, 3] = tri_f
    tri_b = (s >= t).astype(np.float32)
    cm[:, 1, 0] = tri_b - (s >= 32).astype(np.float32)
    cm[:, 1, 1] = tri_b
    cm[:, 1, 2] = (s < t).astype(np.float32)
    cm[:, 1, 3] = tri_b
    sel = np.zeros((2, 2, 128), np.float32)
    sel[0, 0, :] = 1.0
    sel[1, 1, :] = 1.0
    pos = np.arange(NLAT)
    row = (pos // 64).astype(np.float32)
    col = (pos % 64).astype(np.float32)
    inv = np.power(np.float32(10000.0), -np.arange(0, 64, 2, dtype=np.float32) / np.float32(64)).astype(np.float32)
    ar_ = (row[:, None] * inv[None, :]).astype(np.float32)
    ac_ = (col[:, None] * inv[None, :]).astype(np.float32)
    rope = np.zeros((NLAT, 2, 128), np.float32)
    rope[:, 0, 0:32] = np.cos(ar_)
    rope[:, 0, 32:64] = np.cos(ar_)
    rope[:, 0, 64:96] = np.cos(ac_)
    rope[:, 0, 96:128] = np.cos(ac_)
    rope[:, 1, 0:32] = -np.sin(ar_)
    rope[:, 1, 32:64] = np.sin(ar_)
    rope[:, 1, 64:96] = -np.sin(ac_)
    rope[:, 1, 96:128] = np.sin(ac_)
    return {"c_ident": np.eye(128, dtype=np.float32), "c_cm": cm, "c_sel": sel}, rope


def make_in_maps(inputs, cores=range(8)):
    f = lambda a: np.ascontiguousarray(np.asarray(a, dtype=np.float32))
    x, c, ctx, c_ctx = f(inputs["x"]), f(inputs["c"]), f(inputs["ctx"]), f(inputs["c_ctx"])
    consts, rope = _consts()
    w_in = f(inputs["ab_w_in"])[0]
    conv = f(inputs["ab_conv"])[0]
    gb = f(inputs["ab_gate_b"])[0].reshape(16)
    lb = f(inputs["hgrn_lb"])
    w_in_r = w_in.copy()
    w_in_r[:, 1536:2048] = w_in[:, 2048:2560]
    w_in_r[:, 2048:2560] = w_in[:, 1536:2048]
    w_in_r[:, 4096:4104] = w_in[:, 4104:4112]
    w_in_r[:, 4104:4112] = w_in[:, 4096:4104]
    conv_r = np.ascontiguousarray(conv[::-1])
    gb_r = np.concatenate([gb[8:16], gb[0:8]])
    lb_r = np.ascontiguousarray(lb[::-1])
    rope_r = np.ascontiguousarray(rope[::-1])
    shared = {
        "ada_w": f(inputs["ada_w"]), "ada_b": f(inputs["ada_b"]), "norm_g": f(inputs["norm_g"]),
        "ffn_w_in": f(inputs["ffn_w_in"]), "ffn_w_out": f(inputs["ffn_w_out"]),
        "ab_out_g": f(inputs["ab_out_g"])[0], "ab_w_out": f(inputs["ab_w_out"])[0],
        "attn_w_qkv": f(inputs["attn_w_qkv"])[0], "attn_qk_g": f(inputs["attn_qk_g"])[0],
        "attn_w_out": f(inputs["attn_w_out"])[0],
    }
    shared.update(consts)
    maps = []
    for core in cores:
        b, half = core // 2, core % 2
        m = dict(shared)
        if half == 0:
            m["xs"] = np.ascontiguousarray(np.concatenate([ctx[b], x[b]], axis=0))
            m.update({"ab_w_in": w_in, "ab_conv": conv, "ab_gate_b": gb, "hgrn_lb": lb, "c_rope": rope})
        else:
            m["xs"] = np.ascontiguousarray(np.concatenate([ctx[b][::-1], x[b][::-1]], axis=0))
            m.update({"ab_w_in": w_in_r, "ab_conv": conv_r, "ab_gate_b": gb_r, "hgrn_lb": lb_r, "c_rope": rope_r})
        m["cvec"] = np.ascontiguousarray(np.stack([c[b], c_ctx], axis=0))
        maps.append(m)
    return maps


_NC_CACHE = {}


def kernel(**inputs):
    if "nc" not in _NC_CACHE:
        _NC_CACHE["nc"] = build_nc()[0]
    nc = _NC_CACHE["nc"]
    maps = make_in_maps(inputs)
    res = run_bass_kernel_spmd(nc, maps, core_ids=list(range(8)))
    out = np.zeros((4, NLAT, D), np.float32)
    for core in range(8):
        b, half = core // 2, core % 2
        y = np.asarray(res.results[core]["y"])
        if half == 0:
            out[b, 0:OWN] = y
        else:
            out[b, OWN:NLAT] = y[::-1]
    return out
```

```python
import contextlib
import math
import numpy as np
import concourse.bass as bass
import concourse.mybir as mybir
from concourse.bass_utils import run_bass_kernel_spmd

F32 = mybir.dt.float32
BF16 = mybir.dt.bfloat16
AF = mybir.ActivationFunctionType
ALU = mybir.AluOpType
AX = mybir.AxisListType

D = 1024
NCTX = 256
NLAT = 4096
TOK = NCTX + NLAT
OWN = 2048
CH = 64
NCH = TOK // CH
DFF = 2816
TOKP = TOK + 4
EPS = 1e-6
LN8 = math.log(8.0)
import os
AHEAD = int(os.environ.get('K_AHEAD', '2'))
PIPE_PREP = int(os.environ.get('K_PIPE', '0'))
ENGS = ("pe", "act", "dve", "pool", "sp")


def bpos(tok):
    return tok + 1 if tok < NCTX else tok + 3


class Op:
    __slots__ = ("eng", "fn", "deps", "is_dma", "semkey", "ndma", "tick", "signal", "idx")


class Prog:
    def __init__(self, nc):
        self.nc = nc
        self.ops = []
        self.last_writer = {}
        self.readers = {}
        self.last_dma = {}
        self.last_eng = {}
        self.barrier_deps = {}
        self.qsem = {}
        self.phase_id = 0

    def _add(self, eng, fn, reads, writes, is_dma=False, semkey=None, ndma=1):
        op = Op()
        op.eng, op.fn, op.is_dma, op.semkey, op.ndma = eng, fn, is_dma, semkey, ndma
        op.idx = len(self.ops)
        op.signal = False
        op.tick = None
        deps = set()
        for r in reads:
            w = self.last_writer.get(r)
            if w is not None:
                deps.add(w)
        for w_ in writes:
            w = self.last_writer.get(w_)
            if w is not None:
                deps.add(w)
            for rd in self.readers.get(w_, ()):
                deps.add(rd)
        if is_dma:
            prev = self.last_dma.get(semkey)
            if prev is not None:
                deps.add(prev)
            self.last_dma[semkey] = op.idx
        bd = self.barrier_deps.pop(eng, None)
        if bd:
            deps.update(bd)
        deps.discard(op.idx)
        if eng == "pe":
            deps = {d_ for d_ in deps if self.ops[d_].eng != "pe" or self.ops[d_].is_dma}
        op.deps = deps
        for r in reads:
            self.readers.setdefault(r, []).append(op.idx)
        for w_ in writes:
            self.last_writer[w_] = op.idx
            self.readers[w_] = []
        self.last_eng[eng] = op.idx
        self.ops.append(op)
        return op

    def op(self, eng, fn, reads=(), writes=()):
        return self._add(eng, fn, tuple(reads), tuple(writes))

    def dma(self, queue, fn, reads, writes, semkey, ndma=1):
        if semkey.startswith("m") and semkey[1:].isdigit():
            cnt = self.qsem.setdefault(queue, {})
            ph = self.phase_id
            k = (ph, semkey)
            if k not in cnt:
                cnt[k] = len([1 for kk in cnt if kk[0] == ph])
            semkey = "%s%d" % (queue[0], cnt[k])
        else:
            semkey = queue[0] + "_" + semkey
        return self._add(queue, fn, tuple(reads), tuple(writes), True, semkey, ndma)

    def barrier(self):
        self.phase_id += 1
        allprev = set(self.last_eng.values()) | set(self.last_dma.values())
        for e in ENGS:
            s = self.barrier_deps.setdefault(e, set())
            s.update(allprev)

    def emit(self, final_wait_keys=()):
        nc = self.nc
        ops = self.ops
        for o in ops:
            for d in o.deps:
                ops[d].signal = True
        finals = [self.last_writer[k] for k in final_wait_keys]
        for f in finals:
            ops[f].signal = True
        eng_count = {e: 0 for e in ENGS}
        dma_count = {}
        for o in ops:
            if o.is_dma:
                c = dma_count.get(o.semkey, 0) + 16 * o.ndma
                dma_count[o.semkey] = c
                o.tick = c
            elif o.signal:
                eng_count[o.eng] += 1
                o.tick = eng_count[o.eng]
        semkeys = sorted(dma_count.keys())
        with contextlib.ExitStack() as es:
            esem = {e: es.enter_context(nc.semaphore("s_" + e)) for e in ENGS}
            dsem = {k: es.enter_context(nc.semaphore("d_" + str(k))) for k in semkeys}
            block = es.enter_context(nc.Block())

            def sem_of(o):
                return dsem[o.semkey] if o.is_dma else esem[o.eng]

            def stream(engname):
                def body(e):
                    waited = {}
                    for o in ops:
                        if o.eng != engname:
                            continue
                        need = {}
                        for d in o.deps:
                            do = ops[d]
                            key = ("d", do.semkey) if do.is_dma else ("e", do.eng)
                            if need.get(key, (0, None))[0] < do.tick:
                                need[key] = (do.tick, do)
                        for key, (tick, do) in need.items():
                            if waited.get(key, 0) >= tick:
                                continue
                            e.wait_ge(sem_of(do), tick)
                            waited[key] = tick
                        res = o.fn(e)
                        if o.is_dma:
                            assert len(res) == o.ndma, (len(res), o.ndma)
                            for ins in res:
                                ins.then_inc(dsem[o.semkey], 16)
                        elif o.signal:
                            res.then_inc(esem[o.eng], 1)
                    if engname == "sp":
                        for f in finals:
                            fo = ops[f]
                            e.wait_ge(sem_of(fo), fo.tick)
                return body

            block.tensor(stream("pe"))
            block.scalar(stream("act"))
            block.vector(stream("dve"))
            block.gpsimd(stream("pool"))
            block.sync(stream("sp"))
        return eng_count, dma_count, len(ops)


class Arena:
    def __init__(self, t, nbytes):
        self.t = t
        self.n = nbytes
        self.lo = 0
        self.hi = nbytes
        self.cnt = 0
        self.semmap = {}
        self.pidx = 0

    def _view(self, off, shape, dt, parts):
        nel = int(np.prod(shape))
        if dt == F32:
            v = self.t[0:parts, off // 2: off // 2 + nel * 2].bitcast(F32)
        else:
            v = self.t[0:parts, off // 2: off // 2 + nel]
        if len(shape) == 2:
            v = v.rearrange("p (a b) -> p a b", a=shape[0])
        elif len(shape) == 3:
            v = v.rearrange("p (a b c) -> p a b c", a=shape[0], b=shape[1])
        elif len(shape) == 4:
            v = v.rearrange("p (a b c d) -> p a b c d", a=shape[0], b=shape[1], c=shape[2])
        return v

    def alloc(self, shape, dt, parts=128, persist=False):
        nb = int(np.prod(shape)) * (4 if dt == F32 else 2)
        nb = (nb + 63) // 64 * 64
        if persist:
            self.hi -= nb
            off = self.hi
        else:
            off = self.lo
            self.lo += nb
        assert self.lo <= self.hi, ("SBUF arena overflow", self.lo, self.hi)
        self.cnt += 1
        key = "sb%d" % self.cnt
        self.semmap[key] = "m%d" % self.pidx
        self.pidx += 1
        return self._view(off, shape, dt, parts), key

    def reset(self):
        self.lo = 0
        self.pidx = 0


class Buf:
    def __init__(self, ar, n, shape, dt, parts=128):
        self.slots = [ar.alloc(shape, dt, parts) for _ in range(n)]
        self.i = -1

    def next(self):
        self.i = (self.i + 1) % len(self.slots)
        return self.slots[self.i]

    def cur(self):
        return self.slots[self.i]


def build_nc(debug=False, stop_after=None):
    nc = bass.Bass("TRN2", target_bir_lowering=False)
    es = contextlib.ExitStack()

    def din(name, shape, dt=F32):
        return nc.dram_tensor(name, list(shape), dt, kind="ExternalInput").ap()

    dbg_names = []

    def dscr(name, shape, dt=F32):
        if debug:
            dbg_names.append(name)
            return nc.dram_tensor(name, list(shape), dt, kind="ExternalOutput").ap()
        return nc.dram_tensor(name, list(shape), dt, kind="Internal").ap()

    xs = din("xs", [TOK, D])
    cvec = din("cvec", [2, D])
    ada_w = din("ada_w", [2, D, 6 * D])
    ada_b = din("ada_b", [2, 6 * D])
    norm_g = din("norm_g", [2, 4, D])
    ffn_w_in = din("ffn_w_in", [2, D, 2 * DFF])
    ffn_w_out = din("ffn_w_out", [2, DFF, D])
    ab_w_in = din("ab_w_in", [D, 4112])
    ab_conv = din("ab_conv", [3, 512])
    ab_gate_b = din("ab_gate_b", [16])
    hgrn_lb = din("hgrn_lb", [2, 3, 512])
    ab_out_g = din("ab_out_g", [D])
    ab_w_out = din("ab_w_out", [D, D])
    attn_w_qkv = din("attn_w_qkv", [D, 1536])
    attn_qk_g = din("attn_qk_g", [2, 128])
    attn_w_out = din("attn_w_out", [D, D])
    c_ident = din("c_ident", [128, 128])
    c_cm = din("c_cm", [64, 2, 4, 64])
    c_sel = din("c_sel", [2, 2, 128])
    c_rope = din("c_rope", [NLAT, 2, 128])
    y_out = nc.dram_tensor("y", [OWN, D], F32, kind="ExternalOutput").ap()

    QA_c = dscr("QA_c", [NCH, 128, 4, CH], BF16)
    KA_c = dscr("KA_c", [2, NCH, 128, 4, CH], BF16)
    KA_tm = dscr("KA_tm", [2, TOK, 512], BF16)
    LF_tm = dscr("LF_tm", [2, TOK, 512], F32)
    VA_tm = dscr("VA_tm", [TOK, 512], BF16)
    BQK_d = dscr("BQK_d", [64, 8, TOKP], F32)
    QK_c = dscr("QK_c", [NCH, 2, 64, 4, CH], BF16)
    VB_tm = dscr("VB_tm", [TOK, 512], BF16)
    GT_d = dscr("GT_d", [TOK, 16], F32)
    G_tm = dscr("G_tm", [TOK, D], BF16)
    O_d = [dscr("O_f", [TOK, D], BF16), dscr("O_b", [TOK, D], BF16)]
    XM = dscr("XM", [TOK, D], F32)
    X1 = dscr("X1", [TOK, D], F32)
    X2 = dscr("X2", [OWN, D], F32)
    QT_d = dscr("QT_d", [OWN // 128, 128, 8, 128], BF16)
    ADA_d = dscr("ADA_d", [2, 2, 6 * D], F32)

    ARB = 206 * 1024
    arena_t = es.enter_context(nc.sbuf_tensor("arena", [128, ARB // 2], BF16))
    ar = Arena(arena_t, ARB)
    banks = [es.enter_context(nc.psum_tensor("psb%d" % i, [128, 512], F32)) for i in range(8)]
    BK = ["bank%d" % i for i in range(8)]

    p = Prog(nc)

    def pv(i, shape, dt=F32, parts=128, off=0):
        nel = int(np.prod(shape))
        if dt == F32:
            v = banks[i][0:parts, off:off + nel]
        else:
            v = banks[i][0:parts, off:off + (nel + 1) // 2].bitcast(BF16)
        if len(shape) == 2:
            v = v.rearrange("p (a b) -> p a b", a=shape[0])
        elif len(shape) == 3:
            v = v.rearrange("p (a b c) -> p a b c", a=shape[0], b=shape[1])
        return v

    def sk(key):
        return ar.semmap[key]

    def load(dst, dkey, src, semkey, rkeys=(), q="sp"):
        p.dma(q, lambda e: [e.dma_start(out=dst, in_=src, allow_slow_non_contiguous=True)], rkeys, [dkey], semkey)

    def store(dst, dkeys, src, skey, semkey, q="pool"):
        p.dma(q, lambda e: [e.dma_start(out=dst, in_=src, allow_slow_non_contiguous=True)], [skey], dkeys, semkey)

    identf, k_identf = ar.alloc([128], F32, persist=True)
    identb, k_identb = ar.alloc([128], BF16, persist=True)
    cm, k_cm = ar.alloc([2, 4, 64], F32, parts=64, persist=True)
    epsc, k_eps = ar.alloc([1], F32, persist=True)
    onec, k_one = ar.alloc([1], F32, persist=True)
    ones64, k_ones64 = ar.alloc([64], F32, parts=64, persist=True)
    sel, k_sel = ar.alloc([2, 128], F32, parts=2, persist=True)
    modc, k_modc = ar.alloc([6, 8, 2], F32, persist=True)
    gm1, k_gm1 = ar.alloc([8, 2], F32, persist=True)
    gm2, k_gm2 = ar.alloc([8, 2], F32, persist=True)
    GG1, k_GG1 = ar.alloc([2, D], F32, persist=True)
    GG2, k_GG2 = ar.alloc([2, D], F32, persist=True)

    load(identf, k_identf, c_ident, "c0")
    p.op("dve", lambda e: e.tensor_copy(out=identb, in_=identf), [k_identf], [k_identb])
    load(cm, k_cm, c_cm, "c1")
    load(sel, k_sel, c_sel.rearrange("r k m -> k r m"), "c2")
    p.op("pool", lambda e: e.memset(epsc, EPS), [], [k_eps])
    p.op("pool", lambda e: e.memset(onec, 1.0), [], [k_one])
    p.op("pool", lambda e: e.memset(ones64, 1.0), [], [k_ones64])

    def rstd_from_ssq(ssq, k_ssq, out, k_out, n, parts=128):
        p.op("act", lambda e: e.activation(out=out, in_=ssq, func=AF.Sqrt, bias=epsc[0:parts, :],
                                           scale=1.0 / n), [k_ssq, k_eps], [k_out])
        p.op("dve", lambda e: e.reciprocal(out=out, in_=out), [k_out], [k_out])

    def ada_layer(l):
        ar.reset()
        scT, k_scT = ar.alloc([8, 2], F32)
        adas, k_adas = ar.alloc([6 * D], F32, parts=2)
        adab, k_adab = ar.alloc([6 * D], F32, parts=2)
        ngc, k_ngc = ar.alloc([4, 8], F32)
        ngb, k_ngb = ar.alloc([2, D], F32)
        wbuf = Buf(ar, 2, [8, 512], F32)
        p.dma("sp", lambda e: [e.dma_start(out=scT[:, :, r_], in_=cvec[r_].rearrange("(k q) -> q k", q=128),
                                           allow_slow_non_contiguous=True) for r_ in range(2)],
              [], [k_scT], "a0", ndma=2)
        p.op("act", lambda e: e.activation(out=scT, in_=scT, func=AF.Silu), [k_scT], [k_scT])
        load(adab, k_adab, ada_b[l:l + 1, :].to_broadcast([2, 6 * D]), "a1")
        p.dma("sp", lambda e: [e.dma_start(out=ngc, in_=norm_g[l].rearrange("v (k q) -> q v k", q=128),
                                           allow_slow_non_contiguous=True)], [], [k_ngc], "a2")
        for n in range(12):
            (wt, k_wt) = wbuf.next()
            load(wt, k_wt, ada_w[l, :, n * 512:(n + 1) * 512].rearrange("(k q) n -> q k n", q=128),
                 "aw%d" % (n % 2))
            bk = n % 2

            def mm(e, wt=wt, bk=bk):
                for k in range(8):
                    r = e.matmul(banks[bk][0:2, :], lhsT=scT[:, k, :], rhs=wt[:, k, :],
                                 start=(k == 0), stop=(k == 7))
                return r
            p.op("pe", mm, [k_scT, k_wt], [BK[bk]])
            p.op("dve", lambda e, bk=bk, n=n: e.tensor_tensor(
                out=adas[:, n * 512:(n + 1) * 512], in0=banks[bk][0:2, :],
                in1=adab[:, n * 512:(n + 1) * 512], op=ALU.add), [BK[bk], k_adab], [k_adas])
        if debug:
            store(ADA_d[l], ["ADA_d"], adas, k_adas, "dbg")
        def colmm(e):
            for v in range(6):
                for k in range(8):
                    c0 = v * D + k * 128
                    r = e.matmul(pv(2, [6, 8, 2])[:, v, k, :], lhsT=adas[:, c0:c0 + 128],
                                 rhs=identf[0:2, 0:2], start=True, stop=True)
            return r
        p.op("pe", colmm, [k_adas, k_identf], [BK[2]])
        p.op("dve", lambda e: e.tensor_copy(out=modc, in_=pv(2, [6, 8, 2])), [BK[2]], [k_modc])
        for (gm, k_gm, vsc, vng) in ((gm1, k_gm1, 1, 0), (gm2, k_gm2, 4, 2)):
            p.op("dve", lambda e, gm=gm, vsc=vsc: e.tensor_scalar(
                out=gm, in0=modc[:, vsc, :, :], scalar1=1.0, scalar2=None, op0=ALU.add),
                [k_modc], [k_gm])
            p.op("dve", lambda e, gm=gm, vng=vng: e.tensor_tensor(
                out=gm, in0=gm, in1=ngc[:, vng, :].unsqueeze(2).to_broadcast([128, 8, 2]), op=ALU.mult),
                [k_gm, k_ngc], [k_gm])
        for (GG, k_GG, vg, vng) in ((GG1, k_GG1, 2, 1), (GG2, k_GG2, 5, 3)):
            load(ngb[:, 0, :], k_ngb, norm_g[l, vng:vng + 1, :].to_broadcast([128, D]), "a3")
            load(ngb[:, 1, :], k_ngb, norm_g[l, vng:vng + 1, :].to_broadcast([128, D]), "a3")
            for r in range(2):
                for hf in range(2):
                    bk = 3 + hf
                    c0 = vg * D + hf * 512
                    p.op("pe", lambda e, bk=bk, c0=c0, r=r: e.matmul(
                        banks[bk][:, :], lhsT=sel[:, r, :], rhs=adas[:, c0:c0 + 512],
                        start=True, stop=True), [k_adas, k_sel], [BK[bk]])
                    p.op("dve", lambda e, bk=bk, GG=GG, r=r, hf=hf: e.tensor_tensor(
                        out=GG[:, r, hf * 512:(hf + 1) * 512], in0=banks[bk][:, :],
                        in1=ngb[:, r, hf * 512:(hf + 1) * 512], op=ALU.mult),
                        [BK[bk], k_ngb], [k_GG])
        p.barrier()

    def load_weight_bf16(dst, dkey, src_rows, kchunks, semkey):
        for k in range(kchunks):
            p.dma("pool", lambda e, k=k: [e.dma_start(out=dst[:, k, :], in_=src_rows[k * 128:(k + 1) * 128, :])],
                  [], [dkey], "W" + str(k % 4))

    def prep_tile(src_ap, hT, k_hT, col0, gm, k_gm, shv, r, bufs, xkeep=None):
        xb, sqb, ssb, xnb = bufs
        if xkeep is None:
            (xt, k_xt) = xb.next()
        else:
            (xt, k_xt) = xkeep
        (sq, k_sq) = sqb.next()
        (ss, k_ss) = ssb.next()
        (xn, k_xn) = xnb.next()
        load(xt, k_xt, src_ap, sk(k_xt))
        p.op("act", lambda e: e.activation(out=sq, in_=xt, func=AF.Square, accum_out=ss[:, 0:1]),
             [k_xt], [k_sq, k_ss])
        rstd_from_ssq(ss[:, 0:1], k_ss, ss[:, 1:2], k_ss, D)
        p.op("act", lambda e: e.activation(out=xn, in_=xt, func=AF.Copy, scale=ss[:, 1:2]),
             [k_xt, k_ss], [k_xn])

        def tr(e):
            for k in range(8):
                r_ = e.transpose(out=pv(k // 4, [4, 128])[:, k % 4, :], in_=xn[:, k * 128:(k + 1) * 128],
                                 identity=identf)
            return r_
        p.op("pe", tr, [k_xn, k_identf], [BK[0], BK[1]])
        for k in range(8):
            p.op("dve", lambda e, k=k: e.tensor_scalar(
                out=hT[:, k, col0:col0 + 128], in0=pv(k // 4, [4, 128])[:, k % 4, :],
                scalar1=gm[:, k, r:r + 1], scalar2=modc[:, shv, k, r:r + 1], op0=ALU.mult, op1=ALU.add),
                [BK[k // 4], k_gm, k_modc], [k_hT])
        return xt, k_xt

    def residual_out(py_banks, xt, k_xt, GG, k_GG, r, dst_ap, dkey, bufs, semkey):
        sqb, ssb, outb = bufs
        (sq, k_sq) = sqb.next()
        (ss, k_ss) = ssb.next()
        (xo, k_xo) = outb.next()
        for hf in range(2):
            bk = py_banks[hf]
            p.op("act", lambda e, bk=bk, hf=hf: e.activation(
                out=sq[:, 0:512], in_=banks[bk][:, :], func=AF.Square, accum_out=ss[:, 2 + hf:3 + hf]),
                [BK[bk]], [k_sq, k_ss])
        p.op("dve", lambda e: e.tensor_tensor(out=ss[:, 0:1], in0=ss[:, 2:3], in1=ss[:, 3:4], op=ALU.add),
             [k_ss], [k_ss])
        rstd_from_ssq(ss[:, 0:1], k_ss, ss[:, 1:2], k_ss, D)
        for hf in range(2):
            bk = py_banks[hf]
            p.op("dve", lambda e, bk=bk, hf=hf: e.scalar_tensor_tensor(
                out=xo[:, hf * 512:(hf + 1) * 512], in0=banks[bk][:, :], scalar=ss[:, 1:2],
                in1=GG[:, r, hf * 512:(hf + 1) * 512], op0=ALU.mult, op1=ALU.mult),
                [BK[bk], k_ss, k_GG], [k_xo])
        p.op("dve", lambda e: e.tensor_tensor(out=xo, in0=xo, in1=xt, op=ALU.add), [k_xo, k_xt], [k_xo])
        store(dst_ap, [dkey], xo, k_xo, sk(k_xo))

    def phase_A():
        ar.reset()
        WA, k_WA = ar.alloc([8, 4112], BF16)
        load_weight_bf16(WA, k_WA, ab_w_in, 8, "wA")
        lbf, k_lbf = ar.alloc([2, 3, 4], F32)
        lbb, k_lbb = ar.alloc([2, 3, 512], F32)
        omlb_c, k_omlbc = ar.alloc([2, 4], F32)
        lb_b, k_lb_b = ar.alloc([2, 512], F32)
        omlb_b, k_omlb_b = ar.alloc([2, 512], F32)
        gbb, k_gbb = ar.alloc([16], F32)
        zt, k_zt = ar.alloc([8, 4], F32, parts=64)
        p.dma("sp", lambda e: [e.dma_start(out=lbf, in_=hgrn_lb.rearrange("r l (h q) -> q r l h", q=128),
                                           allow_slow_non_contiguous=True)], [], [k_lbf], "l0")
        load(lbb, k_lbb, hgrn_lb.rearrange("r l c -> (r l c)").unsqueeze(0).to_broadcast([128, 3072])
             .rearrange("p (r l c) -> p r l c", r=2, l=3), "l1")
        load(gbb, k_gbb, ab_gate_b.unsqueeze(0).to_broadcast([128, 16]), "l2")
        for (t, kt_, o1, ko1, o2, ko2) in ((lbf, k_lbf, omlb_c, k_omlbc, None, None),
                                           (lbb, k_lbb, omlb_b, k_omlb_b, lb_b, k_lb_b)):
            p.op("act", lambda e, t=t: e.activation(out=t, in_=t, func=AF.Exp), [kt_], [kt_])
            p.op("dve", lambda e, t=t, o1=o1: e.tensor_tensor(out=o1, in0=t[:, :, 0, :], in1=t[:, :, 1, :], op=ALU.add),
                 [kt_], [ko1])
            p.op("dve", lambda e, t=t, o1=o1: e.tensor_tensor(out=o1, in0=o1, in1=t[:, :, 2, :], op=ALU.add),
                 [kt_, ko1], [ko1])
            p.op("dve", lambda e, o1=o1: e.reciprocal(out=o1, in_=o1), [ko1], [ko1])
            p.op("dve", lambda e, t=t, o1=o1: e.tensor_tensor(out=o1, in0=o1, in1=t[:, :, 0, :], op=ALU.mult),
                 [kt_, ko1], [ko1])
            if o2 is not None:
                p.op("dve", lambda e, o1=o1, o2=o2: e.tensor_copy(out=o2, in_=o1), [ko1], [ko2])
            p.op("dve", lambda e, o1=o1: e.tensor_scalar(out=o1, in0=o1, scalar1=-1.0, scalar2=1.0,
                                                         op0=ALU.mult, op1=ALU.add), [ko1], [ko1])
        p.op("pool", lambda e: e.memset(zt, 0.0), [], [k_zt])
        for i, pos in enumerate((0, NCTX + 1, NCTX + 2, TOKP - 1)):
            store(BQK_d[:, :, pos:pos + 1], ["BQK_d"], zt[:, :, 0:1], k_zt, "z%d" % i, q="sp")

        NT = 256
        xb = Buf(ar, 2, [D], F32)
        sqb = Buf(ar, 1, [D], BF16)
        ssb = Buf(ar, 2, [4], F32)
        xnb = Buf(ar, 2, [D], F32)
        hTb = Buf(ar, 2, [8, NT], BF16)
        qst = Buf(ar, 2, [NT // CH, 4, CH], BF16)
        kst = Buf(ar, 2, [NT // CH, 4, CH], BF16)
        sgt = Buf(ar, 2, [NT], F32)
        bst = Buf(ar, 2, [8, NT], F32, parts=64)
        vst = Buf(ar, 2, [512], BF16)
        gst = Buf(ar, 2, [D], BF16)
        vbst = Buf(ar, 2, [512], BF16)
        sst = Buf(ar, 2, [512], F32)
        ust = Buf(ar, 2, [512], F32)
        lfst = Buf(ar, 2, [512], F32)
        ktst = Buf(ar, 2, [512], BF16)
        gtst = Buf(ar, 2, [16], F32)
        gt2 = Buf(ar, 2, [16], F32)
        fmb = [2, 3]
        tmb = [4, 5, 6]
        fm_i = [0]
        tm_i = [0]

        def fm_slot():
            i = fm_i[0]
            fm_i[0] = (i + 1) % 4
            return fmb[i // 2], (i % 2) * 256

        def tm_bank():
            i = tm_i[0]
            tm_i[0] = (i + 1) % 3
            return tmb[i]

        def prep_st(st):
            tok0 = st * NT
            r = 1 if tok0 < NCTX else 0
            (hT, k_hT) = hTb.next()
            for j in range(NT // 128):
                t0 = tok0 + j * 128
                prep_tile(xs[t0:t0 + 128, :], hT, k_hT, j * 128, gm1, k_gm1, 0, r, (xb, sqb, ssb, xnb))
            return hT, k_hT

        nxt = prep_st(0)
        for st in range(TOK // NT):
            tok0 = st * NT
            (hT, k_hT) = nxt if (PIPE_PREP or st == 0) else prep_st(st)
            if PIPE_PREP and st + 1 < TOK // NT:
                nxt = prep_st(st + 1)
            c0 = tok0 // CH
            nchk = NT // CH

            def fm_mm(bk, off, col0, hT=hT):
                def f(e):
                    for k in range(8):
                        r_ = e.matmul(banks[bk][:, off:off + NT], lhsT=WA[:, k, col0:col0 + 128], rhs=hT[:, k, :],
                                      start=(k == 0), stop=(k == 7))
                    return r_
                return f
            (qs, k_qs) = qst.next()
            for h in range(4):
                bk, off = fm_slot()
                p.op("pe", fm_mm(bk, off, h * 128), [k_hT, k_WA], [BK[bk]])
                p.op("act", lambda e, bk=bk, off=off, h=h, qs=qs: e.activation(
                    out=qs[:, :, h, :], in_=banks[bk][:, off:off + NT].rearrange("q (c t) -> q c t", t=CH),
                    func=AF.Copy), [BK[bk]], [k_qs])
            store(QA_c[c0:c0 + nchk].rearrange("c q h t -> q c (h t)"), ["QA_c"],
                  qs.rearrange("q c h t -> q c (h t)"), k_qs, sk(k_qs))
            for d in range(2):
                (ks, k_ks) = kst.next()
                for h in range(4):
                    bk, off = fm_slot()
                    (sg, k_sg) = sgt.next()
                    p.op("pe", fm_mm(bk, off, 1536 + d * 512 + h * 128), [k_hT, k_WA], [BK[bk]])
                    p.op("act", lambda e, bk=bk, off=off, sg=sg: e.activation(
                        out=sg, in_=banks[bk][:, off:off + NT], func=AF.Sigmoid, scale=-1.0), [BK[bk]], [k_sg])
                    p.op("dve", lambda e, sg=sg, ks=ks, d=d, h=h: e.tensor_scalar(
                        out=ks[:, :, h, :], in0=sg.rearrange("q (c t) -> q c t", t=CH),
                        scalar1=omlb_c[:, d, h:h + 1], scalar2=None, op0=ALU.mult),
                        [k_sg, k_omlbc], [k_ks])
                store(KA_c[d, c0:c0 + nchk].rearrange("c q h t -> q c (h t)"), ["KA_c"],
                      ks.rearrange("q c h t -> q c (h t)"), k_ks, sk(k_ks))
            (bs, k_bs) = bst.next()
            for g in range(8):
                bk, off = fm_slot()

                def f(e, bk=bk, off=off, g=g, hT=hT):
                    for k in range(8):
                        r_ = e.matmul(banks[bk][0:64, off:off + NT], lhsT=WA[:, k, 2560 + g * 64:2560 + (g + 1) * 64],
                                      rhs=hT[:, k, :], start=(k == 0), stop=(k == 7))
                    return r_
                p.op("pe", f, [k_hT, k_WA], [BK[bk]])
                p.op("act", lambda e, bk=bk, off=off, g=g, bs=bs: e.activation(
                    out=bs[:, g, :], in_=banks[bk][0:64, off:off + NT], func=AF.Copy), [BK[bk]], [k_bs])
            store(BQK_d[:, :, bpos(tok0):bpos(tok0) + NT], ["BQK_d"], bs, k_bs, sk(k_bs))
            for j in range(NT // 128):
                t0 = tok0 + j * 128

                def tm_mm(bk, col0, ncol=512, j=j, hT=hT):
                    def f(e):
                        for k in range(8):
                            r_ = e.matmul(banks[bk][:, 0:ncol], lhsT=hT[:, k, j * 128:(j + 1) * 128],
                                          rhs=WA[:, k, col0:col0 + ncol], start=(k == 0), stop=(k == 7))
                        return r_
                    return f
                bk = tm_bank()
                (vs, k_vs) = vst.next()
                p.op("pe", tm_mm(bk, 512), [k_hT, k_WA], [BK[bk]])
                p.op("act", lambda e, bk=bk, vs=vs: e.activation(out=vs, in_=banks[bk][:, :], func=AF.Copy),
                     [BK[bk]], [k_vs])
                store(VA_tm[t0:t0 + 128, :], ["VA_tm"], vs, k_vs, sk(k_vs))
                (gs, k_gs) = gst.next()
                bk = tm_bank()
                p.op("pe", tm_mm(bk, 1024), [k_hT, k_WA], [BK[bk]])
                p.op("act", lambda e, bk=bk, gs=gs: e.activation(out=gs[:, 0:512], in_=banks[bk][:, :], func=AF.Silu),
                     [BK[bk]], [k_gs])
                bk = tm_bank()
                p.op("pe", tm_mm(bk, 3584), [k_hT, k_WA], [BK[bk]])
                p.op("act", lambda e, bk=bk, gs=gs: e.activation(out=gs[:, 512:1024], in_=banks[bk][:, :],
                                                                 func=AF.Sigmoid), [BK[bk]], [k_gs])
                store(G_tm[t0:t0 + 128, :], ["G_tm"], gs, k_gs, sk(k_gs))
                bk = tm_bank()
                (vb, k_vb) = vbst.next()
                p.op("pe", tm_mm(bk, 3072), [k_hT, k_WA], [BK[bk]])
                p.op("act", lambda e, bk=bk, vb=vb: e.activation(out=vb, in_=banks[bk][:, :], func=AF.Copy),
                     [BK[bk]], [k_vb])
                store(VB_tm[t0:t0 + 128, :], ["VB_tm"], vb, k_vb, sk(k_vb))
                for d in range(2):
                    bk = tm_bank()
                    (s_, k_s) = sst.next()
                    (u_, k_u) = ust.next()
                    (lf, k_lf) = lfst.next()
                    (kt, k_kt) = ktst.next()
                    p.op("pe", tm_mm(bk, 1536 + d * 512), [k_hT, k_WA], [BK[bk]])
                    p.op("act", lambda e, bk=bk, s_=s_: e.activation(out=s_, in_=banks[bk][:, :], func=AF.Sigmoid),
                         [BK[bk]], [k_s])
                    p.op("dve", lambda e, s_=s_, u_=u_, d=d: e.tensor_tensor(out=u_, in0=s_, in1=omlb_b[:, d, :],
                                                                             op=ALU.mult), [k_s, k_omlb_b], [k_u])
                    p.op("dve", lambda e, s_=s_, u_=u_, d=d: e.tensor_tensor(out=s_, in0=u_, in1=lb_b[:, d, :],
                                                                             op=ALU.add), [k_u, k_lb_b], [k_s])
                    p.op("act", lambda e, s_=s_, lf=lf: e.activation(out=lf, in_=s_, func=AF.Ln), [k_s], [k_lf])
                    store(LF_tm[d, t0:t0 + 128, :], ["LF_tm"], lf, k_lf, sk(k_lf))
                    p.op("dve", lambda e, u_=u_, kt=kt, d=d: e.tensor_tensor(out=kt, in0=omlb_b[:, d, :], in1=u_,
                                                                             op=ALU.subtract), [k_u, k_omlb_b], [k_kt])
                    store(KA_tm[d, t0:t0 + 128, :], ["KA_tm"], kt, k_kt, sk(k_kt))
                bk = tm_bank()
                (g1_, k_g1) = gtst.next()
                (g2_, k_g2) = gt2.next()
                p.op("pe", tm_mm(bk, 4096, 16), [k_hT, k_WA], [BK[bk]])
                p.op("dve", lambda e, bk=bk, g1_=g1_: e.tensor_tensor(out=g1_, in0=banks[bk][:, 0:16], in1=gbb,
                                                                       op=ALU.add), [BK[bk], k_gbb], [k_g1])
                p.op("act", lambda e, g1_=g1_, g2_=g2_: e.activation(out=g2_, in_=g1_, func=AF.Exp, scale=-1.0),
                     [k_g1], [k_g2])
                p.op("act", lambda e, g2_=g2_: e.activation(out=g2_, in_=g2_, func=AF.Ln, bias=onec, scale=1.0),
                     [k_g2, k_one], [k_g2])
                p.op("dve", lambda e, g1_=g1_, g2_=g2_: e.tensor_scalar(
                    out=g1_.rearrange("q (a b) -> q a b", a=2)[:, :, 4:8],
                    in0=g2_.rearrange("q (a b) -> q a b", a=2)[:, :, 4:8],
                    scalar1=-1.0, scalar2=None, op0=ALU.mult), [k_g1, k_g2], [k_g1])
                store(GT_d[t0:t0 + 128, :], ["GT_d"], g1_, k_g1, sk(k_g1))
        p.barrier()


    def phase_A2():
        ar.reset()
        NT = 256
        cw2, k_cw2 = ar.alloc([4, 3], F32)
        p.dma("sp", lambda e: [e.dma_start(
            out=cw2[gg * 64:(gg + 1) * 64, :, w_],
            in_=ab_conv[w_].rearrange("(j gg q) -> gg q j", gg=2, q=64)[gg],
            allow_slow_non_contiguous=True) for w_ in range(3) for gg in range(2)],
            [], [k_cw2], "b0", ndma=6)
        xb2 = Buf(ar, 2, [4, NT + 2], F32)
        acb = Buf(ar, 2, [4, NT], F32)
        qkb2 = Buf(ar, 2, [NT // CH, 4, CH], BF16)
        for st in range(TOK // NT):
            tok0 = st * NT
            c0 = tok0 // CH
            pos0 = bpos(tok0)
            (X, k_X) = xb2.next()
            (acc, k_acc) = acb.next()
            (qo, k_qo) = qkb2.next()
            p.dma("sp", lambda e, X=X, pos0=pos0: [e.dma_start(
                out=X[gg * 64:(gg + 1) * 64, :, :],
                in_=BQK_d.rearrange("q (j gg) t -> gg q j t", gg=2)[gg, :, :, pos0 - 1:pos0 + NT + 1],
                allow_slow_non_contiguous=True) for gg in range(2)], ["BQK_d"], [k_X], sk(k_X), ndma=2)
            for j in range(4):
                p.op("dve", lambda e, X=X, acc=acc, j=j: e.tensor_scalar(
                    out=acc[:, j, :], in0=X[:, j, 0:NT], scalar1=cw2[:, j, 0:1], scalar2=None, op0=ALU.mult),
                    [k_X, k_cw2], [k_acc])
                for w in (1, 2):
                    p.op("dve", lambda e, X=X, acc=acc, j=j, w=w: e.scalar_tensor_tensor(
                        out=acc[:, j, :], in0=X[:, j, w:w + NT], scalar=cw2[:, j, w:w + 1], in1=acc[:, j, :],
                        op0=ALU.mult, op1=ALU.add), [k_X, k_cw2, k_acc], [k_acc])
            p.op("act", lambda e, acc=acc, qo=qo: e.activation(
                out=qo, in_=acc.rearrange("q j (c t) -> q c j t", t=CH), func=AF.Silu), [k_acc], [k_qo])
            p.dma("pool", lambda e, qo=qo, c0=c0: [e.dma_start(
                out=QK_c[c0:c0 + NT // CH, gg].rearrange("c q j t -> q c (j t)"),
                in_=qo[gg * 64:(gg + 1) * 64].rearrange("q c j t -> q c (j t)"),
                allow_slow_non_contiguous=True) for gg in range(2)], [k_qo], ["QK_c"], sk(k_qo), ndma=2)
        p.barrier()

    def phase_B():
        ar.reset()
        Sf = [ar.alloc([4, 128], F32) for _ in range(2)]
        Sb = [ar.alloc([4, 128], BF16) for _ in range(2)]
        Cf = [ar.alloc([4, 132], F32, parts=64) for _ in range(2)]
        Cb = [ar.alloc([4, 132], BF16, parts=64) for _ in range(2)]
        for (t, k) in Sf + Sb + Cf + Cb:
            p.op("pool", lambda e, t=t: e.memset(t, 0.0), [], [k])
        lfb = Buf(ar, 2, [512], F32, parts=64)
        qfb = Buf(ar, 2, [4, CH], BF16)
        kfb = Buf(ar, 2, [4, CH], BF16)
        ktb = Buf(ar, 2, [512], BF16, parts=64)
        vtb = Buf(ar, 2, [512], BF16, parts=64)
        E1b = Buf(ar, 2, [4, 128], F32)
        E2b = Buf(ar, 2, [4, CH], F32)
        EUb = Buf(ar, 2, [512], F32, parts=64)
        qqb = Buf(ar, 2, [4, 128], BF16)
        kkb = Buf(ar, 2, [4, CH], BF16)
        kSb = Buf(ar, 2, [512], BF16, parts=64)
        ATb = Buf(ar, 2, [4, CH], BF16, parts=64)
        osb = Buf(ar, 2, [512], BF16, parts=64)
        Vxb = Buf(ar, 2, [4, 132], BF16, parts=64)
        gtb = Buf(ar, 2, [16], F32, parts=64)
        qkb = Buf(ar, 2, [8, CH], BF16, parts=64)
        argb = Buf(ar, 2, [16], F32, parts=64)
        EXb = Buf(ar, 2, [16], F32, parts=64)
        ATmb = Buf(ar, 2, [4, CH], BF16, parts=64)
        kSmb = Buf(ar, 2, [4, CH], BF16, parts=64)
        adb = Buf(ar, 2, [8], F32, parts=64)
        omb = Buf(ar, 2, [512], BF16, parts=64)
        for (t, k) in Vxb.slots:
            p.op("pool", lambda e, t=t: e.memset(t, 1.0), [], [k])

        def qpos(h):
            return (h % 2) * 4 + h // 2

        def kpos(h):
            return (h % 2) * 4 + 2 + h // 2

        for it in range(NCH):
            for d in range(2):
                if d == 0:
                    c = it
                else:
                    c = (3 - it) if it < 4 else (NCH + 3 - it)
                tok0 = c * CH
                tl = CH - 1 if d == 0 else 0
                (lf, k_lf) = lfb.next()
                (qf, k_qf) = qfb.next()
                (kf, k_kf) = kfb.next()
                (kt, k_kt) = ktb.next()
                (vt, k_vt) = vtb.next()
                load(lf, k_lf, LF_tm[d, tok0:tok0 + CH, :], sk(k_lf), ["LF_tm"])
                load(qf, k_qf, QA_c[c], sk(k_qf), ["QA_c"])
                load(kf, k_kf, KA_c[d, c], sk(k_kf), ["KA_c"])
                load(kt, k_kt, KA_tm[d, tok0:tok0 + CH, :], sk(k_kt), ["KA_tm"])
                load(vt, k_vt, VA_tm[tok0:tok0 + CH, :], sk(k_vt), ["VA_tm"])
                (E1, k_E1) = E1b.next()
                (E2, k_E2) = E2b.next()
                (EU, k_EU) = EUb.next()
                (qq, k_qq) = qqb.next()
                (kk, k_kk) = kkb.next()
                (kS, k_kS) = kSb.next()
                (AT, k_AT) = ATb.next()
                (os_, k_os) = osb.next()

                def p1(e, lf=lf, d=d):
                    for h in range(4):
                        r_ = e.matmul(pv(0, [4, 128])[:, h, :], lhsT=lf[:, h * 128:(h + 1) * 128],
                                      rhs=cm[:, d, 0:2, :], start=True, stop=True)
                    return r_
                p.op("pe", p1, [k_lf, k_cm], [BK[0]])
                p.op("pe", lambda e, lf=lf, d=d: e.matmul(banks[1][0:64, :], lhsT=cm[:, d, 2, :], rhs=lf,
                                                          start=True, stop=True), [k_lf, k_cm], [BK[1]])
                p.op("act", lambda e, E1=E1: e.activation(out=E1, in_=pv(0, [4, 128]), func=AF.Exp), [BK[0]], [k_E1])
                p.op("act", lambda e, E2=E2: e.activation(out=E2, in_=pv(0, [4, 128])[:, :, 0:CH], func=AF.Exp,
                                                          scale=-1.0), [BK[0]], [k_E2])
                p.op("act", lambda e, EU=EU: e.activation(out=EU, in_=banks[1][0:64, :], func=AF.Exp), [BK[1]], [k_EU])
                p.op("dve", lambda e, qq=qq, E1=E1, qf=qf: e.tensor_tensor(
                    out=qq.rearrange("q h (a t) -> q h a t", a=2), in0=E1.rearrange("q h (a t) -> q h a t", a=2),
                    in1=qf.unsqueeze(2).to_broadcast([128, 4, 2, CH]), op=ALU.mult), [k_E1, k_qf], [k_qq])
                p.op("dve", lambda e, kk=kk, E2=E2, kf=kf: e.tensor_tensor(out=kk, in0=E2, in1=kf, op=ALU.mult),
                     [k_E2, k_kf], [k_kk])
                p.op("dve", lambda e, kS=kS, EU=EU, kt=kt: e.tensor_tensor(out=kS, in0=EU, in1=kt, op=ALU.mult),
                     [k_EU, k_kt], [k_kS])

                def p3(e, kk=kk, qq=qq):
                    for h in range(4):
                        r_ = e.matmul(pv(2, [4, CH], parts=64)[:, h, :], lhsT=kk[:, h, :], rhs=qq[:, h, 0:CH],
                                      start=True, stop=True)
                    return r_
                p.op("pe", p3, [k_kk, k_qq], [BK[2]])
                p.op("dve", lambda e, AT=AT, d=d: e.tensor_tensor(
                    out=AT, in0=pv(2, [4, CH], parts=64),
                    in1=cm[:, d, 3, :].unsqueeze(1).to_broadcast([64, 4, CH]), op=ALU.mult), [BK[2], k_cm], [k_AT])

                def p4(e, AT=AT, vt=vt, qq=qq, d=d):
                    for h in range(4):
                        e.matmul(banks[3][0:64, h * 128:(h + 1) * 128], lhsT=AT[:, h, :],
                                 rhs=vt[:, h * 128:(h + 1) * 128], start=True, stop=False)
                        r_ = e.matmul(banks[3][0:64, h * 128:(h + 1) * 128], lhsT=qq[:, h, CH:2 * CH],
                                      rhs=Sb[d][0][:, h, :], start=False, stop=True)
                    return r_
                p.op("pe", p4, [k_AT, k_vt, k_qq, Sb[d][1]], [BK[3]])

                def p5(e, kS=kS, vt=vt):
                    for h in range(4):
                        r_ = e.matmul(pv(4, [4, 128])[:, h, :], lhsT=kS[:, h * 128:(h + 1) * 128],
                                      rhs=vt[:, h * 128:(h + 1) * 128], start=True, stop=True)
                    return r_
                p.op("pe", p5, [k_kS, k_vt], [BK[4]])
                p.op("act", lambda e, os_=os_: e.activation(out=os_, in_=banks[3][0:64, :], func=AF.Copy),
                     [BK[3]], [k_os])
                store(O_d[d][tok0:tok0 + CH, 0:512], ["O%d" % d], os_, k_os, sk(k_os))
                for h in range(4):
                    p.op("dve", lambda e, h=h, d=d, E1=E1, tl=tl: e.scalar_tensor_tensor(
                        out=Sf[d][0][:, h, :], in0=Sf[d][0][:, h, :], scalar=E1[:, h, CH + tl:CH + tl + 1],
                        in1=pv(4, [4, 128])[:, h, :], op0=ALU.mult, op1=ALU.add),
                        [Sf[d][1], k_E1, BK[4]], [Sf[d][1]])
                p.op("act", lambda e, d=d: e.activation(out=Sb[d][0], in_=Sf[d][0], func=AF.Copy),
                     [Sf[d][1]], [Sb[d][1]])

                (Vx, k_Vx) = Vxb.next()
                (gt, k_gt) = gtb.next()
                (qk, k_qk) = qkb.next()
                (arg, k_arg) = argb.next()
                (EX, k_EX) = EXb.next()
                (ATm, k_ATm) = ATmb.next()
                (kSm, k_kSm) = kSmb.next()
                (ad, k_ad) = adb.next()
                (om, k_om) = omb.next()
                load(qk.rearrange("q (gg j) t -> q gg (j t)", gg=2), k_qk,
                     QK_c[c].rearrange("gg q j t -> q gg (j t)"), sk(k_qk), ["QK_c"])
                load(Vx[:, :, 0:128], k_Vx, VB_tm[tok0:tok0 + CH, :].rearrange("t (h e) -> t h e", h=4),
                     sk(k_Vx), ["VB_tm"])
                load(gt, k_gt, GT_d[tok0:tok0 + CH, :], sk(k_gt), ["GT_d"])

                lfc = gt[:, 8 * d + 4:8 * d + 8]
                igc = gt[:, 8 * d:8 * d + 4]

                def pg(e, lfc=lfc, d=d):
                    e.matmul(banks[6][0:64, 0:4], lhsT=cm[:, d, 1, :], rhs=lfc, start=True, stop=True)
                    e.matmul(banks[6][0:64, 4:8], lhsT=cm[:, d, 2, :], rhs=lfc, start=True, stop=True)
                    return e.matmul(banks[6][0:64, 8:12], lhsT=ones64, rhs=lfc, start=True, stop=True)
                p.op("pe", pg, [k_gt, k_cm, k_ones64], [BK[6]])
                p.op("dve", lambda e, arg=arg, igc=igc: e.tensor_tensor(out=arg[:, 0:4], in0=igc,
                                                                        in1=banks[6][0:64, 0:4], op=ALU.subtract),
                     [k_gt, BK[6]], [k_arg])
                p.op("dve", lambda e, arg=arg, igc=igc: e.tensor_tensor(out=arg[:, 4:8], in0=banks[6][0:64, 4:8],
                                                                        in1=igc, op=ALU.add), [k_gt, BK[6]], [k_arg])
                p.op("dve", lambda e, arg=arg: e.tensor_scalar(out=arg[:, 8:12], in0=banks[6][0:64, 0:4],
                                                               scalar1=-1.0, scalar2=LN8, op0=ALU.mult, op1=ALU.add),
                     [BK[6]], [k_arg])
                p.op("dve", lambda e, arg=arg: e.tensor_copy(out=arg[:, 12:16], in_=banks[6][0:64, 8:12]),
                     [BK[6]], [k_arg])
                p.op("act", lambda e, arg=arg, EX=EX: e.activation(out=EX, in_=arg, func=AF.Exp), [k_arg], [k_EX])

                def pst(e, qk=qk):
                    for h in range(4):
                        e.matmul(pv(7, [4, CH], parts=64)[:, h, :], lhsT=qk[:, kpos(h), :], rhs=qk[:, qpos(h), :],
                                 start=True, stop=True)
                    for h in range(4):
                        r_ = e.transpose(out=pv(7, [4, CH], BF16, parts=64, off=256)[:, h, :], in_=qk[:, kpos(h), :],
                                         identity=identb[0:64, 0:64])
                    return r_
                p.op("pe", pst, [k_qk, k_identb], [BK[7]])
                for h in range(4):
                    p.op("dve", lambda e, h=h, ATm=ATm, EX=EX, d=d: e.scalar_tensor_tensor(
                        out=ATm[:, h, :], in0=pv(7, [4, CH], parts=64)[:, h, :], scalar=EX[:, h:h + 1],
                        in1=cm[:, d, 3, :], op0=ALU.mult, op1=ALU.mult), [BK[7], k_EX, k_cm], [k_ATm])
                p.op("dve", lambda e, kSm=kSm, EX=EX: e.tensor_tensor(
                    out=kSm, in0=pv(7, [4, CH], BF16, parts=64, off=256),
                    in1=EX[:, 4:8].unsqueeze(2).to_broadcast([64, 4, CH]), op=ALU.mult), [BK[7], k_EX], [k_kSm])

                def pn(e, ATm=ATm, Vx=Vx, qk=qk, d=d):
                    for h in range(4):
                        bk = 0 if h < 2 else 1
                        o = pv(bk, [2, 132], parts=64)[:, h % 2, 0:129]
                        e.matmul(o, lhsT=ATm[:, h, :], rhs=Vx[:, h, 0:129], start=True, stop=False)
                        r_ = e.matmul(o, lhsT=qk[:, qpos(h), :], rhs=Cb[d][0][:, h, 0:129], start=False, stop=True)
                    return r_
                p.op("pe", pn, [k_ATm, k_Vx, k_qk, Cb[d][1]], [BK[0], BK[1]])

                def pc(e, kSm=kSm, Vx=Vx):
                    for h in range(4):
                        bk = 2 if h < 2 else 4
                        r_ = e.matmul(pv(bk, [2, 132], parts=64)[:, h % 2, 0:129], lhsT=kSm[:, h, :],
                                      rhs=Vx[:, h, 0:129], start=True, stop=True)
                    return r_
                p.op("pe", pc, [k_kSm, k_Vx], [BK[2], BK[4]])
                for hb in range(2):
                    p.op("act", lambda e, hb=hb, ad=ad: e.activation(
                        out=ad[:, 2 * hb:2 * hb + 2], in_=pv(hb, [2, 132], parts=64)[:, :, 128], func=AF.Abs),
                        [BK[hb]], [k_ad])
                p.op("dve", lambda e, ad=ad, EX=EX: e.tensor_tensor(out=ad[:, 0:4], in0=ad[:, 0:4], in1=EX[:, 8:12],
                                                                    op=ALU.max), [k_ad, k_EX], [k_ad])
                p.op("dve", lambda e, ad=ad: e.reciprocal(out=ad[:, 4:8], in_=ad[:, 0:4]), [k_ad], [k_ad])
                for h in range(4):
                    p.op("act", lambda e, h=h, om=om, ad=ad: e.activation(
                        out=om[:, h * 128:(h + 1) * 128], in_=pv(h // 2, [2, 132], parts=64)[:, h % 2, 0:128],
                        func=AF.Copy, scale=ad[:, 4 + h:5 + h]), [BK[h // 2], k_ad], [k_om])
                store(O_d[d][tok0:tok0 + CH, 512:1024], ["O%d" % d], om, k_om, sk(k_om))
                for h in range(4):
                    bk = 2 if h < 2 else 4
                    p.op("dve", lambda e, h=h, d=d, EX=EX, bk=bk: e.scalar_tensor_tensor(
                        out=Cf[d][0][:, h, 0:129], in0=Cf[d][0][:, h, 0:129], scalar=EX[:, 12 + h:13 + h],
                        in1=pv(bk, [2, 132], parts=64)[:, h % 2, 0:129], op0=ALU.mult, op1=ALU.add),
                        [Cf[d][1], k_EX, BK[bk]], [Cf[d][1]])
                p.op("act", lambda e, d=d: e.activation(out=Cb[d][0], in_=Cf[d][0], func=AF.Copy),
                     [Cf[d][1]], [Cb[d][1]])
        p.barrier()

    def phase_C1():
        ar.reset()
        WO, k_WO = ar.alloc([8, D], BF16)
        load_weight_bf16(WO, k_WO, ab_w_out, 8, "wO")
        OG, k_OG = ar.alloc([D], F32)
        load(OG, k_OG, ab_out_g.unsqueeze(0).to_broadcast([128, D]), "c1og")
        ofb = Buf(ar, 2, [D], BF16)
        obb = Buf(ar, 2, [D], BF16)
        o32b = Buf(ar, 2, [D], F32)
        gb = Buf(ar, 2, [D], BF16)
        xb = Buf(ar, 2, [D], F32)
        sqb = Buf(ar, 2, [D], F32)
        s8b = Buf(ar, 2, [16], F32)
        omb = Buf(ar, 2, [D], BF16)
        oTb = Buf(ar, 2, [8, 128], BF16)
        ssb = Buf(ar, 2, [4], F32)
        xob = Buf(ar, 2, [D], F32)
        for t in range(TOK // 128):
            t0 = t * 128
            r = 1 if t0 < NCTX else 0
            (of, k_of) = ofb.next()
            (ob, k_ob) = obb.next()
            (g, k_g) = gb.next()
            (xt, k_xt) = xb.next()
            (sq, k_sq) = sqb.next()
            (s8, k_s8) = s8b.next()
            (om, k_om) = omb.next()
            (oT, k_oT) = oTb.next()
            load(of, k_of, O_d[0][t0:t0 + 128, :], sk(k_of), ["O0"])
            load(ob, k_ob, O_d[1][t0:t0 + 128, :], sk(k_ob), ["O1"])
            load(g, k_g, G_tm[t0:t0 + 128, :], sk(k_g), ["G_tm"])
            load(xt, k_xt, xs[t0:t0 + 128, :], sk(k_xt))
            (o32, k_o32) = o32b.next()
            p.op("dve", lambda e, of=of, ob=ob, o32=o32: e.tensor_tensor(out=o32, in0=of, in1=ob, op=ALU.add),
                 [k_of, k_ob], [k_o32])
            of, k_of = o32, k_o32
            p.op("act", lambda e, of=of, sq=sq: e.activation(out=sq, in_=of, func=AF.Square), [k_of], [k_sq])
            p.op("dve", lambda e, sq=sq, s8=s8: e.tensor_reduce(
                out=s8[:, 0:8], in_=sq.rearrange("q (h e) -> q h e", h=8), axis=AX.X, op=ALU.add), [k_sq], [k_s8])
            rstd_from_ssq(s8[:, 0:8], k_s8, s8[:, 8:16], k_s8, 128)
            p.op("dve", lambda e, of=of, s8=s8: e.tensor_tensor(
                out=of.rearrange("q (h e) -> q h e", h=8), in0=of.rearrange("q (h e) -> q h e", h=8),
                in1=s8[:, 8:16].unsqueeze(2).to_broadcast([128, 8, 128]), op=ALU.mult), [k_of, k_s8], [k_of])
            p.op("dve", lambda e, of=of: e.tensor_tensor(out=of, in0=of, in1=OG, op=ALU.mult), [k_of, k_OG], [k_of])
            p.op("dve", lambda e, of=of, g=g, om=om: e.tensor_tensor(out=om, in0=of, in1=g, op=ALU.mult),
                 [k_of, k_g], [k_om])

            def tr(e, om=om):
                for k in range(8):
                    r_ = e.transpose(out=pv(0, [8, 128], BF16)[:, k, :], in_=om[:, k * 128:(k + 1) * 128],
                                     identity=identb)
                return r_
            p.op("pe", tr, [k_om, k_identb], [BK[0]])
            p.op("act", lambda e, oT=oT: e.activation(out=oT, in_=pv(0, [8, 128], BF16), func=AF.Copy),
                 [BK[0]], [k_oT])
            pyb = (1 + 2 * (t % 2), 2 + 2 * (t % 2))
            for hf in range(2):
                def mm(e, hf=hf, oT=oT, bk=pyb[hf]):
                    for k in range(8):
                        r_ = e.matmul(banks[bk][:, :], lhsT=oT[:, k, :], rhs=WO[:, k, hf * 512:(hf + 1) * 512],
                                      start=(k == 0), stop=(k == 7))
                    return r_
                p.op("pe", mm, [k_oT, k_WO], [BK[pyb[hf]]])
            residual_out(pyb, xt, k_xt, GG1, k_GG1, r, XM[t0:t0 + 128, :], "XM", (sqb, ssb, xob), "sxm")
        p.barrier()

    def phase_FFN(l, src, skey, dst, dkey, ntok, ctx_tokens):
        ar.reset()
        W1, k_W1 = ar.alloc([8, 2 * DFF], BF16)
        W2, k_W2 = ar.alloc([22, D], BF16)
        load_weight_bf16(W1, k_W1, ffn_w_in[l], 8, "w1")
        load_weight_bf16(W2, k_W2, ffn_w_out[l], 22, "w2")
        NT = 256
        xb = Buf(ar, 4, [D], F32)
        sqb = Buf(ar, 1, [D], BF16)
        ssb = Buf(ar, 4, [4], F32)
        xnb = Buf(ar, 1, [D], F32)
        hTb = Buf(ar, 2, [8, NT], BF16)
        aTb = Buf(ar, 1, [22, NT], BF16)
        sgb = Buf(ar, 2, [NT], F32)
        xob = Buf(ar, 1, [D], F32)
        fmb = [2, 3, 4, 5]
        fm_i = [0]

        def fm_slot():
            i = fm_i[0]
            fm_i[0] = (i + 1) % 8
            return fmb[i // 2], (i % 2) * 256

        def prep_st(st):
            tok0 = st * NT
            r = 1 if tok0 < ctx_tokens else 0
            (hT, k_hT) = hTb.next()
            xts = []
            for j in range(NT // 128):
                t0 = tok0 + j * 128
                xts.append(prep_tile(src[t0:t0 + 128, :], hT, k_hT, j * 128, gm2, k_gm2, 3, r,
                                     (xb, sqb, ssb, xnb)))
            return hT, k_hT, xts

        nxt = prep_st(0)
        for st in range(ntok // NT):
            tok0 = st * NT
            r = 1 if tok0 < ctx_tokens else 0
            (hT, k_hT, xts) = nxt if (PIPE_PREP or st == 0) else prep_st(st)
            if PIPE_PREP and st + 1 < ntok // NT:
                nxt = prep_st(st + 1)
            (aT, k_aT) = aTb.next()
            for cb in range(22):
                bg, og = fm_slot()
                bu, ou = fm_slot()
                (sg, k_sg) = sgb.next()

                def mm(e, bk, off, col0, hT=hT):
                    for k in range(8):
                        r_ = e.matmul(banks[bk][:, off:off + NT], lhsT=W1[:, k, col0:col0 + 128], rhs=hT[:, k, :],
                                      start=(k == 0), stop=(k == 7))
                    return r_
                p.op("pe", lambda e, bg=bg, og=og, cb=cb, mm=mm: mm(e, bg, og, cb * 128), [k_hT, k_W1], [BK[bg]])
                p.op("pe", lambda e, bu=bu, ou=ou, cb=cb, mm=mm: mm(e, bu, ou, DFF + cb * 128), [k_hT, k_W1], [BK[bu]])
                p.op("act", lambda e, bg=bg, og=og, sg=sg: e.activation(out=sg, in_=banks[bg][:, og:og + NT],
                                                                        func=AF.Silu), [BK[bg]], [k_sg])
                p.op("dve", lambda e, bu=bu, ou=ou, sg=sg, aT=aT, cb=cb: e.tensor_tensor(
                    out=aT[:, cb, :], in0=sg, in1=banks[bu][:, ou:ou + NT], op=ALU.mult), [k_sg, BK[bu]], [k_aT])
            for j in range(NT // 128):
                t0 = tok0 + j * 128
                pyb = (6, 7)
                for hf in range(2):
                    def mm2(e, hf=hf, aT=aT, j=j, bk=pyb[hf]):
                        for cb in range(22):
                            r_ = e.matmul(banks[bk][:, :], lhsT=aT[:, cb, j * 128:(j + 1) * 128],
                                          rhs=W2[:, cb, hf * 512:(hf + 1) * 512], start=(cb == 0), stop=(cb == 21))
                        return r_
                    p.op("pe", mm2, [k_aT, k_W2], [BK[pyb[hf]]])
                (xt, k_xt) = xts[j]
                residual_out(pyb, xt, k_xt, GG2, k_GG2, r, dst[t0:t0 + 128, :], dkey, (sqb, ssb, xob), "sff")
        p.barrier()

    state = {}

    def phase_D():
        ar.reset()
        state["hi0"] = ar.hi
        KT, k_KT = ar.alloc([2, TOK], BF16, persist=True)
        Vs, k_Vs = ar.alloc([TOK // 128, 2, 132], BF16, persist=True)
        state["KT"] = (KT, k_KT)
        state["Vs"] = (Vs, k_Vs)
        p.op("pool", lambda e: e.memset(Vs, 1.0), [], [k_Vs])
        WQ, k_WQ = ar.alloc([8, 1536], BF16)
        load_weight_bf16(WQ, k_WQ, attn_w_qkv, 8, "wq")
        QKG, k_QKG = ar.alloc([10, 128], F32)
        load(QKG[:, 0:8, :], k_QKG, attn_qk_g[0:1, :].unsqueeze(1).to_broadcast([128, 8, 128]), "d0")
        load(QKG[:, 8:10, :], k_QKG, attn_qk_g[1:2, :].unsqueeze(1).to_broadcast([128, 2, 128]), "d1")
        p.op("dve", lambda e: e.tensor_scalar(out=QKG[:, 0:8, :], in0=QKG[:, 0:8, :], scalar1=128.0 ** -0.5,
                                              scalar2=None, op0=ALU.mult), [k_QKG], [k_QKG])
        NT = 256
        xb = Buf(ar, 2, [D], F32)
        sqb = Buf(ar, 1, [D], BF16)
        ssb = Buf(ar, 2, [4], F32)
        xnb = Buf(ar, 2, [D], F32)
        hTb = Buf(ar, 2, [8, NT], BF16)
        rpb = Buf(ar, 2, [2, 128], F32)
        sq2 = Buf(ar, 2, [10, 128], F32)
        s10 = Buf(ar, 2, [20], F32)
        t1b = Buf(ar, 2, [10, 128], F32)
        t2b = Buf(ar, 2, [10, 128], F32)
        qrb = Buf(ar, 2, [10, 128], BF16)
        qsb = Buf(ar, 2, [8, 128], BF16)
        def prep_st(st):
            tok0 = st * NT
            r = 1 if tok0 < NCTX else 0
            (hT, k_hT) = hTb.next()
            for j in range(NT // 128):
                t0 = tok0 + j * 128
                prep_tile(X1[t0:t0 + 128, :], hT, k_hT, j * 128, gm1, k_gm1, 0, r, (xb, sqb, ssb, xnb))
            return hT, k_hT

        nxt = prep_st(0)
        for st in range(TOK // NT):
            tok0 = st * NT
            is_ctx = tok0 < NCTX
            r = 1 if is_ctx else 0
            (hT, k_hT) = nxt if (PIPE_PREP or st == 0) else prep_st(st)
            if PIPE_PREP and st + 1 < TOK // NT:
                nxt = prep_st(st + 1)
            for j in range(NT // 128):
                t0 = tok0 + j * 128
                tile = t0 // 128
                own = (not is_ctx) and (t0 - NCTX) < OWN
                nh = 10 if own else 2
                h0 = 0 if own else 8
                def mm(e, bk, col0, j=j, hT=hT):
                    for k in range(8):
                        r_ = e.matmul(banks[bk][:, :], lhsT=hT[:, k, j * 128:(j + 1) * 128],
                                      rhs=WQ[:, k, col0:col0 + 512], start=(k == 0), stop=(k == 7))
                    return r_
                p.op("pe", lambda e, mm=mm: mm(e, 4, 1024), [k_hT, k_WQ], [BK[4]])
                if own:
                    p.op("pe", lambda e, mm=mm: mm(e, 2, 0), [k_hT, k_WQ], [BK[2]])
                    p.op("pe", lambda e, mm=mm: mm(e, 3, 512), [k_hT, k_WQ], [BK[3]])
                p.op("act", lambda e, tile=tile: e.activation(
                    out=Vs[:, tile, :, 0:128], in_=pv(4, [4, 128])[:, 2:4, :], func=AF.Copy), [BK[4]], [k_Vs])
                (sq, k_sq) = sq2.next()
                (s1, k_s1) = s10.next()
                (t1, k_t1) = t1b.next()
                (t2, k_t2) = t2b.next()
                (qr, k_qr) = qrb.next()
                srcs = []
                if own:
                    srcs += [(2, 0, 4), (3, 4, 4)]
                srcs += [(4, 8, 2)]
                for (bk, hh, n) in srcs:
                    p.op("act", lambda e, bk=bk, hh=hh, n=n, sq=sq: e.activation(
                        out=sq[:, hh:hh + n, :], in_=pv(bk, [4, 128])[:, 0:n, :], func=AF.Square), [BK[bk]], [k_sq])
                p.op("dve", lambda e, sq=sq, s1=s1, h0=h0, nh=nh: e.tensor_reduce(
                    out=s1[:, h0:h0 + nh], in_=sq[:, h0:h0 + nh, :], axis=AX.X, op=ALU.add), [k_sq], [k_s1])
                rstd_from_ssq(s1[:, h0:h0 + nh], k_s1, s1[:, 10 + h0:10 + h0 + nh], k_s1, 128)
                for (bk, hh, n) in srcs:
                    p.op("dve", lambda e, bk=bk, hh=hh, n=n, t1=t1, s1=s1: e.tensor_tensor(
                        out=t1[:, hh:hh + n, :], in0=pv(bk, [4, 128])[:, 0:n, :],
                        in1=s1[:, 10 + hh:10 + hh + n].unsqueeze(2).to_broadcast([128, n, 128]), op=ALU.mult),
                        [BK[bk], k_s1], [k_t1])
                if is_ctx:
                    p.op("dve", lambda e, t1=t1, qr=qr: e.tensor_tensor(
                        out=qr[:, 8:10, :], in0=t1[:, 8:10, :], in1=QKG[:, 8:10, :], op=ALU.mult),
                        [k_t1, k_QKG], [k_qr])
                else:
                    (rp, k_rp) = rpb.next()
                    lt0 = t0 - NCTX
                    load(rp, k_rp, c_rope[lt0:lt0 + 128], sk(k_rp))
                    sl = slice(h0, h0 + nh)
                    p.op("dve", lambda e, t1=t1, sl=sl: e.tensor_tensor(
                        out=t1[:, sl, :], in0=t1[:, sl, :], in1=QKG[:, sl, :], op=ALU.mult), [k_t1, k_QKG], [k_t1])
                    p.op("dve", lambda e, t1=t1, t2=t2, rp=rp, sl=sl, nh=nh: e.tensor_tensor(
                        out=t2[:, sl, :], in0=t1[:, sl, :],
                        in1=rp[:, 0, :].unsqueeze(1).to_broadcast([128, nh, 128]), op=ALU.mult),
                        [k_t1, k_rp], [k_t2])

                    def v4(a, sl=sl):
                        return a[:, sl, :].rearrange("q h (a b) -> q h a b", a=2)
                    for (ho, hi) in ((0, 32), (32, 0)):
                        p.op("dve", lambda e, t1=t1, sq=sq, rp=rp, ho=ho, hi=hi, nh=nh, v4=v4: e.tensor_tensor(
                            out=v4(sq)[:, :, :, ho:ho + 32], in0=v4(t1)[:, :, :, hi:hi + 32],
                            in1=rp[:, 1, :].rearrange("q (a b) -> q a b", a=2)[:, :, ho:ho + 32]
                            .unsqueeze(1).to_broadcast([128, nh, 2, 32]), op=ALU.mult),
                            [k_t1, k_rp], [k_sq])
                    p.op("dve", lambda e, t2=t2, sq=sq, qr=qr, sl=sl: e.tensor_tensor(
                        out=qr[:, sl, :], in0=t2[:, sl, :], in1=sq[:, sl, :], op=ALU.add), [k_t2, k_sq], [k_qr])
                def trk(e, qr=qr):
                    for g in range(2):
                        r_ = e.transpose(out=pv(5, [8, 128], BF16)[:, g, :], in_=qr[:, 8 + g, :], identity=identb)
                    return r_
                p.op("pe", trk, [k_qr, k_identb], [BK[5]])
                p.op("act", lambda e, t0=t0: e.activation(out=KT[:, :, t0:t0 + 128],
                                                          in_=pv(5, [8, 128], BF16)[:, 0:2, :], func=AF.Copy),
                     [BK[5]], [k_KT])
                if own:
                    (qs, k_qs) = qsb.next()

                    def trq(e, qr=qr):
                        for h in range(8):
                            r_ = e.transpose(out=pv(6, [8, 128], BF16)[:, h, :], in_=qr[:, h, :], identity=identb)
                        return r_
                    p.op("pe", trq, [k_qr, k_identb], [BK[6]])
                    p.op("act", lambda e, qs=qs: e.activation(out=qs, in_=pv(6, [8, 128], BF16), func=AF.Copy),
                         [BK[6]], [k_qs])
                    store(QT_d[(t0 - NCTX) // 128], ["QT_d"], qs, k_qs, sk(k_qs))
        p.barrier()

    def phase_E():
        ar.reset()
        (KT, k_KT) = state["KT"]
        (Vs, k_Vs) = state["Vs"]
        WO, k_WO = ar.alloc([8, D], BF16)
        load_weight_bf16(WO, k_WO, attn_w_out, 8, "wo1")
        qTb = Buf(ar, 2, [8, 128], BF16)
        xb = Buf(ar, 2, [D], F32)
        PTb = Buf(ar, 4, [512], BF16)
        atb = Buf(ar, 2, [8, 128], BF16)
        aTb = Buf(ar, 2, [8, 128], BF16)
        rdb = Buf(ar, 2, [4], F32)
        rdnb = Buf(ar, 2, [512], F32)
        onesb, k_onesb = ar.alloc([128], BF16)
        p.op("pool", lambda e: e.memset(onesb, 1.0), [], [k_onesb])
        sqb = Buf(ar, 1, [D], BF16)
        ssb = Buf(ar, 2, [4], F32)
        xob = Buf(ar, 2, [D], F32)
        NKT = TOK // 128
        NQ = OWN // 128
        units = [(qi, g, kt) for qi in range(NQ) for g in range(2) for kt in range(NKT)]
        qbuf = {}

        def get_q(qi):
            if qi not in qbuf:
                (qT, k_qT) = qTb.next()
                load(qT, k_qT, QT_d[qi], sk(k_qT), ["QT_d"])
                qbuf[qi] = (qT, k_qT)
            return qbuf[qi]

        pts = {}

        def issue_score(u):
            (qi, g, kt) = units[u]
            (qT, k_qT) = get_q(qi)
            sb_ = u % 2
            (PT, k_PT) = PTb.next()
            pts[u] = (PT, k_PT)
            p.op("pe", lambda e, sb_=sb_, g=g, kt=kt, qT=qT: e.matmul(
                banks[sb_][:, :], lhsT=KT[:, g, kt * 128:(kt + 1) * 128],
                rhs=qT[:, 4 * g:4 * g + 4, :].rearrange("q h t -> q (h t)"), start=True, stop=True),
                [k_KT, k_qT], [BK[sb_]])
            p.op("act", lambda e, sb_=sb_, PT=PT: e.activation(out=PT, in_=banks[sb_][:, :], func=AF.Exp),
                 [BK[sb_]], [k_PT])

        for u_ in range(AHEAD):
            issue_score(u_)
        cur = {}
        for u, (qi, g, kt) in enumerate(units):
            if g == 0 and kt == 0:
                (xt, k_xt) = xb.next()
                (at, k_at) = atb.next()
                (aT, k_aT) = aTb.next()
                load(xt, k_xt, X1[NCTX + qi * 128:NCTX + (qi + 1) * 128, :], sk(k_xt), ["X1"])
                cur = dict(xt=xt, k_xt=k_xt, at=at, k_at=k_at, aT=aT, k_aT=k_aT)
            if kt == 0:
                (rd, k_rd) = rdb.next()
                cur["rd"], cur["k_rd"] = rd, k_rd
            pob = (3 + 2 * g, 4 + 2 * g)
            if AHEAD == 0:
                issue_score(u)
            (PT, k_PT) = pts.pop(u)

            def pvm(e, PT=PT, kt=kt, g=g, pob=pob):
                e.matmul(banks[pob[0]][:, :], lhsT=Vs[:, kt, g, 0:128], rhs=PT,
                         start=(kt == 0), stop=(kt == NKT - 1))
                return e.matmul(banks[pob[1]][:, :], lhsT=onesb, rhs=PT,
                                start=(kt == 0), stop=(kt == NKT - 1))
            if AHEAD > 0 and u + AHEAD < len(units):
                issue_score(u + AHEAD)
            p.op("pe", pvm, [k_PT, k_Vs, k_onesb], [BK[pob[0]], BK[pob[1]]])
            if kt == NKT - 1:
                aT, k_aT = cur["aT"], cur["k_aT"]
                (rdn, k_rdn) = rdnb.next()
                p.op("dve", lambda e, rdn=rdn, pob=pob: e.reciprocal(out=rdn, in_=banks[pob[1]][:, :]),
                     [BK[pob[1]]], [k_rdn])
                p.op("dve", lambda e, rdn=rdn, pob=pob, aT=aT, g=g: e.tensor_tensor(
                    out=aT[:, 4 * g:4 * g + 4, :].rearrange("q h t -> q (h t)"), in0=banks[pob[0]][:, :],
                    in1=rdn, op=ALU.mult), [BK[pob[0]], k_rdn], [k_aT])
                if g == 1:
                    xt, k_xt = cur["xt"], cur["k_xt"]
                    pyb = (2, 7)
                    for hf in range(2):
                        def mm(e, hf=hf, aT=aT, bk=pyb[hf]):
                            for k in range(8):
                                r_ = e.matmul(banks[bk][:, :], lhsT=aT[:, k, :], rhs=WO[:, k, hf * 512:(hf + 1) * 512],
                                              start=(k == 0), stop=(k == 7))
                            return r_
                        p.op("pe", mm, [k_aT, k_WO], [BK[pyb[hf]]])
                    residual_out(pyb, xt, k_xt, GG1, k_GG1, 0, X2[qi * 128:(qi + 1) * 128, :], "X2",
                                 (sqb, ssb, xob), "sx2")
        p.barrier()
        ar.hi = state["hi0"]

    phases = [
        ("ada0", lambda: ada_layer(0)),
        ("A", phase_A),
        ("A2", phase_A2),
        ("B", phase_B),
        ("C1", phase_C1),
        ("C2", lambda: phase_FFN(0, XM, "XM", X1, "X1", TOK, NCTX)),
        ("ada1", lambda: ada_layer(1)),
        ("D", phase_D),
        ("E", phase_E),
        ("F", lambda: phase_FFN(1, X2, "X2", y_out, "y", OWN, 0)),
    ]
    finals = []
    for name, fn in phases:
        fn()
        if stop_after == name:
            break
    else:
        finals = ["y"]
    if debug:
        finals = finals + [k for k in ("ADA_d", "QA_c", "KA_c", "QK_c", "KA_tm", "LF_tm", "VA_tm", "BQK_d", "VB_tm", "GT_d",
                                       "G_tm", "O0", "O1", "XM", "X1", "X2", "QT_d") if k in p.last_writer]
    stats = p.emit(final_wait_keys=finals)
    es.close()
    return nc, stats


def _consts():
    s = np.arange(64)[:, None]
    t = np.arange(64)[None, :]
    cm = np.zeros((64, 2, 4, 64), np.float32)
    tri_f = (s <= t).astype(np.float32)
    cm[:, 0, 0] = tri_f - (s <= 31).astype(np.float32)
    cm[:, 0, 1] = tri_f
    cm[:, 0, 2] = (s > t).astype(np.float32)
    cm[:, 0, 3] = tri_f
    tri_b = (s >= t).astype(np.float32)
    cm[:, 1, 0] = tri_b - (s >= 32).astype(np.float32)
    cm[:, 1, 1] = tri_b
    cm[:, 1, 2] = (s < t).astype(np.float32)
    cm[:, 1, 3] = tri_b
    sel = np.zeros((2, 2, 128), np.float32)
    sel[0, 0, :] = 1.0
    sel[1, 1, :] = 1.0
    pos = np.arange(NLAT)
    row = (pos // 64).astype(np.float32)
    col = (pos % 64).astype(np.float32)
    inv = np.power(np.float32(10000.0), -np.arange(0, 64, 2, dtype=np.float32) / np.float32(64)).astype(np.float32)
    ar_ = (row[:, None] * inv[None, :]).astype(np.float32)
    ac_ = (col[:, None] * inv[None, :]).astype(np.float32)
    rope = np.zeros((NLAT, 2, 128), np.float32)
    rope[:, 0, 0:32] = np.cos(ar_)
    rope[:, 0, 32:64] = np.cos(ar_)
    rope[:, 0, 64:96] = np.cos(ac_)
    rope[:, 0, 96:128] = np.cos(ac_)
    rope[:, 1, 0:32] = -np.sin(ar_)
    rope[:, 1, 32:64] = np.sin(ar_)
    rope[:, 1, 64:96] = -np.sin(ac_)
    rope[:, 1, 96:128] = np.sin(ac_)
    return {"c_ident": np.eye(128, dtype=np.float32), "c_cm": cm, "c_sel": sel}, rope


def make_in_maps(inputs, cores=range(8)):
    f = lambda a: np.ascontiguousarray(np.asarray(a, dtype=np.float32))
    x, c, ctx, c_ctx = f(inputs["x"]), f(inputs["c"]), f(inputs["ctx"]), f(inputs["c_ctx"])
    consts, rope = _consts()
    w_in = f(inputs["ab_w_in"])[0]
    conv = f(inputs["ab_conv"])[0]
    gb = f(inputs["ab_gate_b"])[0].reshape(16)
    lb = f(inputs["hgrn_lb"])
    w_in_r = w_in.copy()
    w_in_r[:, 1536:2048] = w_in[:, 2048:2560]
    w_in_r[:, 2048:2560] = w_in[:, 1536:2048]
    w_in_r[:, 4096:4104] = w_in[:, 4104:4112]
    w_in_r[:, 4104:4112] = w_in[:, 4096:4104]
    conv_r = np.ascontiguousarray(conv[::-1])
    gb_r = np.concatenate([gb[8:16], gb[0:8]])
    lb_r = np.ascontiguousarray(lb[::-1])
    rope_r = np.ascontiguousarray(rope[::-1])
    shared = {
        "ada_w": f(inputs["ada_w"]), "ada_b": f(inputs["ada_b"]), "norm_g": f(inputs["norm_g"]),
        "ffn_w_in": f(inputs["ffn_w_in"]), "ffn_w_out": f(inputs["ffn_w_out"]),
        "ab_out_g": f(inputs["ab_out_g"])[0], "ab_w_out": f(inputs["ab_w_out"])[0],
        "attn_w_qkv": f(inputs["attn_w_qkv"])[0], "attn_qk_g": f(inputs["attn_qk_g"])[0],
        "attn_w_out": f(inputs["attn_w_out"])[0],
    }
    shared.update(consts)
    maps = []
    for core in cores:
        b, half = core // 2, core % 2
        m = dict(shared)
        if half == 0:
            m["xs"] = np.ascontiguousarray(np.concatenate([ctx[b], x[b]], axis=0))
            m.update({"ab_w_in": w_in, "ab_conv": conv, "ab_gate_b": gb, "hgrn_lb": lb, "c_rope": rope})
        else:
            m["xs"] = np.ascontiguousarray(np.concatenate([ctx[b][::-1], x[b][::-1]], axis=0))
            m.update({"ab_w_in": w_in_r, "ab_conv": conv_r, "ab_gate_b": gb_r, "hgrn_lb": lb_r, "c_rope": rope_r})
        m["cvec"] = np.ascontiguousarray(np.stack([c[b], c_ctx], axis=0))
        maps.append(m)
    return maps


_NC_CACHE = {}


def kernel(**inputs):
    if "nc" not in _NC_CACHE:
        _NC_CACHE["nc"] = build_nc()[0]
    nc = _NC_CACHE["nc"]
    maps = make_in_maps(inputs)
    res = run_bass_kernel_spmd(nc, maps, core_ids=list(range(8)))
    out = np.zeros((4, NLAT, D), np.float32)
    for core in range(8):
        b, half = core // 2, core % 2
        y = np.asarray(res.results[core]["y"])
        if half == 0:
            out[b, 0:OWN] = y
        else:
            out[b, OWN:NLAT] = y[::-1]
    return out
```

```python
import contextlib
import math
import numpy as np
import concourse.bass as bass
import concourse.mybir as mybir
from concourse.bass_utils import run_bass_kernel_spmd

F32 = mybir.dt.float32
BF16 = mybir.dt.bfloat16
AF = mybir.ActivationFunctionType
ALU = mybir.AluOpType
AX = mybir.AxisListType

D = 1024
NCTX = 256
NLAT = 4096
TOK = NCTX + NLAT
OWN = 2048
CH = 64
NCH = TOK // CH
DFF = 2816
TOKP = TOK + 4
EPS = 1e-6
LN8 = math.log(8.0)
import os
AHEAD = int(os.environ.get('K_AHEAD', '2'))
PIPE_PREP = int(os.environ.get('K_PIPE', '0'))
ENGS = ("pe", "act", "dve", "pool", "sp")


def bpos(tok):
    return tok + 1 if tok < NCTX else tok + 3


class Op:
    __slots__ = ("eng", "fn", "deps", "is_dma", "semkey", "ndma", "tick", "signal", "idx")


class Prog:
    def __init__(self, nc):
        self.nc = nc
        self.ops = []
        self.last_writer = {}
        self.readers = {}
        self.last_dma = {}
        self.last_eng = {}
        self.barrier_deps = {}
        self.qsem = {}
        self.phase_id = 0

    def _add(self, eng, fn, reads, writes, is_dma=False, semkey=None, ndma=1):
        op = Op()
        op.eng, op.fn, op.is_dma, op.semkey, op.ndma = eng, fn, is_dma, semkey, ndma
        op.idx = len(self.ops)
        op.signal = False
        op.tick = None
        deps = set()
        for r in reads:
            w = self.last_writer.get(r)
            if w is not None:
                deps.add(w)
        for w_ in writes:
            w = self.last_writer.get(w_)
            if w is not None:
                deps.add(w)
            for rd in self.readers.get(w_, ()):
                deps.add(rd)
        if is_dma:
            prev = self.last_dma.get(semkey)
            if prev is not None:
                deps.add(prev)
            self.last_dma[semkey] = op.idx
        bd = self.barrier_deps.pop(eng, None)
        if bd:
            deps.update(bd)
        deps.discard(op.idx)
        if eng == "pe":
            deps = {d_ for d_ in deps if self.ops[d_].eng != "pe" or self.ops[d_].is_dma}
        op.deps = deps
        for r in reads:
            self.readers.setdefault(r, []).append(op.idx)
        for w_ in writes:
            self.last_writer[w_] = op.idx
            self.readers[w_] = []
        self.last_eng[eng] = op.idx
        self.ops.append(op)
        return op

    def op(self, eng, fn, reads=(), writes=()):
        return self._add(eng, fn, tuple(reads), tuple(writes))

    def dma(self, queue, fn, reads, writes, semkey, ndma=1):
        if semkey.startswith("m") and semkey[1:].isdigit():
            cnt = self.qsem.setdefault(queue, {})
            ph = self.phase_id
            k = (ph, semkey)
            if k not in cnt:
                cnt[k] = len([1 for kk in cnt if kk[0] == ph])
            semkey = "%s%d" % (queue[0], cnt[k])
        else:
            semkey = queue[0] + "_" + semkey
        return self._add(queue, fn, tuple(reads), tuple(writes), True, semkey, ndma)

    def barrier(self):
        self.phase_id += 1
        allprev = set(self.last_eng.values()) | set(self.last_dma.values())
        for e in ENGS:
            s = self.barrier_deps.setdefault(e, set())
            s.update(allprev)

    def emit(self, final_wait_keys=()):
        nc = self.nc
        ops = self.ops
        for o in ops:
            for d in o.deps:
                ops[d].signal = True
        finals = [self.last_writer[k] for k in final_wait_keys]
        for f in finals:
            ops[f].signal = True
        eng_count = {e: 0 for e in ENGS}
        dma_count = {}
        for o in ops:
            if o.is_dma:
                c = dma_count.get(o.semkey, 0) + 16 * o.ndma
                dma_count[o.semkey] = c
                o.tick = c
            elif o.signal:
                eng_count[o.eng] += 1
                o.tick = eng_count[o.eng]
        semkeys = sorted(dma_count.keys())
        with contextlib.ExitStack() as es:
            esem = {e: es.enter_context(nc.semaphore("s_" + e)) for e in ENGS}
            dsem = {k: es.enter_context(nc.semaphore("d_" + str(k))) for k in semkeys}
            block = es.enter_context(nc.Block())

            def sem_of(o):
                return dsem[o.semkey] if o.is_dma else esem[o.eng]

            def stream(engname):
                def body(e):
                    waited = {}
                    for o in ops:
                        if o.eng != engname:
                            continue
                        need = {}
                        for d in o.deps:
                            do = ops[d]
                            key = ("d", do.semkey) if do.is_dma else ("e", do.eng)
                            if need.get(key, (0, None))[0] < do.tick:
                                need[key] = (do.tick, do)
                        for key, (tick, do) in need.items():
                            if waited.get(key, 0) >= tick:
                                continue
                            e.wait_ge(sem_of(do), tick)
                            waited[key] = tick
                        res = o.fn(e)
                        if o.is_dma:
                            assert len(res) == o.ndma, (len(res), o.ndma)
                            for ins in res:
                                ins.then_inc(dsem[o.semkey], 16)
                        elif o.signal:
                            res.then_inc(esem[o.eng], 1)
                    if engname == "sp":
                        for f in finals:
                            fo = ops[f]
                            e.wait_ge(sem_of(fo), fo.tick)
                return body

            block.tensor(stream("pe"))
            block.scalar(stream("act"))
            block.vector(stream("dve"))
            block.gpsimd(stream("pool"))
            block.sync(stream("sp"))
        return eng_count, dma_count, len(ops)


class Arena:
    def __init__(self, t, nbytes):
        self.t = t
        self.n = nbytes
        self.lo = 0
        self.hi = nbytes
        self.cnt = 0
        self.semmap = {}
        self.pidx = 0

    def _view(self, off, shape, dt, parts):
        nel = int(np.prod(shape))
        if dt == F32:
            v = self.t[0:parts, off // 2: off // 2 + nel * 2].bitcast(F32)
        else:
            v = self.t[0:parts, off // 2: off // 2 + nel]
        if len(shape) == 2:
            v = v.rearrange("p (a b) -> p a b", a=shape[0])
        elif len(shape) == 3:
            v = v.rearrange("p (a b c) -> p a b c", a=shape[0], b=shape[1])
        elif len(shape) == 4:
            v = v.rearrange("p (a b c d) -> p a b c d", a=shape[0], b=shape[1], c=shape[2])
        return v

    def alloc(self, shape, dt, parts=128, persist=False):
        nb = int(np.prod(shape)) * (4 if dt == F32 else 2)
        nb = (nb + 63) // 64 * 64
        if persist:
            self.hi -= nb
            off = self.hi
        else:
            off = self.lo
            self.lo += nb
        assert self.lo <= self.hi, ("SBUF arena overflow", self.lo, self.hi)
        self.cnt += 1
        key = "sb%d" % self.cnt
        self.semmap[key] = "m%d" % self.pidx
        self.pidx += 1
        return self._view(off, shape, dt, parts), key

    def reset(self):
        self.lo = 0
        self.pidx = 0


class Buf:
    def __init__(self, ar, n, shape, dt, parts=128):
        self.slots = [ar.alloc(shape, dt, parts) for _ in range(n)]
        self.i = -1

    def next(self):
        self.i = (self.i + 1) % len(self.slots)
        return self.slots[self.i]

    def cur(self):
        return self.slots[self.i]


def build_nc(debug=False, stop_after=None):
    nc = bass.Bass("TRN2", target_bir_lowering=False)
    es = contextlib.ExitStack()

    def din(name, shape, dt=F32):
        return nc.dram_tensor(name, list(shape), dt, kind="ExternalInput").ap()

    dbg_names = []

    def dscr(name, shape, dt=F32):
        if debug:
            dbg_names.append(name)
            return nc.dram_tensor(name, list(shape), dt, kind="ExternalOutput").ap()
        return nc.dram_tensor(name, list(shape), dt, kind="Internal").ap()

    xs = din("xs", [TOK, D])
    cvec = din("cvec", [2, D])
    ada_w = din("ada_w", [2, D, 6 * D])
    ada_b = din("ada_b", [2, 6 * D])
    norm_g = din("norm_g", [2, 4, D])
    ffn_w_in = din("ffn_w_in", [2, D, 2 * DFF])
    ffn_w_out = din("ffn_w_out", [2, DFF, D])
    ab_w_in = din("ab_w_in", [D, 4112])
    ab_conv = din("ab_conv", [3, 512])
    ab_gate_b = din("ab_gate_b", [16])
    hgrn_lb = din("hgrn_lb", [2, 3, 512])
    ab_out_g = din("ab_out_g", [D])
    ab_w_out = din("ab_w_out", [D, D])
    attn_w_qkv = din("attn_w_qkv", [D, 1536])
    attn_qk_g = din("attn_qk_g", [2, 128])
    attn_w_out = din("attn_w_out", [D, D])
    c_ident = din("c_ident", [128, 128])
    c_cm = din("c_cm", [64, 2, 4, 64])
    c_sel = din("c_sel", [2, 2, 128])
    c_rope = din("c_rope", [NLAT, 2, 128])
    y_out = nc.dram_tensor("y", [OWN, D], F32, kind="ExternalOutput").ap()

    QA_c = dscr("QA_c", [NCH, 128, 4, CH], BF16)
    KA_c = dscr("KA_c", [2, NCH, 128, 4, CH], BF16)
    KA_tm = dscr("KA_tm", [2, TOK, 512], BF16)
    LF_tm = dscr("LF_tm", [2, TOK, 512], F32)
    VA_tm = dscr("VA_tm", [TOK, 512], BF16)
    BQK_d = dscr("BQK_d", [64, 8, TOKP], F32)
    QK_c = dscr("QK_c", [NCH, 2, 64, 4, CH], BF16)
    VB_tm = dscr("VB_tm", [TOK, 512], BF16)
    GT_d = dscr("GT_d", [TOK, 16], F32)
    G_tm = dscr("G_tm", [TOK, D], BF16)
    O_d = [dscr("O_f", [TOK, D], BF16), dscr("O_b", [TOK, D], BF16)]
    XM = dscr("XM", [TOK, D], F32)
    X1 = dscr("X1", [TOK, D], F32)
    X2 = dscr("X2", [OWN, D], F32)
    QT_d = dscr("QT_d", [OWN // 128, 128, 8, 128], BF16)
    ADA_d = dscr("ADA_d", [2, 2, 6 * D], F32)

    ARB = 206 * 1024
    arena_t = es.enter_context(nc.sbuf_tensor("arena", [128, ARB // 2], BF16))
    ar = Arena(arena_t, ARB)
    banks = [es.enter_context(nc.psum_tensor("psb%d" % i, [128, 512], F32)) for i in range(8)]
    BK = ["bank%d" % i for i in range(8)]

    p = Prog(nc)

    def pv(i, shape, dt=F32, parts=128, off=0):
        nel = int(np.prod(shape))
        if dt == F32:
            v = banks[i][0:parts, off:off + nel]
        else:
            v = banks[i][0:parts, off:off + (nel + 1) // 2].bitcast(BF16)
        if len(shape) == 2:
            v = v.rearrange("p (a b) -> p a b", a=shape[0])
        elif len(shape) == 3:
            v = v.rearrange("p (a b c) -> p a b c", a=shape[0], b=shape[1])
        return v

    def sk(key):
        return ar.semmap[key]

    def load(dst, dkey, src, semkey, rkeys=(), q="sp"):
        p.dma(q, lambda e: [e.dma_start(out=dst, in_=src, allow_slow_non_contiguous=True)], rkeys, [dkey], semkey)

    def store(dst, dkeys, src, skey, semkey, q="pool"):
        p.dma(q, lambda e: [e.dma_start(out=dst, in_=src, allow_slow_non_contiguous=True)], [skey], dkeys, semkey)

    identf, k_identf = ar.alloc([128], F32, persist=True)
    identb, k_identb = ar.alloc([128], BF16, persist=True)
    cm, k_cm = ar.alloc([2, 4, 64], F32, parts=64, persist=True)
    epsc, k_eps = ar.alloc([1], F32, persist=True)
    onec, k_one = ar.alloc([1], F32, persist=True)
    ones64, k_ones64 = ar.alloc([64], F32, parts=64, persist=True)
    sel, k_sel = ar.alloc([2, 128], F32, parts=2, persist=True)
    modc, k_modc = ar.alloc([6, 8, 2], F32, persist=True)
    gm1, k_gm1 = ar.alloc([8, 2], F32, persist=True)
    gm2, k_gm2 = ar.alloc([8, 2], F32, persist=True)
    GG1, k_GG1 = ar.alloc([2, D], F32, persist=True)
    GG2, k_GG2 = ar.alloc([2, D], F32, persist=True)

    load(identf, k_identf, c_ident, "c0")
    p.op("dve", lambda e: e.tensor_copy(out=identb, in_=identf), [k_identf], [k_identb])
    load(cm, k_cm, c_cm, "c1")
    load(sel, k_sel, c_sel.rearrange("r k m -> k r m"), "c2")
    p.op("pool", lambda e: e.memset(epsc, EPS), [], [k_eps])
    p.op("pool", lambda e: e.memset(onec, 1.0), [], [k_one])
    p.op("pool", lambda e: e.memset(ones64, 1.0), [], [k_ones64])

    def rstd_from_ssq(ssq, k_ssq, out, k_out, n, parts=128):
        p.op("act", lambda e: e.activation(out=out, in_=ssq, func=AF.Sqrt, bias=epsc[0:parts, :],
                                           scale=1.0 / n), [k_ssq, k_eps], [k_out])
        p.op("dve", lambda e: e.reciprocal(out=out, in_=out), [k_out], [k_out])

    def ada_layer(l):
        ar.reset()
        scT, k_scT = ar.alloc([8, 2], F32)
        adas, k_adas = ar.alloc([6 * D], F32, parts=2)
        adab, k_adab = ar.alloc([6 * D], F32, parts=2)
        ngc, k_ngc = ar.alloc([4, 8], F32)
        ngb, k_ngb = ar.alloc([2, D], F32)
        wbuf = Buf(ar, 2, [8, 512], F32)
        p.dma("sp", lambda e: [e.dma_start(out=scT[:, :, r_], in_=cvec[r_].rearrange("(k q) -> q k", q=128),
                                           allow_slow_non_contiguous=True) for r_ in range(2)],
              [], [k_scT], "a0", ndma=2)
        p.op("act", lambda e: e.activation(out=scT, in_=scT, func=AF.Silu), [k_scT], [k_scT])
        load(adab, k_adab, ada_b[l:l + 1, :].to_broadcast([2, 6 * D]), "a1")
        p.dma("sp", lambda e: [e.dma_start(out=ngc, in_=norm_g[l].rearrange("v (k q) -> q v k", q=128),
                                           allow_slow_non_contiguous=True)], [], [k_ngc], "a2")
        for n in range(12):
            (wt, k_wt) = wbuf.next()
            load(wt, k_wt, ada_w[l, :, n * 512:(n + 1) * 512].rearrange("(k q) n -> q k n", q=128),
                 "aw%d" % (n % 2))
            bk = n % 2

            def mm(e, wt=wt, bk=bk):
                for k in range(8):
                    r = e.matmul(banks[bk][0:2, :], lhsT=scT[:, k, :], rhs=wt[:, k, :],
                                 start=(k == 0), stop=(k == 7))
                return r
            p.op("pe", mm, [k_scT, k_wt], [BK[bk]])
            p.op("dve", lambda e, bk=bk, n=n: e.tensor_tensor(
                out=adas[:, n * 512:(n + 1) * 512], in0=banks[bk][0:2, :],
                in1=adab[:, n * 512:(n + 1) * 512], op=ALU.add), [BK[bk], k_adab], [k_adas])
        if debug:
            store(ADA_d[l], ["ADA_d"], adas, k_adas, "dbg")
        def colmm(e):
            for v in range(6):
                for k in range(8):
                    c0 = v * D + k * 128
                    r = e.matmul(pv(2, [6, 8, 2])[:, v, k, :], lhsT=adas[:, c0:c0 + 128],
                                 rhs=identf[0:2, 0:2], start=True, stop=True)
            return r
        p.op("pe", colmm, [k_adas, k_identf], [BK[2]])
        p.op("dve", lambda e: e.tensor_copy(out=modc, in_=pv(2, [6, 8, 2])), [BK[2]], [k_modc])
        for (gm, k_gm, vsc, vng) in ((gm1, k_gm1, 1, 0), (gm2, k_gm2, 4, 2)):
            p.op("dve", lambda e, gm=gm, vsc=vsc: e.tensor_scalar(
                out=gm, in0=modc[:, vsc, :, :], scalar1=1.0, scalar2=None, op0=ALU.add),
                [k_modc], [k_gm])
            p.op("dve", lambda e, gm=gm, vng=vng: e.tensor_tensor(
                out=gm, in0=gm, in1=ngc[:, vng, :].unsqueeze(2).to_broadcast([128, 8, 2]), op=ALU.mult),
                [k_gm, k_ngc], [k_gm])
        for (GG, k_GG, vg, vng) in ((GG1, k_GG1, 2, 1), (GG2, k_GG2, 5, 3)):
            load(ngb[:, 0, :], k_ngb, norm_g[l, vng:vng + 1, :].to_broadcast([128, D]), "a3")
            load(ngb[:, 1, :], k_ngb, norm_g[l, vng:vng + 1, :].to_broadcast([128, D]), "a3")
            for r in range(2):
                for hf in range(2):
                    bk = 3 + hf
                    c0 = vg * D + hf * 512
                    p.op("pe", lambda e, bk=bk, c0=c0, r=r: e.matmul(
                        banks[bk][:, :], lhsT=sel[:, r, :], rhs=adas[:, c0:c0 + 512],
                        start=True, stop=True), [k_adas, k_sel], [BK[bk]])
                    p.op("dve", lambda e, bk=bk, GG=GG, r=r, hf=hf: e.tensor_tensor(
                        out=GG[:, r, hf * 512:(hf + 1) * 512], in0=banks[bk][:, :],
                        in1=ngb[:, r, hf * 512:(hf + 1) * 512], op=ALU.mult),
                        [BK[bk], k_ngb], [k_GG])
        p.barrier()

    def load_weight_bf16(dst, dkey, src_rows, kchunks, semkey):
        for k in range(kchunks):
            p.dma("pool", lambda e, k=k: [e.dma_start(out=dst[:, k, :], in_=src_rows[k * 128:(k + 1) * 128, :])],
                  [], [dkey], "W" + str(k % 4))

    def prep_tile(src_ap, hT, k_hT, col0, gm, k_gm, shv, r, bufs, xkeep=None):
        xb, sqb, ssb, xnb = bufs
        if xkeep is None:
            (xt, k_xt) = xb.next()
        else:
            (xt, k_xt) = xkeep
        (sq, k_sq) = sqb.next()
        (ss, k_ss) = ssb.next()
        (xn, k_xn) = xnb.next()
        load(xt, k_xt, src_ap, sk(k_xt))
        p.op("act", lambda e: e.activation(out=sq, in_=xt, func=AF.Square, accum_out=ss[:, 0:1]),
             [k_xt], [k_sq, k_ss])
        rstd_from_ssq(ss[:, 0:1], k_ss, ss[:, 1:2], k_ss, D)
        p.op("act", lambda e: e.activation(out=xn, in_=xt, func=AF.Copy, scale=ss[:, 1:2]),
             [k_xt, k_ss], [k_xn])

        def tr(e):
            for k in range(8):
                r_ = e.transpose(out=pv(k // 4, [4, 128])[:, k % 4, :], in_=xn[:, k * 128:(k + 1) * 128],
                                 identity=identf)
            return r_
        p.op("pe", tr, [k_xn, k_identf], [BK[0], BK[1]])
        for k in range(8):
            p.op("dve", lambda e, k=k: e.tensor_scalar(
                out=hT[:, k, col0:col0 + 128], in0=pv(k // 4, [4, 128])[:, k % 4, :],
                scalar1=gm[:, k, r:r + 1], scalar2=modc[:, shv, k, r:r + 1], op0=ALU.mult, op1=ALU.add),
                [BK[k // 4], k_gm, k_modc], [k_hT])
        return xt, k_xt

    def residual_out(py_banks, xt, k_xt, GG, k_GG, r, dst_ap, dkey, bufs, semkey):
        sqb, ssb, outb = bufs
        (sq, k_sq) = sqb.next()
        (ss, k_ss) = ssb.next()
        (xo, k_xo) = outb.next()
        for hf in range(2):
            bk = py_banks[hf]
            p.op("act", lambda e, bk=bk, hf=hf: e.activation(
                out=sq[:, 0:512], in_=banks[bk][:, :], func=AF.Square, accum_out=ss[:, 2 + hf:3 + hf]),
                [BK[bk]], [k_sq, k_ss])
        p.op("dve", lambda e: e.tensor_tensor(out=ss[:, 0:1], in0=ss[:, 2:3], in1=ss[:, 3:4], op=ALU.add),
             [k_ss], [k_ss])
        rstd_from_ssq(ss[:, 0:1], k_ss, ss[:, 1:2], k_ss, D)
        for hf in range(2):
            bk = py_banks[hf]
            p.op("dve", lambda e, bk=bk, hf=hf: e.scalar_tensor_tensor(
                out=xo[:, hf * 512:(hf + 1) * 512], in0=banks[bk][:, :], scalar=ss[:, 1:2],
                in1=GG[:, r, hf * 512:(hf + 1) * 512], op0=ALU.mult, op1=ALU.mult),
                [BK[bk], k_ss, k_GG], [k_xo])
        p.op("dve", lambda e: e.tensor_tensor(out=xo, in0=xo, in1=xt, op=ALU.add), [k_xo, k_xt], [k_xo])
        store(dst_ap, [dkey], xo, k_xo, sk(k_xo))

    def phase_A():
        ar.reset()
        WA, k_WA = ar.alloc([8, 4112], BF16)
        load_weight_bf16(WA, k_WA, ab_w_in, 8, "wA")
        lbf, k_lbf = ar.alloc([2, 3, 4], F32)
        lbb, k_lbb = ar.alloc([2, 3, 512], F32)
        omlb_c, k_omlbc = ar.alloc([2, 4], F32)
        lb_b, k_lb_b = ar.alloc([2, 512], F32)
        omlb_b, k_omlb_b = ar.alloc([2, 512], F32)
        gbb, k_gbb = ar.alloc([16], F32)
        zt, k_zt = ar.alloc([8, 4], F32, parts=64)
        p.dma("sp", lambda e: [e.dma_start(out=lbf, in_=hgrn_lb.rearrange("r l (h q) -> q r l h", q=128),
                                           allow_slow_non_contiguous=True)], [], [k_lbf], "l0")
        load(lbb, k_lbb, hgrn_lb.rearrange("r l c -> (r l c)").unsqueeze(0).to_broadcast([128, 3072])
             .rearrange("p (r l c) -> p r l c", r=2, l=3), "l1")
        load(gbb, k_gbb, ab_gate_b.unsqueeze(0).to_broadcast([128, 16]), "l2")
        for (t, kt_, o1, ko1, o2, ko2) in ((lbf, k_lbf, omlb_c, k_omlbc, None, None),
                                           (lbb, k_lbb, omlb_b, k_omlb_b, lb_b, k_lb_b)):
            p.op("act", lambda e, t=t: e.activation(out=t, in_=t, func=AF.Exp), [kt_], [kt_])
            p.op("dve", lambda e, t=t, o1=o1: e.tensor_tensor(out=o1, in0=t[:, :, 0, :], in1=t[:, :, 1, :], op=ALU.add),
                 [kt_], [ko1])
            p.op("dve", lambda e, t=t, o1=o1: e.tensor_tensor(out=o1, in0=o1, in1=t[:, :, 2, :], op=ALU.add),
                 [kt_, ko1], [ko1])
            p.op("dve", lambda e, o1=o1: e.reciprocal(out=o1, in_=o1), [ko1], [ko1])
            p.op("dve", lambda e, t=t, o1=o1: e.tensor_tensor(out=o1, in0=o1, in1=t[:, :, 0, :], op=ALU.mult),
                 [kt_, ko1], [ko1])
            if o2 is not None:
                p.op("dve", lambda e, o1=o1, o2=o2: e.tensor_copy(out=o2, in_=o1), [ko1], [ko2])
            p.op("dve", lambda e, o1=o1: e.tensor_scalar(out=o1, in0=o1, scalar1=-1.0, scalar2=1.0,
                                                         op0=ALU.mult, op1=ALU.add), [ko1], [ko1])
        p.op("pool", lambda e: e.memset(zt, 0.0), [], [k_zt])
        for i, pos in enumerate((0, NCTX + 1, NCTX + 2, TOKP - 1)):
            store(BQK_d[:, :, pos:pos + 1], ["BQK_d"], zt[:, :, 0:1], k_zt, "z%d" % i, q="sp")

        NT = 256
        xb = Buf(ar, 2, [D], F32)
        sqb = Buf(ar, 1, [D], BF16)
        ssb = Buf(ar, 2, [4], F32)
        xnb = Buf(ar, 2, [D], F32)
        hTb = Buf(ar, 2, [8, NT], BF16)
        qst = Buf(ar, 2, [NT // CH, 4, CH], BF16)
        kst = Buf(ar, 2, [NT // CH, 4, CH], BF16)
        sgt = Buf(ar, 2, [NT], F32)
        bst = Buf(ar, 2, [8, NT], F32, parts=64)
        vst = Buf(ar, 2, [512], BF16)
        gst = Buf(ar, 2, [D], BF16)
        vbst = Buf(ar, 2, [512], BF16)
        sst = Buf(ar, 2, [512], F32)
        ust = Buf(ar, 2, [512], F32)
        lfst = Buf(ar, 2, [512], F32)
        ktst = Buf(ar, 2, [512], BF16)
        gtst = Buf(ar, 2, [16], F32)
        gt2 = Buf(ar, 2, [16], F32)
        fmb = [2, 3]
        tmb = [4, 5, 6]
        fm_i = [0]
        tm_i = [0]

        def fm_slot():
            i = fm_i[0]
            fm_i[0] = (i + 1) % 4
            return fmb[i // 2], (i % 2) * 256

        def tm_bank():
            i = tm_i[0]
            tm_i[0] = (i + 1) % 3
            return tmb[i]

        def prep_st(st):
            tok0 = st * NT
            r = 1 if tok0 < NCTX else 0
            (hT, k_hT) = hTb.next()
            for j in range(NT // 128):
                t0 = tok0 + j * 128
                prep_tile(xs[t0:t0 + 128, :], hT, k_hT, j * 128, gm1, k_gm1, 0, r, (xb, sqb, ssb, xnb))
            return hT, k_hT

        nxt = prep_st(0)
        for st in range(TOK // NT):
            tok0 = st * NT
            (hT, k_hT) = nxt if (PIPE_PREP or st == 0) else prep_st(st)
            if PIPE_PREP and st + 1 < TOK // NT:
                nxt = prep_st(st + 1)
            c0 = tok0 // CH
            nchk = NT // CH

            def fm_mm(bk, off, col0, hT=hT):
                def f(e):
                    for k in range(8):
                        r_ = e.matmul(banks[bk][:, off:off + NT], lhsT=WA[:, k, col0:col0 + 128], rhs=hT[:, k, :],
                                      start=(k == 0), stop=(k == 7))
                    return r_
                return f
            (qs, k_qs) = qst.next()
            for h in range(4):
                bk, off = fm_slot()
                p.op("pe", fm_mm(bk, off, h * 128), [k_hT, k_WA], [BK[bk]])
                p.op("act", lambda e, bk=bk, off=off, h=h, qs=qs: e.activation(
                    out=qs[:, :, h, :], in_=banks[bk][:, off:off + NT].rearrange("q (c t) -> q c t", t=CH),
                    func=AF.Copy), [BK[bk]], [k_qs])
            store(QA_c[c0:c0 + nchk].rearrange("c q h t -> q c (h t)"), ["QA_c"],
                  qs.rearrange("q c h t -> q c (h t)"), k_qs, sk(k_qs))
            for d in range(2):
                (ks, k_ks) = kst.next()
                for h in range(4):
                    bk, off = fm_slot()
                    (sg, k_sg) = sgt.next()
                    p.op("pe", fm_mm(bk, off, 1536 + d * 512 + h * 128), [k_hT, k_WA], [BK[bk]])
                    p.op("act", lambda e, bk=bk, off=off, sg=sg: e.activation(
                        out=sg, in_=banks[bk][:, off:off + NT], func=AF.Sigmoid, scale=-1.0), [BK[bk]], [k_sg])
                    p.op("dve", lambda e, sg=sg, ks=ks, d=d, h=h: e.tensor_scalar(
                        out=ks[:, :, h, :], in0=sg.rearrange("q (c t) -> q c t", t=CH),
                        scalar1=omlb_c[:, d, h:h + 1], scalar2=None, op0=ALU.mult),
                        [k_sg, k_omlbc], [k_ks])
                store(KA_c[d, c0:c0 + nchk].rearrange("c q h t -> q c (h t)"), ["KA_c"],
                      ks.rearrange("q c h t -> q c (h t)"), k_ks, sk(k_ks))
            (bs, k_bs) = bst.next()
            for g in range(8):
                bk, off = fm_slot()

                def f(e, bk=bk, off=off, g=g, hT=hT):
                    for k in range(8):
                        r_ = e.matmul(banks[bk][0:64, off:off + NT], lhsT=WA[:, k, 2560 + g * 64:2560 + (g + 1) * 64],
                                      rhs=hT[:, k, :], start=(k == 0), stop=(k == 7))
                    return r_
                p.op("pe", f, [k_hT, k_WA], [BK[bk]])
                p.op("act", lambda e, bk=bk, off=off, g=g, bs=bs: e.activation(
                    out=bs[:, g, :], in_=banks[bk][0:64, off:off + NT], func=AF.Copy), [BK[bk]], [k_bs])
            store(BQK_d[:, :, bpos(tok0):bpos(tok0) + NT], ["BQK_d"], bs, k_bs, sk(k_bs))
            for j in range(NT // 128):
                t0 = tok0 + j * 128

                def tm_mm(bk, col0, ncol=512, j=j, hT=hT):
                    def f(e):
                        for k in range(8):
                            r_ = e.matmul(banks[bk][:, 0:ncol], lhsT=hT[:, k, j * 128:(j + 1) * 128],
                                          rhs=WA[:, k, col0:col0 + ncol], start=(k == 0), stop=(k == 7))
                        return r_
                    return f
                bk = tm_bank()
                (vs, k_vs) = vst.next()
                p.op("pe", tm_mm(bk, 512), [k_hT, k_WA], [BK[bk]])
                p.op("act", lambda e, bk=bk, vs=vs: e.activation(out=vs, in_=banks[bk][:, :], func=AF.Copy),
                     [BK[bk]], [k_vs])
                store(VA_tm[t0:t0 + 128, :], ["VA_tm"], vs, k_vs, sk(k_vs))
                (gs, k_gs) = gst.next()
                bk = tm_bank()
                p.op("pe", tm_mm(bk, 1024), [k_hT, k_WA], [BK[bk]])
                p.op("act", lambda e, bk=bk, gs=gs: e.activation(out=gs[:, 0:512], in_=banks[bk][:, :], func=AF.Silu),
                     [BK[bk]], [k_gs])
                bk = tm_bank()
                p.op("pe", tm_mm(bk, 3584), [k_hT, k_WA], [BK[bk]])
                p.op("act", lambda e, bk=bk, gs=gs: e.activation(out=gs[:, 512:1024], in_=banks[bk][:, :],
                                                                 func=AF.Sigmoid), [BK[bk]], [k_gs])
                store(G_tm[t0:t0 + 128, :], ["G_tm"], gs, k_gs, sk(k_gs))
                bk = tm_bank()
                (vb, k_vb) = vbst.next()
                p.op("pe", tm_mm(bk, 3072), [k_hT, k_WA], [BK[bk]])
                p.op("act", lambda e, bk=bk, vb=vb: e.activation(out=vb, in_=banks[bk][:, :], func=AF.Copy),
                     [BK[bk]], [k_vb])
                store(VB_tm[t0:t0 + 128, :], ["VB_tm"], vb, k_vb, sk(k_vb))
                for d in range(2):
                    bk = tm_bank()
                    (s_, k_s) = sst.next()
                    (u_, k_u) = ust.next()
                    (lf, k_lf) = lfst.next()
                    (kt, k_kt) = ktst.next()
                    p.op("pe", tm_mm(bk, 1536 + d * 512), [k_hT, k_WA], [BK[bk]])
                    p.op("act", lambda e, bk=bk, s_=s_: e.activation(out=s_, in_=banks[bk][:, :], func=AF.Sigmoid),
                         [BK[bk]], [k_s])
                    p.op("dve", lambda e, s_=s_, u_=u_, d=d: e.tensor_tensor(out=u_, in0=s_, in1=omlb_b[:, d, :],
                                                                             op=ALU.mult), [k_s, k_omlb_b], [k_u])
                    p.op("dve", lambda e, s_=s_, u_=u_, d=d: e.tensor_tensor(out=s_, in0=u_, in1=lb_b[:, d, :],
                                                                             op=ALU.add), [k_u, k_lb_b], [k_s])
                    p.op("act", lambda e, s_=s_, lf=lf: e.activation(out=lf, in_=s_, func=AF.Ln), [k_s], [k_lf])
                    store(LF_tm[d, t0:t0 + 128, :], ["LF_tm"], lf, k_lf, sk(k_lf))
                    p.op("dve", lambda e, u_=u_, kt=kt, d=d: e.tensor_tensor(out=kt, in0=omlb_b[:, d, :], in1=u_,
                                                                             op=ALU.subtract), [k_u, k_omlb_b], [k_kt])
                    store(KA_tm[d, t0:t0 + 128, :], ["KA_tm"], kt, k_kt, sk(k_kt))
                bk = tm_bank()
                (g1_, k_g1) = gtst.next()
                (g2_, k_g2) = gt2.next()
                p.op("pe", tm_mm(bk, 4096, 16), [k_hT, k_WA], [BK[bk]])
                p.op("dve", lambda e, bk=bk, g1_=g1_: e.tensor_tensor(out=g1_, in0=banks[bk][:, 0:16], in1=gbb,
                                                                       op=ALU.add), [BK[bk], k_gbb], [k_g1])
                p.op("act", lambda e, g1_=g1_, g2_=g2_: e.activation(out=g2_, in_=g1_, func=AF.Exp, scale=-1.0),
                     [k_g1], [k_g2])
                p.op("act", lambda e, g2_=g2_: e.activation(out=g2_, in_=g2_, func=AF.Ln, bias=onec, scale=1.0),
                     [k_g2, k_one], [k_g2])
                p.op("dve", lambda e, g1_=g1_, g2_=g2_: e.tensor_scalar(
                    out=g1_.rearrange("q (a b) -> q a b", a=2)[:, :, 4:8],
                    in0=g2_.rearrange("q (a b) -> q a b", a=2)[:, :, 4:8],
                    scalar1=-1.0, scalar2=None, op0=ALU.mult), [k_g1, k_g2], [k_g1])
                store(GT_d[t0:t0 + 128, :], ["GT_d"], g1_, k_g1, sk(k_g1))
        p.barrier()


    def phase_A2():
        ar.reset()
        NT = 256
        cw2, k_cw2 = ar.alloc([4, 3], F32)
        p.dma("sp", lambda e: [e.dma_start(
            out=cw2[gg * 64:(gg + 1) * 64, :, w_],
            in_=ab_conv[w_].rearrange("(j gg q) -> gg q j", gg=2, q=64)[gg],
            allow_slow_non_contiguous=True) for w_ in range(3) for gg in range(2)],
            [], [k_cw2], "b0", ndma=6)
        xb2 = Buf(ar, 2, [4, NT + 2], F32)
        acb = Buf(ar, 2, [4, NT], F32)
        qkb2 = Buf(ar, 2, [NT // CH, 4, CH], BF16)
        for st in range(TOK // NT):
            tok0 = st * NT
            c0 = tok0 // CH
            pos0 = bpos(tok0)
            (X, k_X) = xb2.next()
            (acc, k_acc) = acb.next()
            (qo, k_qo) = qkb2.next()
            p.dma("sp", lambda e, X=X, pos0=pos0: [e.dma_start(
                out=X[gg * 64:(gg + 1) * 64, :, :],
                in_=BQK_d.rearrange("q (j gg) t -> gg q j t", gg=2)[gg, :, :, pos0 - 1:pos0 + NT + 1],
                allow_slow_non_contiguous=True) for gg in range(2)], ["BQK_d"], [k_X], sk(k_X), ndma=2)
            for j in range(4):
                p.op("dve", lambda e, X=X, acc=acc, j=j: e.tensor_scalar(
                    out=acc[:, j, :], in0=X[:, j, 0:NT], scalar1=cw2[:, j, 0:1], scalar2=None, op0=ALU.mult),
                    [k_X, k_cw2], [k_acc])
                for w in (1, 2):
                    p.op("dve", lambda e, X=X, acc=acc, j=j, w=w: e.scalar_tensor_tensor(
                        out=acc[:, j, :], in0=X[:, j, w:w + NT], scalar=cw2[:, j, w:w + 1], in1=acc[:, j, :],
                        op0=ALU.mult, op1=ALU.add), [k_X, k_cw2, k_acc], [k_acc])
            p.op("act", lambda e, acc=acc, qo=qo: e.activation(
                out=qo, in_=acc.rearrange("q j (c t) -> q c j t", t=CH), func=AF.Silu), [k_acc], [k_qo])
            p.dma("pool", lambda e, qo=qo, c0=c0: [e.dma_start(
                out=QK_c[c0:c0 + NT // CH, gg].rearrange("c q j t -> q c (j t)"),
                in_=qo[gg * 64:(gg + 1) * 64].rearrange("q c j t -> q c (j t)"),
                allow_slow_non_contiguous=True) for gg in range(2)], [k_qo], ["QK_c"], sk(k_qo), ndma=2)
        p.barrier()

    def phase_B():
        ar.reset()
        Sf = [ar.alloc([4, 128], F32) for _ in range(2)]
        Sb = [ar.alloc([4, 128], BF16) for _ in range(2)]
        Cf = [ar.alloc([4, 132], F32, parts=64) for _ in range(2)]
        Cb = [ar.alloc([4, 132], BF16, parts=64) for _ in range(2)]
        for (t, k) in Sf + Sb + Cf + Cb:
            p.op("pool", lambda e, t=t: e.memset(t, 0.0), [], [k])
        lfb = Buf(ar, 2, [512], F32, parts=64)
        qfb = Buf(ar, 2, [4, CH], BF16)
        kfb = Buf(ar, 2, [4, CH], BF16)
        ktb = Buf(ar, 2, [512], BF16, parts=64)
        vtb = Buf(ar, 2, [512], BF16, parts=64)
        E1b = Buf(ar, 2, [4, 128], F32)
        E2b = Buf(ar, 2, [4, CH], F32)
        EUb = Buf(ar, 2, [512], F32, parts=64)
        qqb = Buf(ar, 2, [4, 128], BF16)
        kkb = Buf(ar, 2, [4, CH], BF16)
        kSb = Buf(ar, 2, [512], BF16, parts=64)
        ATb = Buf(ar, 2, [4, CH], BF16, parts=64)
        osb = Buf(ar, 2, [512], BF16, parts=64)
        Vxb = Buf(ar, 2, [4, 132], BF16, parts=64)
        gtb = Buf(ar, 2, [16], F32, parts=64)
        qkb = Buf(ar, 2, [8, CH], BF16, parts=64)
        argb = Buf(ar, 2, [16], F32, parts=64)
        EXb = Buf(ar, 2, [16], F32, parts=64)
        ATmb = Buf(ar, 2, [4, CH], BF16, parts=64)
        kSmb = Buf(ar, 2, [4, CH], BF16, parts=64)
        adb = Buf(ar, 2, [8], F32, parts=64)
        omb = Buf(ar, 2, [512], BF16, parts=64)
        for (t, k) in Vxb.slots:
            p.op("pool", lambda e, t=t: e.memset(t, 1.0), [], [k])

        def qpos(h):
            return (h % 2) * 4 + h // 2

        def kpos(h):
            return (h % 2) * 4 + 2 + h // 2

        for it in range(NCH):
            for d in range(2):
                if d == 0:
                    c = it
                else:
                    c = (3 - it) if it < 4 else (NCH + 3 - it)
                tok0 = c * CH
                tl = CH - 1 if d == 0 else 0
                (lf, k_lf) = lfb.next()
                (qf, k_qf) = qfb.next()
                (kf, k_kf) = kfb.next()
                (kt, k_kt) = ktb.next()
                (vt, k_vt) = vtb.next()
                load(lf, k_lf, LF_tm[d, tok0:tok0 + CH, :], sk(k_lf), ["LF_tm"])
                load(qf, k_qf, QA_c[c], sk(k_qf), ["QA_c"])
                load(kf, k_kf, KA_c[d, c], sk(k_kf), ["KA_c"])
                load(kt, k_kt, KA_tm[d, tok0:tok0 + CH, :], sk(k_kt), ["KA_tm"])
                load(vt, k_vt, VA_tm[tok0:tok0 + CH, :], sk(k_vt), ["VA_tm"])
                (E1, k_E1) = E1b.next()
                (E2, k_E2) = E2b.next()
                (EU, k_EU) = EUb.next()
                (qq, k_qq) = qqb.next()
                (kk, k_kk) = kkb.next()
                (kS, k_kS) = kSb.next()
                (AT, k_AT) = ATb.next()
                (os_, k_os) = osb.next()

                def p1(e, lf=lf, d=d):
                    for h in range(4):
                        r_ = e.matmul(pv(0, [4, 128])[:, h, :], lhsT=lf[:, h * 128:(h + 1) * 128],
                                      rhs=cm[:, d, 0:2, :], start=True, stop=True)
                    return r_
                p.op("pe", p1, [k_lf, k_cm], [BK[0]])
                p.op("pe", lambda e, lf=lf, d=d: e.matmul(banks[1][0:64, :], lhsT=cm[:, d, 2, :], rhs=lf,
                                                          start=True, stop=True), [k_lf, k_cm], [BK[1]])
                p.op("act", lambda e, E1=E1: e.activation(out=E1, in_=pv(0, [4, 128]), func=AF.Exp), [BK[0]], [k_E1])
                p.op("act", lambda e, E2=E2: e.activation(out=E2, in_=pv(0, [4, 128])[:, :, 0:CH], func=AF.Exp,
                                                          scale=-1.0), [BK[0]], [k_E2])
                p.op("act", lambda e, EU=EU: e.activation(out=EU, in_=banks[1][0:64, :], func=AF.Exp), [BK[1]], [k_EU])
                p.op("dve", lambda e, qq=qq, E1=E1, qf=qf: e.tensor_tensor(
                    out=qq.rearrange("q h (a t) -> q h a t", a=2), in0=E1.rearrange("q h (a t) -> q h a t", a=2),
                    in1=qf.unsqueeze(2).to_broadcast([128, 4, 2, CH]), op=ALU.mult), [k_E1, k_qf], [k_qq])
                p.op("dve", lambda e, kk=kk, E2=E2, kf=kf: e.tensor_tensor(out=kk, in0=E2, in1=kf, op=ALU.mult),
                     [k_E2, k_kf], [k_kk])
                p.op("dve", lambda e, kS=kS, EU=EU, kt=kt: e.tensor_tensor(out=kS, in0=EU, in1=kt, op=ALU.mult),
                     [k_EU, k_kt], [k_kS])

                def p3(e, kk=kk, qq=qq):
                    for h in range(4):
                        r_ = e.matmul(pv(2, [4, CH], parts=64)[:, h, :], lhsT=kk[:, h, :], rhs=qq[:, h, 0:CH],
                                      start=True, stop=True)
                    return r_
                p.op("pe", p3, [k_kk, k_qq], [BK[2]])
                p.op("dve", lambda e, AT=AT, d=d: e.tensor_tensor(
                    out=AT, in0=pv(2, [4, CH], parts=64),
                    in1=cm[:, d, 3, :].unsqueeze(1).to_broadcast([64, 4, CH]), op=ALU.mult), [BK[2], k_cm], [k_AT])

                def p4(e, AT=AT, vt=vt, qq=qq, d=d):
                    for h in range(4):
                        e.matmul(banks[3][0:64, h * 128:(h + 1) * 128], lhsT=AT[:, h, :],
                                 rhs=vt[:, h * 128:(h + 1) * 128], start=True, stop=False)
                        r_ = e.matmul(banks[3][0:64, h * 128:(h + 1) * 128], lhsT=qq[:, h, CH:2 * CH],
                                      rhs=Sb[d][0][:, h, :], start=False, stop=True)
                    return r_
                p.op("pe", p4, [k_AT, k_vt, k_qq, Sb[d][1]], [BK[3]])

                def p5(e, kS=kS, vt=vt):
                    for h in range(4):
                        r_ = e.matmul(pv(4, [4, 128])[:, h, :], lhsT=kS[:, h * 128:(h + 1) * 128],
                                      rhs=vt[:, h * 128:(h + 1) * 128], start=True, stop=True)
                    return r_
                p.op("pe", p5, [k_kS, k_vt], [BK[4]])
                p.op("act", lambda e, os_=os_: e.activation(out=os_, in_=banks[3][0:64, :], func=AF.Copy),
                     [BK[3]], [k_os])
                store(O_d[d][tok0:tok0 + CH, 0:512], ["O%d" % d], os_, k_os, sk(k_os))
                for h in range(4):
                    p.op("dve", lambda e, h=h, d=d, E1=E1, tl=tl: e.scalar_tensor_tensor(
                        out=Sf[d][0][:, h, :], in0=Sf[d][0][:, h, :], scalar=E1[:, h, CH + tl:CH + tl + 1],
                        in1=pv(4, [4, 128])[:, h, :], op0=ALU.mult, op1=ALU.add),
                        [Sf[d][1], k_E1, BK[4]], [Sf[d][1]])
                p.op("act", lambda e, d=d: e.activation(out=Sb[d][0], in_=Sf[d][0], func=AF.Copy),
                     [Sf[d][1]], [Sb[d][1]])

                (Vx, k_Vx) = Vxb.next()
                (gt, k_gt) = gtb.next()
                (qk, k_qk) = qkb.next()
                (arg, k_arg) = argb.next()
                (EX, k_EX) = EXb.next()
                (ATm, k_ATm) = ATmb.next()
                (kSm, k_kSm) = kSmb.next()
                (ad, k_ad) = adb.next()
                (om, k_om) = omb.next()
                load(qk.rearrange("q (gg j) t -> q gg (j t)", gg=2), k_qk,
                     QK_c[c].rearrange("gg q j t -> q gg (j t)"), sk(k_qk), ["QK_c"])
                load(Vx[:, :, 0:128], k_Vx, VB_tm[tok0:tok0 + CH, :].rearrange("t (h e) -> t h e", h=4),
                     sk(k_Vx), ["VB_tm"])
                load(gt, k_gt, GT_d[tok0:tok0 + CH, :], sk(k_gt), ["GT_d"])

                lfc = gt[:, 8 * d + 4:8 * d + 8]
                igc = gt[:, 8 * d:8 * d + 4]

                def pg(e, lfc=lfc, d=d):
                    e.matmul(banks[6][0:64, 0:4], lhsT=cm[:, d, 1, :], rhs=lfc, start=True, stop=True)
                    e.matmul(banks[6][0:64, 4:8], lhsT=cm[:, d, 2, :], rhs=lfc, start=True, stop=True)
                    return e.matmul(banks[6][0:64, 8:12], lhsT=ones64, rhs=lfc, start=True, stop=True)
                p.op("pe", pg, [k_gt, k_cm, k_ones64], [BK[6]])
                p.op("dve", lambda e, arg=arg, igc=igc: e.tensor_tensor(out=arg[:, 0:4], in0=igc,
                                                                        in1=banks[6][0:64, 0:4], op=ALU.subtract),
                     [k_gt, BK[6]], [k_arg])
                p.op("dve", lambda e, arg=arg, igc=igc: e.tensor_tensor(out=arg[:, 4:8], in0=banks[6][0:64, 4:8],
                                                                        in1=igc, op=ALU.add), [k_gt, BK[6]], [k_arg])
                p.op("dve", lambda e, arg=arg: e.tensor_scalar(out=arg[:, 8:12], in0=banks[6][0:64, 0:4],
                                                               scalar1=-1.0, scalar2=LN8, op0=ALU.mult, op1=ALU.add),
                     [BK[6]], [k_arg])
                p.op("dve", lambda e, arg=arg: e.tensor_copy(out=arg[:, 12:16], in_=banks[6][0:64, 8:12]),
                     [BK[6]], [k_arg])
                p.op("act", lambda e, arg=arg, EX=EX: e.activation(out=EX, in_=arg, func=AF.Exp), [k_arg], [k_EX])

                def pst(e, qk=qk):
                    for h in range(4):
                        e.matmul(pv(7, [4, CH], parts=64)[:, h, :], lhsT=qk[:, kpos(h), :], rhs=qk[:, qpos(h), :],
                                 start=True, stop=True)
                    for h in range(4):
                        r_ = e.transpose(out=pv(7, [4, CH], BF16, parts=64, off=256)[:, h, :], in_=qk[:, kpos(h), :],
                                         identity=identb[0:64, 0:64])
                    return r_
                p.op("pe", pst, [k_qk, k_identb], [BK[7]])
                for h in range(4):
                    p.op("dve", lambda e, h=h, ATm=ATm, EX=EX, d=d: e.scalar_tensor_tensor(
                        out=ATm[:, h, :], in0=pv(7, [4, CH], parts=64)[:, h, :], scalar=EX[:, h:h + 1],
                        in1=cm[:, d, 3, :], op0=ALU.mult, op1=ALU.mult), [BK[7], k_EX, k_cm], [k_ATm])
                p.op("dve", lambda e, kSm=kSm, EX=EX: e.tensor_tensor(
                    out=kSm, in0=pv(7, [4, CH], BF16, parts=64, off=256),
                    in1=EX[:, 4:8].unsqueeze(2).to_broadcast([64, 4, CH]), op=ALU.mult), [BK[7], k_EX], [k_kSm])

                def pn(e, ATm=ATm, Vx=Vx, qk=qk, d=d):
                    for h in range(4):
                        bk = 0 if h < 2 else 1
                        o = pv(bk, [2, 132], parts=64)[:, h % 2, 0:129]
                        e.matmul(o, lhsT=ATm[:, h, :], rhs=Vx[:, h, 0:129], start=True, stop=False)
                        r_ = e.matmul(o, lhsT=qk[:, qpos(h), :], rhs=Cb[d][0][:, h, 0:129], start=False, stop=True)
                    return r_
                p.op("pe", pn, [k_ATm, k_Vx, k_qk, Cb[d][1]], [BK[0], BK[1]])

                def pc(e, kSm=kSm, Vx=Vx):
                    for h in range(4):
                        bk = 2 if h < 2 else 4
                        r_ = e.matmul(pv(bk, [2, 132], parts=64)[:, h % 2, 0:129], lhsT=kSm[:, h, :],
                                      rhs=Vx[:, h, 0:129], start=True, stop=True)
                    return r_
                p.op("pe", pc, [k_kSm, k_Vx], [BK[2], BK[4]])
                for hb in range(2):
                    p.op("act", lambda e, hb=hb, ad=ad: e.activation(
                        out=ad[:, 2 * hb:2 * hb + 2], in_=pv(hb, [2, 132], parts=64)[:, :, 128], func=AF.Abs),
                        [BK[hb]], [k_ad])
                p.op("dve", lambda e, ad=ad, EX=EX: e.tensor_tensor(out=ad[:, 0:4], in0=ad[:, 0:4], in1=EX[:, 8:12],
                                                                    op=ALU.max), [k_ad, k_EX], [k_ad])
                p.op("dve", lambda e, ad=ad: e.reciprocal(out=ad[:, 4:8], in_=ad[:, 0:4]), [k_ad], [k_ad])
                for h in range(4):
                    p.op("act", lambda e, h=h, om=om, ad=ad: e.activation(
                        out=om[:, h * 128:(h + 1) * 128], in_=pv(h // 2, [2, 132], parts=64)[:, h % 2, 0:128],
                        func=AF.Copy, scale=ad[:, 4 + h:5 + h]), [BK[h // 2], k_ad], [k_om])
                store(O_d[d][tok0:tok0 + CH, 512:1024], ["O%d" % d], om, k_om, sk(k_om))
                for h in range(4):
                    bk = 2 if h < 2 else 4
                    p.op("dve", lambda e, h=h, d=d, EX=EX, bk=bk: e.scalar_tensor_tensor(
                        out=Cf[d][0][:, h, 0:129], in0=Cf[d][0][:, h, 0:129], scalar=EX[:, 12 + h:13 + h],
                        in1=pv(bk, [2, 132], parts=64)[:, h % 2, 0:129], op0=ALU.mult, op1=ALU.add),
                        [Cf[d][1], k_EX, BK[bk]], [Cf[d][1]])
                p.op("act", lambda e, d=d: e.activation(out=Cb[d][0], in_=Cf[d][0], func=AF.Copy),
                     [Cf[d][1]], [Cb[d][1]])
        p.barrier()

    def phase_C1():
        ar.reset()
        WO, k_WO = ar.alloc([8, D], BF16)
        load_weight_bf16(WO, k_WO, ab_w_out, 8, "wO")
        OG, k_OG = ar.alloc([D], F32)
        load(OG, k_OG, ab_out_g.unsqueeze(0).to_broadcast([128, D]), "c1og")
        ofb = Buf(ar, 2, [D], BF16)
        obb = Buf(ar, 2, [D], BF16)
        o32b = Buf(ar, 2, [D], F32)
        gb = Buf(ar, 2, [D], BF16)
        xb = Buf(ar, 2, [D], F32)
        sqb = Buf(ar, 2, [D], F32)
        s8b = Buf(ar, 2, [16], F32)
        omb = Buf(ar, 2, [D], BF16)
        oTb = Buf(ar, 2, [8, 128], BF16)
        ssb = Buf(ar, 2, [4], F32)
        xob = Buf(ar, 2, [D], F32)
        for t in range(TOK // 128):
            t0 = t * 128
            r = 1 if t0 < NCTX else 0
            (of, k_of) = ofb.next()
            (ob, k_ob) = obb.next()
            (g, k_g) = gb.next()
            (xt, k_xt) = xb.next()
            (sq, k_sq) = sqb.next()
            (s8, k_s8) = s8b.next()
            (om, k_om) = omb.next()
            (oT, k_oT) = oTb.next()
            load(of, k_of, O_d[0][t0:t0 + 128, :], sk(k_of), ["O0"])
            load(ob, k_ob, O_d[1][t0:t0 + 128, :], sk(k_ob), ["O1"])
            load(g, k_g, G_tm[t0:t0 + 128, :], sk(k_g), ["G_tm"])
            load(xt, k_xt, xs[t0:t0 + 128, :], sk(k_xt))
            (o32, k_o32) = o32b.next()
            p.op("dve", lambda e, of=of, ob=ob, o32=o32: e.tensor_tensor(out=o32, in0=of, in1=ob, op=ALU.add),
                 [k_of, k_ob], [k_o32])
            of, k_of = o32, k_o32
            p.op("act", lambda e, of=of, sq=sq: e.activation(out=sq, in_=of, func=AF.Square), [k_of], [k_sq])
            p.op("dve", lambda e, sq=sq, s8=s8: e.tensor_reduce(
                out=s8[:, 0:8], in_=sq.rearrange("q (h e) -> q h e", h=8), axis=AX.X, op=ALU.add), [k_sq], [k_s8])
            rstd_from_ssq(s8[:, 0:8], k_s8, s8[:, 8:16], k_s8, 128)
            p.op("dve", lambda e, of=of, s8=s8: e.tensor_tensor(
                out=of.rearrange("q (h e) -> q h e", h=8), in0=of.rearrange("q (h e) -> q h e", h=8),
                in1=s8[:, 8:16].unsqueeze(2).to_broadcast([128, 8, 128]), op=ALU.mult), [k_of, k_s8], [k_of])
            p.op("dve", lambda e, of=of: e.tensor_tensor(out=of, in0=of, in1=OG, op=ALU.mult), [k_of, k_OG], [k_of])
            p.op("dve", lambda e, of=of, g=g, om=om: e.tensor_tensor(out=om, in0=of, in1=g, op=ALU.mult),
                 [k_of, k_g], [k_om])

            def tr(e, om=om):
                for k in range(8):
                    r_ = e.transpose(out=pv(0, [8, 128], BF16)[:, k, :], in_=om[:, k * 128:(k + 1) * 128],
                                     identity=identb)
                return r_
            p.op("pe", tr, [k_om, k_identb], [BK[0]])
            p.op("act", lambda e, oT=oT: e.activation(out=oT, in_=pv(0, [8, 128], BF16), func=AF.Copy),
                 [BK[0]], [k_oT])
            pyb = (1 + 2 * (t % 2), 2 + 2 * (t % 2))
            for hf in range(2):
                def mm(e, hf=hf, oT=oT, bk=pyb[hf]):
                    for k in range(8):
                        r_ = e.matmul(banks[bk][:, :], lhsT=oT[:, k, :], rhs=WO[:, k, hf * 512:(hf + 1) * 512],
                                      start=(k == 0), stop=(k == 7))
                    return r_
                p.op("pe", mm, [k_oT, k_WO], [BK[pyb[hf]]])
            residual_out(pyb, xt, k_xt, GG1, k_GG1, r, XM[t0:t0 + 128, :], "XM", (sqb, ssb, xob), "sxm")
        p.barrier()

    def phase_FFN(l, src, skey, dst, dkey, ntok, ctx_tokens):
        ar.reset()
        W1, k_W1 = ar.alloc([8, 2 * DFF], BF16)
        W2, k_W2 = ar.alloc([22, D], BF16)
        load_weight_bf16(W1, k_W1, ffn_w_in[l], 8, "w1")
        load_weight_bf16(W2, k_W2, ffn_w_out[l], 22, "w2")
        NTM = 512
        xb = Buf(ar, 2, [D], F32)
        sqb = Buf(ar, 1, [D], BF16)
        ssb = Buf(ar, 4, [4], F32)
        xnb = Buf(ar, 1, [D], F32)
        hTb = Buf(ar, 1, [8, NTM], BF16)
        aTb = Buf(ar, 1, [22, NTM], BF16)
        sgb = Buf(ar, 2, [NTM], F32)
        xob = Buf(ar, 1, [D], F32)
        fmb = [2, 3, 4, 5]
        fm_i = [0]

        def fm_slot():
            i = fm_i[0]
            fm_i[0] = (i + 1) % 4
            return fmb[i]

        sts = []
        t_ = 0
        if ctx_tokens:
            sts.append((0, ctx_tokens))
            t_ = ctx_tokens
        while t_ < ntok:
            sts.append((t_, NTM))
            t_ += NTM
        assert t_ == ntok
        for (tok0, NT) in sts:
            r = 1 if tok0 < ctx_tokens else 0
            (hT, k_hT) = hTb.next()
            for j in range(NT // 128):
                t0 = tok0 + j * 128
                prep_tile(src[t0:t0 + 128, :], hT, k_hT, j * 128, gm2, k_gm2, 3, r, (xb, sqb, ssb, xnb))
            (aT, k_aT) = aTb.next()
            for cb in range(22):
                bg = fm_slot()
                bu = fm_slot()
                (sg, k_sg) = sgb.next()

                def mm(e, bk, col0, hT=hT, NT=NT):
                    for k in range(8):
                        r_ = e.matmul(banks[bk][:, 0:NT], lhsT=W1[:, k, col0:col0 + 128], rhs=hT[:, k, 0:NT],
                                      start=(k == 0), stop=(k == 7))
                    return r_
                p.op("pe", lambda e, bg=bg, cb=cb, mm=mm: mm(e, bg, cb * 128), [k_hT, k_W1], [BK[bg]])
                p.op("pe", lambda e, bu=bu, cb=cb, mm=mm: mm(e, bu, DFF + cb * 128), [k_hT, k_W1], [BK[bu]])
                p.op("act", lambda e, bg=bg, sg=sg, NT=NT: e.activation(out=sg[:, 0:NT], in_=banks[bg][:, 0:NT],
                                                                        func=AF.Silu), [BK[bg]], [k_sg])
                p.op("dve", lambda e, bu=bu, sg=sg, aT=aT, cb=cb, NT=NT: e.tensor_tensor(
                    out=aT[:, cb, 0:NT], in0=sg[:, 0:NT], in1=banks[bu][:, 0:NT], op=ALU.mult),
                    [k_sg, BK[bu]], [k_aT])
            for j in range(NT // 128):
                t0 = tok0 + j * 128
                pyb = (6, 7)
                for hf in range(2):
                    def mm2(e, hf=hf, aT=aT, j=j, bk=pyb[hf]):
                        for cb in range(22):
                            r_ = e.matmul(banks[bk][:, :], lhsT=aT[:, cb, j * 128:(j + 1) * 128],
                                          rhs=W2[:, cb, hf * 512:(hf + 1) * 512], start=(cb == 0), stop=(cb == 21))
                        return r_
                    p.op("pe", mm2, [k_aT, k_W2], [BK[pyb[hf]]])
                (xt, k_xt) = xb.next()
                load(xt, k_xt, src[t0:t0 + 128, :], sk(k_xt))
                residual_out(pyb, xt, k_xt, GG2, k_GG2, r, dst[t0:t0 + 128, :], dkey, (sqb, ssb, xob), "sff")
        p.barrier()

    state = {}

    def phase_D():
        ar.reset()
        state["hi0"] = ar.hi
        KT, k_KT = ar.alloc([2, TOK], BF16, persist=True)
        Vs, k_Vs = ar.alloc([TOK // 128, 2, 132], BF16, persist=True)
        state["KT"] = (KT, k_KT)
        state["Vs"] = (Vs, k_Vs)
        p.op("pool", lambda e: e.memset(Vs, 1.0), [], [k_Vs])
        WQ, k_WQ = ar.alloc([8, 1536], BF16)
        load_weight_bf16(WQ, k_WQ, attn_w_qkv, 8, "wq")
        QKG, k_QKG = ar.alloc([10, 128], F32)
        load(QKG[:, 0:8, :], k_QKG, attn_qk_g[0:1, :].unsqueeze(1).to_broadcast([128, 8, 128]), "d0")
        load(QKG[:, 8:10, :], k_QKG, attn_qk_g[1:2, :].unsqueeze(1).to_broadcast([128, 2, 128]), "d1")
        p.op("dve", lambda e: e.tensor_scalar(out=QKG[:, 0:8, :], in0=QKG[:, 0:8, :], scalar1=128.0 ** -0.5,
                                              scalar2=None, op0=ALU.mult), [k_QKG], [k_QKG])
        NT = 256
        xb = Buf(ar, 2, [D], F32)
        sqb = Buf(ar, 1, [D], BF16)
        ssb = Buf(ar, 2, [4], F32)
        xnb = Buf(ar, 2, [D], F32)
        hTb = Buf(ar, 2, [8, NT], BF16)
        rpb = Buf(ar, 2, [2, 128], F32)
        sq2 = Buf(ar, 2, [10, 128], F32)
        s10 = Buf(ar, 2, [20], F32)
        t1b = Buf(ar, 2, [10, 128], F32)
        t2b = Buf(ar, 2, [10, 128], F32)
        qrb = Buf(ar, 2, [10, 128], BF16)
        qsb = Buf(ar, 2, [8, 128], BF16)
        def prep_st(st):
            tok0 = st * NT
            r = 1 if tok0 < NCTX else 0
            (hT, k_hT) = hTb.next()
            for j in range(NT // 128):
                t0 = tok0 + j * 128
                prep_tile(X1[t0:t0 + 128, :], hT, k_hT, j * 128, gm1, k_gm1, 0, r, (xb, sqb, ssb, xnb))
            return hT, k_hT

        nxt = prep_st(0)
        for st in range(TOK // NT):
            tok0 = st * NT
            is_ctx = tok0 < NCTX
            r = 1 if is_ctx else 0
            (hT, k_hT) = nxt if (PIPE_PREP or st == 0) else prep_st(st)
            if PIPE_PREP and st + 1 < TOK // NT:
                nxt = prep_st(st + 1)
            for j in range(NT // 128):
                t0 = tok0 + j * 128
                tile = t0 // 128
                own = (not is_ctx) and (t0 - NCTX) < OWN
                nh = 10 if own else 2
                h0 = 0 if own else 8
                def mm(e, bk, col0, j=j, hT=hT):
                    for k in range(8):
                        r_ = e.matmul(banks[bk][:, :], lhsT=hT[:, k, j * 128:(j + 1) * 128],
                                      rhs=WQ[:, k, col0:col0 + 512], start=(k == 0), stop=(k == 7))
                    return r_
                p.op("pe", lambda e, mm=mm: mm(e, 4, 1024), [k_hT, k_WQ], [BK[4]])
                if own:
                    p.op("pe", lambda e, mm=mm: mm(e, 2, 0), [k_hT, k_WQ], [BK[2]])
                    p.op("pe", lambda e, mm=mm: mm(e, 3, 512), [k_hT, k_WQ], [BK[3]])
                p.op("act", lambda e, tile=tile: e.activation(
                    out=Vs[:, tile, :, 0:128], in_=pv(4, [4, 128])[:, 2:4, :], func=AF.Copy), [BK[4]], [k_Vs])
                (sq, k_sq) = sq2.next()
                (s1, k_s1) = s10.next()
                (t1, k_t1) = t1b.next()
                (t2, k_t2) = t2b.next()
                (qr, k_qr) = qrb.next()
                srcs = []
                if own:
                    srcs += [(2, 0, 4), (3, 4, 4)]
                srcs += [(4, 8, 2)]
                for (bk, hh, n) in srcs:
                    p.op("act", lambda e, bk=bk, hh=hh, n=n, sq=sq: e.activation(
                        out=sq[:, hh:hh + n, :], in_=pv(bk, [4, 128])[:, 0:n, :], func=AF.Square), [BK[bk]], [k_sq])
                p.op("dve", lambda e, sq=sq, s1=s1, h0=h0, nh=nh: e.tensor_reduce(
                    out=s1[:, h0:h0 + nh], in_=sq[:, h0:h0 + nh, :], axis=AX.X, op=ALU.add), [k_sq], [k_s1])
                rstd_from_ssq(s1[:, h0:h0 + nh], k_s1, s1[:, 10 + h0:10 + h0 + nh], k_s1, 128)
                for (bk, hh, n) in srcs:
                    p.op("dve", lambda e, bk=bk, hh=hh, n=n, t1=t1, s1=s1: e.tensor_tensor(
                        out=t1[:, hh:hh + n, :], in0=pv(bk, [4, 128])[:, 0:n, :],
                        in1=s1[:, 10 + hh:10 + hh + n].unsqueeze(2).to_broadcast([128, n, 128]), op=ALU.mult),
                        [BK[bk], k_s1], [k_t1])
                if is_ctx:
                    p.op("dve", lambda e, t1=t1, qr=qr: e.tensor_tensor(
                        out=qr[:, 8:10, :], in0=t1[:, 8:10, :], in1=QKG[:, 8:10, :], op=ALU.mult),
                        [k_t1, k_QKG], [k_qr])
                else:
                    (rp, k_rp) = rpb.next()
                    lt0 = t0 - NCTX
                    load(rp, k_rp, c_rope[lt0:lt0 + 128], sk(k_rp))
                    sl = slice(h0, h0 + nh)
                    p.op("dve", lambda e, t1=t1, sl=sl: e.tensor_tensor(
                        out=t1[:, sl, :], in0=t1[:, sl, :], in1=QKG[:, sl, :], op=ALU.mult), [k_t1, k_QKG], [k_t1])
                    p.op("dve", lambda e, t1=t1, t2=t2, rp=rp, sl=sl, nh=nh: e.tensor_tensor(
                        out=t2[:, sl, :], in0=t1[:, sl, :],
                        in1=rp[:, 0, :].unsqueeze(1).to_broadcast([128, nh, 128]), op=ALU.mult),
                        [k_t1, k_rp], [k_t2])

                    def v4(a, sl=sl):
                        return a[:, sl, :].rearrange("q h (a b) -> q h a b", a=2)
                    for (ho, hi) in ((0, 32), (32, 0)):
                        p.op("dve", lambda e, t1=t1, sq=sq, rp=rp, ho=ho, hi=hi, nh=nh, v4=v4: e.tensor_tensor(
                            out=v4(sq)[:, :, :, ho:ho + 32], in0=v4(t1)[:, :, :, hi:hi + 32],
                            in1=rp[:, 1, :].rearrange("q (a b) -> q a b", a=2)[:, :, ho:ho + 32]
                            .unsqueeze(1).to_broadcast([128, nh, 2, 32]), op=ALU.mult),
                            [k_t1, k_rp], [k_sq])
                    p.op("dve", lambda e, t2=t2, sq=sq, qr=qr, sl=sl: e.tensor_tensor(
                        out=qr[:, sl, :], in0=t2[:, sl, :], in1=sq[:, sl, :], op=ALU.add), [k_t2, k_sq], [k_qr])
                def trk(e, qr=qr):
                    for g in range(2):
                        r_ = e.transpose(out=pv(5, [8, 128], BF16)[:, g, :], in_=qr[:, 8 + g, :], identity=identb)
                    return r_
                p.op("pe", trk, [k_qr, k_identb], [BK[5]])
                p.op("act", lambda e, t0=t0: e.activation(out=KT[:, :, t0:t0 + 128],
                                                          in_=pv(5, [8, 128], BF16)[:, 0:2, :], func=AF.Copy),
                     [BK[5]], [k_KT])
                if own:
                    (qs, k_qs) = qsb.next()

                    def trq(e, qr=qr):
                        for h in range(8):
                            r_ = e.transpose(out=pv(6, [8, 128], BF16)[:, h, :], in_=qr[:, h, :], identity=identb)
                        return r_
                    p.op("pe", trq, [k_qr, k_identb], [BK[6]])
                    p.op("act", lambda e, qs=qs: e.activation(out=qs, in_=pv(6, [8, 128], BF16), func=AF.Copy),
                         [BK[6]], [k_qs])
                    store(QT_d[(t0 - NCTX) // 128], ["QT_d"], qs, k_qs, sk(k_qs))
        p.barrier()

    def phase_E():
        ar.reset()
        (KT, k_KT) = state["KT"]
        (Vs, k_Vs) = state["Vs"]
        WO, k_WO = ar.alloc([8, D], BF16)
        load_weight_bf16(WO, k_WO, attn_w_out, 8, "wo1")
        qTb = Buf(ar, 2, [8, 128], BF16)
        xb = Buf(ar, 2, [D], F32)
        PTb = Buf(ar, 4, [512], BF16)
        atb = Buf(ar, 2, [8, 128], BF16)
        aTb = Buf(ar, 2, [8, 128], BF16)
        rdb = Buf(ar, 2, [4], F32)
        rdnb = Buf(ar, 2, [512], F32)
        onesb, k_onesb = ar.alloc([128], BF16)
        p.op("pool", lambda e: e.memset(onesb, 1.0), [], [k_onesb])
        sqb = Buf(ar, 1, [D], BF16)
        ssb = Buf(ar, 2, [4], F32)
        xob = Buf(ar, 2, [D], F32)
        NKT = TOK // 128
        NQ = OWN // 128
        units = [(qi, g, kt) for qi in range(NQ) for g in range(2) for kt in range(NKT)]
        qbuf = {}

        def get_q(qi):
            if qi not in qbuf:
                (qT, k_qT) = qTb.next()
                load(qT, k_qT, QT_d[qi], sk(k_qT), ["QT_d"])
                qbuf[qi] = (qT, k_qT)
            return qbuf[qi]

        pts = {}

        def issue_score(u):
            (qi, g, kt) = units[u]
            (qT, k_qT) = get_q(qi)
            sb_ = u % 2
            (PT, k_PT) = PTb.next()
            pts[u] = (PT, k_PT)
            p.op("pe", lambda e, sb_=sb_, g=g, kt=kt, qT=qT: e.matmul(
                banks[sb_][:, :], lhsT=KT[:, g, kt * 128:(kt + 1) * 128],
                rhs=qT[:, 4 * g:4 * g + 4, :].rearrange("q h t -> q (h t)"), start=True, stop=True),
                [k_KT, k_qT], [BK[sb_]])
            p.op("act", lambda e, sb_=sb_, PT=PT: e.activation(out=PT, in_=banks[sb_][:, :], func=AF.Exp),
                 [BK[sb_]], [k_PT])

        for u_ in range(AHEAD):
            issue_score(u_)
        cur = {}
        for u, (qi, g, kt) in enumerate(units):
            if g == 0 and kt == 0:
                (xt, k_xt) = xb.next()
                (at, k_at) = atb.next()
                (aT, k_aT) = aTb.next()
                load(xt, k_xt, X1[NCTX + qi * 128:NCTX + (qi + 1) * 128, :], sk(k_xt), ["X1"])
                cur = dict(xt=xt, k_xt=k_xt, at=at, k_at=k_at, aT=aT, k_aT=k_aT)
            if kt == 0:
                (rd, k_rd) = rdb.next()
                cur["rd"], cur["k_rd"] = rd, k_rd
            pob = (3 + 2 * g, 4 + 2 * g)
            if AHEAD == 0:
                issue_score(u)
            (PT, k_PT) = pts.pop(u)

            def pvm(e, PT=PT, kt=kt, g=g, pob=pob):
                e.matmul(banks[pob[0]][:, :], lhsT=Vs[:, kt, g, 0:128], rhs=PT,
                         start=(kt == 0), stop=(kt == NKT - 1))
                return e.matmul(banks[pob[1]][:, :], lhsT=onesb, rhs=PT,
                                start=(kt == 0), stop=(kt == NKT - 1))
            if AHEAD > 0 and u + AHEAD < len(units):
                issue_score(u + AHEAD)
            p.op("pe", pvm, [k_PT, k_Vs, k_onesb], [BK[pob[0]], BK[pob[1]]])
            if kt == NKT - 1:
                aT, k_aT = cur["aT"], cur["k_aT"]
                (rdn, k_rdn) = rdnb.next()
                p.op("dve", lambda e, rdn=rdn, pob=pob: e.reciprocal(out=rdn, in_=banks[pob[1]][:, :]),
                     [BK[pob[1]]], [k_rdn])
                p.op("dve", lambda e, rdn=rdn, pob=pob, aT=aT, g=g: e.tensor_tensor(
                    out=aT[:, 4 * g:4 * g + 4, :].rearrange("q h t -> q (h t)"), in0=banks[pob[0]][:, :],
                    in1=rdn, op=ALU.mult), [BK[pob[0]], k_rdn], [k_aT])
                if g == 1:
                    xt, k_xt = cur["xt"], cur["k_xt"]
                    pyb = (2, 7)
                    for hf in range(2):
                        def mm(e, hf=hf, aT=aT, bk=pyb[hf]):
                            for k in range(8):
                                r_ = e.matmul(banks[bk][:, :], lhsT=aT[:, k, :], rhs=WO[:, k, hf * 512:(hf + 1) * 512],
                                              start=(k == 0), stop=(k == 7))
                            return r_
                        p.op("pe", mm, [k_aT, k_WO], [BK[pyb[hf]]])
                    residual_out(pyb, xt, k_xt, GG1, k_GG1, 0, X2[qi * 128:(qi + 1) * 128, :], "X2",
                                 (sqb, ssb, xob), "sx2")
        p.barrier()
        ar.hi = state["hi0"]

    phases = [
        ("ada0", lambda: ada_layer(0)),
        ("A", phase_A),
        ("A2", phase_A2),
        ("B", phase_B),
        ("C1", phase_C1),
        ("C2", lambda: phase_FFN(0, XM, "XM", X1, "X1", TOK, NCTX)),
        ("ada1", lambda: ada_layer(1)),
        ("D", phase_D),
        ("E", phase_E),
        ("F", lambda: phase_FFN(1, X2, "X2", y_out, "y", OWN, 0)),
    ]
    finals = []
    for name, fn in phases:
        fn()
        if stop_after == name:
            break
    else:
        finals = ["y"]
    if debug:
        finals = finals + [k for k in ("ADA_d", "QA_c", "KA_c", "QK_c", "KA_tm", "LF_tm", "VA_tm", "BQK_d", "VB_tm", "GT_d",
                                       "G_tm", "O0", "O1", "XM", "X1", "X2", "QT_d") if k in p.last_writer]
    stats = p.emit(final_wait_keys=finals)
    es.close()
    return nc, stats


def _consts():
    s = np.arange(64)[:, None]
    t = np.arange(64)[None, :]
    cm = np.zeros((64, 2, 4, 64), np.float32)
    tri_f = (s <= t).astype(np.float32)
    cm[:, 0, 0] = tri_f - (s <= 31).astype(np.float32)
    cm[:, 0, 1] = tri_f
    cm[:, 0, 2] = (s > t).astype(np.float32)
    cm[:, 0, 3] = tri_f
    tri_b = (s >= t).astype(np.float32)
    cm[:, 1, 0] = tri_b - (s >= 32).astype(np.float32)
    cm[:, 1, 1] = tri_b
    cm[:, 1, 2] = (s < t).astype(np.float32)
    cm[:, 1, 3] = tri_b
    sel = np.zeros((2, 2, 128), np.float32)
    sel[0, 0, :] = 1.0
    sel[1, 1, :] = 1.0
    pos = np.arange(NLAT)
    row = (pos // 64).astype(np.float32)
    col = (pos % 64).astype(np.float32)
    inv = np.power(np.float32(10000.0), -np.arange(0, 64, 2, dtype=np.float32) / np.float32(64)).astype(np.float32)
    ar_ = (row[:, None] * inv[None, :]).astype(np.float32)
    ac_ = (col[:, None] * inv[None, :]).astype(np.float32)
    rope = np.zeros((NLAT, 2, 128), np.float32)
    rope[:, 0, 0:32] = np.cos(ar_)
    rope[:, 0, 32:64] = np.cos(ar_)
    rope[:, 0, 64:96] = np.cos(ac_)
    rope[:, 0, 96:128] = np.cos(ac_)
    rope[:, 1, 0:32] = -np.sin(ar_)
    rope[:, 1, 32:64] = np.sin(ar_)
    rope[:, 1, 64:96] = -np.sin(ac_)
    rope[:, 1, 96:128] = np.sin(ac_)
    return {"c_ident": np.eye(128, dtype=np.float32), "c_cm": cm, "c_sel": sel}, rope


def make_in_maps(inputs, cores=range(8)):
    f = lambda a: np.ascontiguousarray(np.asarray(a, dtype=np.float32))
    x, c, ctx, c_ctx = f(inputs["x"]), f(inputs["c"]), f(inputs["ctx"]), f(inputs["c_ctx"])
    consts, rope = _consts()
    w_in = f(inputs["ab_w_in"])[0]
    conv = f(inputs["ab_conv"])[0]
    gb = f(inputs["ab_gate_b"])[0].reshape(16)
    lb = f(inputs["hgrn_lb"])
    w_in_r = w_in.copy()
    w_in_r[:, 1536:2048] = w_in[:, 2048:2560]
    w_in_r[:, 2048:2560] = w_in[:, 1536:2048]
    w_in_r[:, 4096:4104] = w_in[:, 4104:4112]
    w_in_r[:, 4104:4112] = w_in[:, 4096:4104]
    conv_r = np.ascontiguousarray(conv[::-1])
    gb_r = np.concatenate([gb[8:16], gb[0:8]])
    lb_r = np.ascontiguousarray(lb[::-1])
    rope_r = np.ascontiguousarray(rope[::-1])
    shared = {
        "ada_w": f(inputs["ada_w"]), "ada_b": f(inputs["ada_b"]), "norm_g": f(inputs["norm_g"]),
        "ffn_w_in": f(inputs["ffn_w_in"]), "ffn_w_out": f(inputs["ffn_w_out"]),
        "ab_out_g": f(inputs["ab_out_g"])[0], "ab_w_out": f(inputs["ab_w_out"])[0],
        "attn_w_qkv": f(inputs["attn_w_qkv"])[0], "attn_qk_g": f(inputs["attn_qk_g"])[0],
        "attn_w_out": f(inputs["attn_w_out"])[0],
    }
    shared.update(consts)
    maps = []
    for core in cores:
        b, half = core // 2, core % 2
        m = dict(shared)
        if half == 0:
            m["xs"] = np.ascontiguousarray(np.concatenate([ctx[b], x[b]], axis=0))
            m.update({"ab_w_in": w_in, "ab_conv": conv, "ab_gate_b": gb, "hgrn_lb": lb, "c_rope": rope})
        else:
            m["xs"] = np.ascontiguousarray(np.concatenate([ctx[b][::-1], x[b][::-1]], axis=0))
            m.update({"ab_w_in": w_in_r, "ab_conv": conv_r, "ab_gate_b": gb_r, "hgrn_lb": lb_r, "c_rope": rope_r})
        m["cvec"] = np.ascontiguousarray(np.stack([c[b], c_ctx], axis=0))
        maps.append(m)
    return maps


_NC_CACHE = {}


def kernel(**inputs):
    if "nc" not in _NC_CACHE:
        _NC_CACHE["nc"] = build_nc()[0]
    nc = _NC_CACHE["nc"]
    maps = make_in_maps(inputs)
    res = run_bass_kernel_spmd(nc, maps, core_ids=list(range(8)))
    out = np.zeros((4, NLAT, D), np.float32)
    for core in range(8):
        b, half = core // 2, core % 2
        y = np.asarray(res.results[core]["y"])
        if half == 0:
            out[b, 0:OWN] = y
        else:
            out[b, OWN:NLAT] = y[::-1]
    return out
```

```python
import contextlib
import math
import numpy as np
import concourse.bass as bass
import concourse.mybir as mybir
from concourse.bass_utils import run_bass_kernel_spmd

F32 = mybir.dt.float32
BF16 = mybir.dt.bfloat16
AF = mybir.ActivationFunctionType
ALU = mybir.AluOpType
AX = mybir.AxisListType

D = 1024
NCTX = 256
NLAT = 4096
TOK = NCTX + NLAT
OWN = 2048
CH = 64
NCH = TOK // CH
DFF = 2816
TOKP = TOK + 4
EPS = 1e-6
LN8 = math.log(8.0)
import os
AHEAD = int(os.environ.get('K_AHEAD', '2'))
PIPE_PREP = int(os.environ.get('K_PIPE', '0'))
ENGS = ("pe", "act", "dve", "pool", "sp")


def bpos(tok):
    return tok + 1 if tok < NCTX else tok + 3


class Op:
    __slots__ = ("eng", "fn", "deps", "is_dma", "semkey", "ndma", "tick", "signal", "idx")


class Prog:
    def __init__(self, nc):
        self.nc = nc
        self.ops = []
        self.last_writer = {}
        self.readers = {}
        self.last_dma = {}
        self.last_eng = {}
        self.barrier_deps = {}
        self.qsem = {}
        self.phase_id = 0

    def _add(self, eng, fn, reads, writes, is_dma=False, semkey=None, ndma=1):
        op = Op()
        op.eng, op.fn, op.is_dma, op.semkey, op.ndma = eng, fn, is_dma, semkey, ndma
        op.idx = len(self.ops)
        op.signal = False
        op.tick = None
        deps = set()
        for r in reads:
            w = self.last_writer.get(r)
            if w is not None:
                deps.add(w)
        for w_ in writes:
            w = self.last_writer.get(w_)
            if w is not None:
                deps.add(w)
            for rd in self.readers.get(w_, ()):
                ro = self.ops[rd]
                if (not is_dma) and (not ro.is_dma) and ro.eng == eng:
                    continue
                deps.add(rd)
        if is_dma:
            prev = self.last_dma.get(semkey)
            if prev is not None:
                deps.add(prev)
            self.last_dma[semkey] = op.idx
        bd = self.barrier_deps.pop(eng, None)
        if bd:
            deps.update(bd)
        deps.discard(op.idx)
        if eng == "pe":
            deps = {d_ for d_ in deps if self.ops[d_].eng != "pe" or self.ops[d_].is_dma}
        op.deps = deps
        for r in reads:
            self.readers.setdefault(r, []).append(op.idx)
        for w_ in writes:
            self.last_writer[w_] = op.idx
            self.readers[w_] = []
        self.last_eng[eng] = op.idx
        self.ops.append(op)
        return op

    def op(self, eng, fn, reads=(), writes=()):
        return self._add(eng, fn, tuple(reads), tuple(writes))

    def dma(self, queue, fn, reads, writes, semkey, ndma=1):
        if semkey.startswith("m") and semkey[1:].isdigit():
            cnt = self.qsem.setdefault(queue, {})
            ph = self.phase_id
            k = (ph, semkey)
            if k not in cnt:
                cnt[k] = len([1 for kk in cnt if kk[0] == ph])
            semkey = "%s%d" % (queue[0], cnt[k])
        else:
            semkey = queue[0] + "_" + semkey
        return self._add(queue, fn, tuple(reads), tuple(writes), True, semkey, ndma)

    def barrier(self):
        self.phase_id += 1
        allprev = set(self.last_eng.values()) | set(self.last_dma.values())
        for e in ENGS:
            s = self.barrier_deps.setdefault(e, set())
            s.update(allprev)

    def emit(self, final_wait_keys=()):
        nc = self.nc
        ops = self.ops
        for o in ops:
            for d in o.deps:
                ops[d].signal = True
        finals = [self.last_writer[k] for k in final_wait_keys]
        for f in finals:
            ops[f].signal = True
        eng_count = {e: 0 for e in ENGS}
        dma_count = {}
        for o in ops:
            if o.is_dma:
                c = dma_count.get(o.semkey, 0) + 16 * o.ndma
                dma_count[o.semkey] = c
                o.tick = c
            elif o.signal:
                eng_count[o.eng] += 1
                o.tick = eng_count[o.eng]
        semkeys = sorted(dma_count.keys())
        with contextlib.ExitStack() as es:
            esem = {e: es.enter_context(nc.semaphore("s_" + e)) for e in ENGS}
            dsem = {k: es.enter_context(nc.semaphore("d_" + str(k))) for k in semkeys}
            block = es.enter_context(nc.Block())

            def sem_of(o):
                return dsem[o.semkey] if o.is_dma else esem[o.eng]

            def stream(engname):
                def body(e):
                    waited = {}
                    for o in ops:
                        if o.eng != engname:
                            continue
                        need = {}
                        for d in o.deps:
                            do = ops[d]
                            key = ("d", do.semkey) if do.is_dma else ("e", do.eng)
                            if need.get(key, (0, None))[0] < do.tick:
                                need[key] = (do.tick, do)
                        for key, (tick, do) in need.items():
                            if waited.get(key, 0) >= tick:
                                continue
                            e.wait_ge(sem_of(do), tick)
                            waited[key] = tick
                        res = o.fn(e)
                        if o.is_dma:
                            assert len(res) == o.ndma, (len(res), o.ndma)
                            for ins in res:
                                ins.then_inc(dsem[o.semkey], 16)
                        elif o.signal:
                            res.then_inc(esem[o.eng], 1)
                    if engname == "sp":
                        for f in finals:
                            fo = ops[f]
                            e.wait_ge(sem_of(fo), fo.tick)
                return body

            block.tensor(stream("pe"))
            block.scalar(stream("act"))
            block.vector(stream("dve"))
            block.gpsimd(stream("pool"))
            block.sync(stream("sp"))
        return eng_count, dma_count, len(ops)


class Arena:
    def __init__(self, t, nbytes):
        self.t = t
        self.n = nbytes
        self.lo = 0
        self.hi = nbytes
        self.cnt = 0
        self.semmap = {}
        self.pidx = 0

    def _view(self, off, shape, dt, parts):
        nel = int(np.prod(shape))
        if dt == F32:
            v = self.t[0:parts, off // 2: off // 2 + nel * 2].bitcast(F32)
        else:
            v = self.t[0:parts, off // 2: off // 2 + nel]
        if len(shape) == 2:
            v = v.rearrange("p (a b) -> p a b", a=shape[0])
        elif len(shape) == 3:
            v = v.rearrange("p (a b c) -> p a b c", a=shape[0], b=shape[1])
        elif len(shape) == 4:
            v = v.rearrange("p (a b c d) -> p a b c d", a=shape[0], b=shape[1], c=shape[2])
        return v

    def alloc(self, shape, dt, parts=128, persist=False):
        nb = int(np.prod(shape)) * (4 if dt == F32 else 2)
        nb = (nb + 63) // 64 * 64
        if persist:
            self.hi -= nb
            off = self.hi
        else:
            off = self.lo
            self.lo += nb
        assert self.lo <= self.hi, ("SBUF arena overflow", self.lo, self.hi)
        self.cnt += 1
        key = "sb%d" % self.cnt
        self.semmap[key] = "m%d" % self.pidx
        self.pidx += 1
        return self._view(off, shape, dt, parts), key

    def reset(self):
        self.lo = 0
        self.pidx = 0


class Buf:
    def __init__(self, ar, n, shape, dt, parts=128):
        self.slots = [ar.alloc(shape, dt, parts) for _ in range(n)]
        self.i = -1

    def next(self):
        self.i = (self.i + 1) % len(self.slots)
        return self.slots[self.i]

    def cur(self):
        return self.slots[self.i]


def build_nc(debug=False, stop_after=None):
    nc = bass.Bass("TRN2", target_bir_lowering=False)
    es = contextlib.ExitStack()

    def din(name, shape, dt=F32):
        return nc.dram_tensor(name, list(shape), dt, kind="ExternalInput").ap()

    dbg_names = []

    def dscr(name, shape, dt=F32):
        if debug:
            dbg_names.append(name)
            return nc.dram_tensor(name, list(shape), dt, kind="ExternalOutput").ap()
        return nc.dram_tensor(name, list(shape), dt, kind="Internal").ap()

    xs = din("xs", [TOK, D])
    cvec = din("cvec", [2, D])
    ada_w = din("ada_w", [2, D, 6 * D])
    ada_b = din("ada_b", [2, 6 * D])
    norm_g = din("norm_g", [2, 4, D])
    ffn_w_in = din("ffn_w_in", [2, D, 2 * DFF])
    ffn_w_out = din("ffn_w_out", [2, DFF, D])
    ab_w_in = din("ab_w_in", [D, 4112])
    ab_conv = din("ab_conv", [3, 512])
    ab_gate_b = din("ab_gate_b", [16])
    hgrn_lb = din("hgrn_lb", [2, 3, 512])
    ab_out_g = din("ab_out_g", [D])
    ab_w_out = din("ab_w_out", [D, D])
    attn_w_qkv = din("attn_w_qkv", [D, 1536])
    attn_qk_g = din("attn_qk_g", [2, 128])
    attn_w_out = din("attn_w_out", [D, D])
    c_ident = din("c_ident", [128, 128])
    c_cm = din("c_cm", [64, 2, 4, 64])
    c_sel = din("c_sel", [2, 2, 128])
    c_rope = din("c_rope", [NLAT, 2, 128])
    y_out = nc.dram_tensor("y", [OWN, D], F32, kind="ExternalOutput").ap()

    QA_c = dscr("QA_c", [NCH, 128, 4, CH], BF16)
    KA_c = dscr("KA_c", [2, NCH, 128, 4, CH], BF16)
    KA_tm = dscr("KA_tm", [2, TOK, 512], BF16)
    LF_tm = dscr("LF_tm", [2, TOK, 512], F32)
    VA_tm = dscr("VA_tm", [TOK, 512], BF16)
    BQK_d = dscr("BQK_d", [64, 8, TOKP], F32)
    QK_c = dscr("QK_c", [NCH, 2, 64, 4, CH], BF16)
    VB_tm = dscr("VB_tm", [TOK, 512], BF16)
    GT_d = dscr("GT_d", [TOK, 16], F32)
    G_tm = dscr("G_tm", [TOK, D], BF16)
    O_d = [dscr("O_f", [TOK, D], BF16), dscr("O_b", [TOK, D], BF16)]
    XM = dscr("XM", [TOK, D], F32)
    X1 = dscr("X1", [TOK, D], F32)
    X2 = dscr("X2", [OWN, D], F32)
    QT_d = dscr("QT_d", [OWN // 128, 128, 8, 128], BF16)
    ADA_d = dscr("ADA_d", [2, 2, 6 * D], F32)

    ARB = 206 * 1024
    arena_t = es.enter_context(nc.sbuf_tensor("arena", [128, ARB // 2], BF16))
    ar = Arena(arena_t, ARB)
    banks = [es.enter_context(nc.psum_tensor("psb%d" % i, [128, 512], F32)) for i in range(8)]
    BK = ["bank%d" % i for i in range(8)]

    p = Prog(nc)

    def pv(i, shape, dt=F32, parts=128, off=0):
        nel = int(np.prod(shape))
        if dt == F32:
            v = banks[i][0:parts, off:off + nel]
        else:
            v = banks[i][0:parts, off:off + (nel + 1) // 2].bitcast(BF16)
        if len(shape) == 2:
            v = v.rearrange("p (a b) -> p a b", a=shape[0])
        elif len(shape) == 3:
            v = v.rearrange("p (a b c) -> p a b c", a=shape[0], b=shape[1])
        return v

    def sk(key):
        return ar.semmap[key]

    def load(dst, dkey, src, semkey, rkeys=(), q="sp"):
        p.dma(q, lambda e: [e.dma_start(out=dst, in_=src, allow_slow_non_contiguous=True)], rkeys, [dkey], semkey)

    def store(dst, dkeys, src, skey, semkey, q="pool"):
        p.dma(q, lambda e: [e.dma_start(out=dst, in_=src, allow_slow_non_contiguous=True)], [skey], dkeys, semkey)

    identf, k_identf = ar.alloc([128], F32, persist=True)
    identb, k_identb = ar.alloc([128], BF16, persist=True)
    cm, k_cm = ar.alloc([2, 4, 64], F32, parts=64, persist=True)
    epsc, k_eps = ar.alloc([1], F32, persist=True)
    onec, k_one = ar.alloc([1], F32, persist=True)
    ones64, k_ones64 = ar.alloc([64], F32, parts=64, persist=True)
    sel, k_sel = ar.alloc([2, 128], F32, parts=2, persist=True)
    modc, k_modc = ar.alloc([6, 8, 2], F32, persist=True)
    gm1, k_gm1 = ar.alloc([8, 2], F32, persist=True)
    gm2, k_gm2 = ar.alloc([8, 2], F32, persist=True)
    GG1, k_GG1 = ar.alloc([2, D], F32, persist=True)
    GG2, k_GG2 = ar.alloc([2, D], F32, persist=True)

    load(identf, k_identf, c_ident, "c0")
    p.op("dve", lambda e: e.tensor_copy(out=identb, in_=identf), [k_identf], [k_identb])
    load(cm, k_cm, c_cm, "c1")
    load(sel, k_sel, c_sel.rearrange("r k m -> k r m"), "c2")
    p.op("pool", lambda e: e.memset(epsc, EPS), [], [k_eps])
    p.op("pool", lambda e: e.memset(onec, 1.0), [], [k_one])
    p.op("pool", lambda e: e.memset(ones64, 1.0), [], [k_ones64])

    def rstd_from_ssq(ssq, k_ssq, out, k_out, n, parts=128):
        p.op("act", lambda e: e.activation(out=out, in_=ssq, func=AF.Sqrt, bias=epsc[0:parts, :],
                                           scale=1.0 / n), [k_ssq, k_eps], [k_out])
        p.op("dve", lambda e: e.reciprocal(out=out, in_=out), [k_out], [k_out])

    def ada_layer(l):
        ar.reset()
        scT, k_scT = ar.alloc([8, 2], F32)
        adas, k_adas = ar.alloc([6 * D], F32, parts=2)
        adab, k_adab = ar.alloc([6 * D], F32, parts=2)
        ngc, k_ngc = ar.alloc([4, 8], F32)
        ngb, k_ngb = ar.alloc([2, D], F32)
        wbuf = Buf(ar, 2, [8, 512], F32)
        p.dma("sp", lambda e: [e.dma_start(out=scT[:, :, r_], in_=cvec[r_].rearrange("(k q) -> q k", q=128),
                                           allow_slow_non_contiguous=True) for r_ in range(2)],
              [], [k_scT], "a0", ndma=2)
        p.op("act", lambda e: e.activation(out=scT, in_=scT, func=AF.Silu), [k_scT], [k_scT])
        load(adab, k_adab, ada_b[l:l + 1, :].to_broadcast([2, 6 * D]), "a1")
        p.dma("sp", lambda e: [e.dma_start(out=ngc, in_=norm_g[l].rearrange("v (k q) -> q v k", q=128),
                                           allow_slow_non_contiguous=True)], [], [k_ngc], "a2")
        for n in range(12):
            (wt, k_wt) = wbuf.next()
            load(wt, k_wt, ada_w[l, :, n * 512:(n + 1) * 512].rearrange("(k q) n -> q k n", q=128),
                 "aw%d" % (n % 2))
            bk = n % 2

            def mm(e, wt=wt, bk=bk):
                for k in range(8):
                    r = e.matmul(banks[bk][0:2, :], lhsT=scT[:, k, :], rhs=wt[:, k, :],
                                 start=(k == 0), stop=(k == 7))
                return r
            p.op("pe", mm, [k_scT, k_wt], [BK[bk]])
            p.op("dve", lambda e, bk=bk, n=n: e.tensor_tensor(
                out=adas[:, n * 512:(n + 1) * 512], in0=banks[bk][0:2, :],
                in1=adab[:, n * 512:(n + 1) * 512], op=ALU.add), [BK[bk], k_adab], [k_adas])
        if debug:
            store(ADA_d[l], ["ADA_d"], adas, k_adas, "dbg")
        def colmm(e):
            for v in range(6):
                for k in range(8):
                    c0 = v * D + k * 128
                    r = e.matmul(pv(2, [6, 8, 2])[:, v, k, :], lhsT=adas[:, c0:c0 + 128],
                                 rhs=identf[0:2, 0:2], start=True, stop=True)
            return r
        p.op("pe", colmm, [k_adas, k_identf], [BK[2]])
        p.op("dve", lambda e: e.tensor_copy(out=modc, in_=pv(2, [6, 8, 2])), [BK[2]], [k_modc])
        for (gm, k_gm, vsc, vng) in ((gm1, k_gm1, 1, 0), (gm2, k_gm2, 4, 2)):
            p.op("dve", lambda e, gm=gm, vsc=vsc: e.tensor_scalar(
                out=gm, in0=modc[:, vsc, :, :], scalar1=1.0, scalar2=None, op0=ALU.add),
                [k_modc], [k_gm])
            p.op("dve", lambda e, gm=gm, vng=vng: e.tensor_tensor(
                out=gm, in0=gm, in1=ngc[:, vng, :].unsqueeze(2).to_broadcast([128, 8, 2]), op=ALU.mult),
                [k_gm, k_ngc], [k_gm])
        for (GG, k_GG, vg, vng) in ((GG1, k_GG1, 2, 1), (GG2, k_GG2, 5, 3)):
            load(ngb[:, 0, :], k_ngb, norm_g[l, vng:vng + 1, :].to_broadcast([128, D]), "a3")
            load(ngb[:, 1, :], k_ngb, norm_g[l, vng:vng + 1, :].to_broadcast([128, D]), "a3")
            for r in range(2):
                for hf in range(2):
                    bk = 3 + hf
                    c0 = vg * D + hf * 512
                    p.op("pe", lambda e, bk=bk, c0=c0, r=r: e.matmul(
                        banks[bk][:, :], lhsT=sel[:, r, :], rhs=adas[:, c0:c0 + 512],
                        start=True, stop=True), [k_adas, k_sel], [BK[bk]])
                    p.op("dve", lambda e, bk=bk, GG=GG, r=r, hf=hf: e.tensor_tensor(
                        out=GG[:, r, hf * 512:(hf + 1) * 512], in0=banks[bk][:, :],
                        in1=ngb[:, r, hf * 512:(hf + 1) * 512], op=ALU.mult),
                        [BK[bk], k_ngb], [k_GG])
        p.barrier()

    def load_weight_bf16(dst, dkey, src_rows, kchunks, semkey):
        for k in range(kchunks):
            p.dma("pool", lambda e, k=k: [e.dma_start(out=dst[:, k, :], in_=src_rows[k * 128:(k + 1) * 128, :])],
                  [], [dkey], "W" + str(k % 4))

    def prep_tile(src_ap, hT, k_hT, col0, gm, k_gm, shv, r, bufs, xkeep=None):
        xb, sqb, ssb, xnb = bufs
        if xkeep is None:
            (xt, k_xt) = xb.next()
        else:
            (xt, k_xt) = xkeep
        (sq, k_sq) = sqb.next()
        (ss, k_ss) = ssb.next()
        (xn, k_xn) = xnb.next()
        load(xt, k_xt, src_ap, sk(k_xt))
        p.op("act", lambda e: e.activation(out=sq, in_=xt, func=AF.Square, accum_out=ss[:, 0:1]),
             [k_xt], [k_sq, k_ss])
        rstd_from_ssq(ss[:, 0:1], k_ss, ss[:, 1:2], k_ss, D)
        p.op("act", lambda e: e.activation(out=xn, in_=xt, func=AF.Copy, scale=ss[:, 1:2]),
             [k_xt, k_ss], [k_xn])

        def tr(e):
            for k in range(8):
                r_ = e.transpose(out=pv(k // 4, [4, 128])[:, k % 4, :], in_=xn[:, k * 128:(k + 1) * 128],
                                 identity=identf)
            return r_
        p.op("pe", tr, [k_xn, k_identf], [BK[0], BK[1]])
        for k in range(8):
            p.op("dve", lambda e, k=k: e.tensor_scalar(
                out=hT[:, k, col0:col0 + 128], in0=pv(k // 4, [4, 128])[:, k % 4, :],
                scalar1=gm[:, k, r:r + 1], scalar2=modc[:, shv, k, r:r + 1], op0=ALU.mult, op1=ALU.add),
                [BK[k // 4], k_gm, k_modc], [k_hT])
        return xt, k_xt

    def residual_out(py_banks, xt, k_xt, GG, k_GG, r, dst_ap, dkey, bufs, semkey):
        sqb, ssb, outb = bufs
        (sq, k_sq) = sqb.next()
        (ss, k_ss) = ssb.next()
        (xo, k_xo) = outb.next()
        for hf in range(2):
            bk = py_banks[hf]
            p.op("act", lambda e, bk=bk, hf=hf: e.activation(
                out=sq[:, 0:512], in_=banks[bk][:, :], func=AF.Square, accum_out=ss[:, 2 + hf:3 + hf]),
                [BK[bk]], [k_sq, k_ss])
        p.op("dve", lambda e: e.tensor_tensor(out=ss[:, 0:1], in0=ss[:, 2:3], in1=ss[:, 3:4], op=ALU.add),
             [k_ss], [k_ss])
        rstd_from_ssq(ss[:, 0:1], k_ss, ss[:, 1:2], k_ss, D)
        for hf in range(2):
            bk = py_banks[hf]
            p.op("dve", lambda e, bk=bk, hf=hf: e.scalar_tensor_tensor(
                out=xo[:, hf * 512:(hf + 1) * 512], in0=banks[bk][:, :], scalar=ss[:, 1:2],
                in1=GG[:, r, hf * 512:(hf + 1) * 512], op0=ALU.mult, op1=ALU.mult),
                [BK[bk], k_ss, k_GG], [k_xo])
        p.op("dve", lambda e: e.tensor_tensor(out=xo, in0=xo, in1=xt, op=ALU.add), [k_xo, k_xt], [k_xo])
        store(dst_ap, [dkey], xo, k_xo, sk(k_xo))

    def phase_A():
        ar.reset()
        WA, k_WA = ar.alloc([8, 4112], BF16)
        load_weight_bf16(WA, k_WA, ab_w_in, 8, "wA")
        lbf, k_lbf = ar.alloc([2, 3, 4], F32)
        lbb, k_lbb = ar.alloc([2, 3, 512], F32)
        omlb_c, k_omlbc = ar.alloc([2, 4], F32)
        lb_b, k_lb_b = ar.alloc([2, 512], F32)
        omlb_b, k_omlb_b = ar.alloc([2, 512], F32)
        gbb, k_gbb = ar.alloc([16], F32)
        zt, k_zt = ar.alloc([8, 4], F32, parts=64)
        p.dma("sp", lambda e: [e.dma_start(out=lbf, in_=hgrn_lb.rearrange("r l (h q) -> q r l h", q=128),
                                           allow_slow_non_contiguous=True)], [], [k_lbf], "l0")
        load(lbb, k_lbb, hgrn_lb.rearrange("r l c -> (r l c)").unsqueeze(0).to_broadcast([128, 3072])
             .rearrange("p (r l c) -> p r l c", r=2, l=3), "l1")
        load(gbb, k_gbb, ab_gate_b.unsqueeze(0).to_broadcast([128, 16]), "l2")
        for (t, kt_, o1, ko1, o2, ko2) in ((lbf, k_lbf, omlb_c, k_omlbc, None, None),
                                           (lbb, k_lbb, omlb_b, k_omlb_b, lb_b, k_lb_b)):
            p.op("act", lambda e, t=t: e.activation(out=t, in_=t, func=AF.Exp), [kt_], [kt_])
            p.op("dve", lambda e, t=t, o1=o1: e.tensor_tensor(out=o1, in0=t[:, :, 0, :], in1=t[:, :, 1, :], op=ALU.add),
                 [kt_], [ko1])
            p.op("dve", lambda e, t=t, o1=o1: e.tensor_tensor(out=o1, in0=o1, in1=t[:, :, 2, :], op=ALU.add),
                 [kt_, ko1], [ko1])
            p.op("dve", lambda e, o1=o1: e.reciprocal(out=o1, in_=o1), [ko1], [ko1])
            p.op("dve", lambda e, t=t, o1=o1: e.tensor_tensor(out=o1, in0=o1, in1=t[:, :, 0, :], op=ALU.mult),
                 [kt_, ko1], [ko1])
            if o2 is not None:
                p.op("dve", lambda e, o1=o1, o2=o2: e.tensor_copy(out=o2, in_=o1), [ko1], [ko2])
            p.op("dve", lambda e, o1=o1: e.tensor_scalar(out=o1, in0=o1, scalar1=-1.0, scalar2=1.0,
                                                         op0=ALU.mult, op1=ALU.add), [ko1], [ko1])
        p.op("pool", lambda e: e.memset(zt, 0.0), [], [k_zt])
        for i, pos in enumerate((0, NCTX + 1, NCTX + 2, TOKP - 1)):
            store(BQK_d[:, :, pos:pos + 1], ["BQK_d"], zt[:, :, 0:1], k_zt, "z%d" % i, q="sp")

        NT = 256
        xb = Buf(ar, 2, [D], F32)
        sqb = Buf(ar, 1, [D], BF16)
        ssb = Buf(ar, 2, [4], F32)
        xnb = Buf(ar, 2, [D], F32)
        hTb = Buf(ar, 2, [8, NT], BF16)
        qst = Buf(ar, 2, [NT // CH, 4, CH], BF16)
        kst = Buf(ar, 2, [NT // CH, 4, CH], BF16)
        sgt = Buf(ar, 2, [NT], F32)
        bst = Buf(ar, 2, [8, NT], F32, parts=64)
        vst = Buf(ar, 2, [512], BF16)
        gst = Buf(ar, 2, [D], BF16)
        vbst = Buf(ar, 2, [512], BF16)
        sst = Buf(ar, 2, [512], F32)
        ust = Buf(ar, 2, [512], F32)
        lfst = Buf(ar, 2, [512], F32)
        ktst = Buf(ar, 2, [512], BF16)
        gtst = Buf(ar, 2, [16], F32)
        gt2 = Buf(ar, 2, [16], F32)
        fmb = [2, 3]
        tmb = [4, 5, 6]
        fm_i = [0]
        tm_i = [0]

        def fm_slot():
            i = fm_i[0]
            fm_i[0] = (i + 1) % 4
            return fmb[i // 2], (i % 2) * 256

        def tm_bank():
            i = tm_i[0]
            tm_i[0] = (i + 1) % 3
            return tmb[i]

        def prep_st(st):
            tok0 = st * NT
            r = 1 if tok0 < NCTX else 0
            (hT, k_hT) = hTb.next()
            for j in range(NT // 128):
                t0 = tok0 + j * 128
                prep_tile(xs[t0:t0 + 128, :], hT, k_hT, j * 128, gm1, k_gm1, 0, r, (xb, sqb, ssb, xnb))
            return hT, k_hT

        nxt = prep_st(0)
        for st in range(TOK // NT):
            tok0 = st * NT
            (hT, k_hT) = nxt if (PIPE_PREP or st == 0) else prep_st(st)
            if PIPE_PREP and st + 1 < TOK // NT:
                nxt = prep_st(st + 1)
            c0 = tok0 // CH
            nchk = NT // CH

            def fm_mm(bk, off, col0, hT=hT):
                def f(e):
                    for k in range(8):
                        r_ = e.matmul(banks[bk][:, off:off + NT], lhsT=WA[:, k, col0:col0 + 128], rhs=hT[:, k, :],
                                      start=(k == 0), stop=(k == 7))
                    return r_
                return f
            (qs, k_qs) = qst.next()
            for h in range(4):
                bk, off = fm_slot()
                p.op("pe", fm_mm(bk, off, h * 128), [k_hT, k_WA], [BK[bk]])
                p.op("act", lambda e, bk=bk, off=off, h=h, qs=qs: e.activation(
                    out=qs[:, :, h, :], in_=banks[bk][:, off:off + NT].rearrange("q (c t) -> q c t", t=CH),
                    func=AF.Copy), [BK[bk]], [k_qs])
            store(QA_c[c0:c0 + nchk].rearrange("c q h t -> q c (h t)"), ["QA_c"],
                  qs.rearrange("q c h t -> q c (h t)"), k_qs, sk(k_qs))
            for d in range(2):
                (ks, k_ks) = kst.next()
                for h in range(4):
                    bk, off = fm_slot()
                    (sg, k_sg) = sgt.next()
                    p.op("pe", fm_mm(bk, off, 1536 + d * 512 + h * 128), [k_hT, k_WA], [BK[bk]])
                    p.op("act", lambda e, bk=bk, off=off, sg=sg: e.activation(
                        out=sg, in_=banks[bk][:, off:off + NT], func=AF.Sigmoid, scale=-1.0), [BK[bk]], [k_sg])
                    p.op("dve", lambda e, sg=sg, ks=ks, d=d, h=h: e.tensor_scalar(
                        out=ks[:, :, h, :], in0=sg.rearrange("q (c t) -> q c t", t=CH),
                        scalar1=omlb_c[:, d, h:h + 1], scalar2=None, op0=ALU.mult),
                        [k_sg, k_omlbc], [k_ks])
                store(KA_c[d, c0:c0 + nchk].rearrange("c q h t -> q c (h t)"), ["KA_c"],
                      ks.rearrange("q c h t -> q c (h t)"), k_ks, sk(k_ks))
            (bs, k_bs) = bst.next()
            for g in range(8):
                bk, off = fm_slot()

                def f(e, bk=bk, off=off, g=g, hT=hT):
                    for k in range(8):
                        r_ = e.matmul(banks[bk][0:64, off:off + NT], lhsT=WA[:, k, 2560 + g * 64:2560 + (g + 1) * 64],
                                      rhs=hT[:, k, :], start=(k == 0), stop=(k == 7))
                    return r_
                p.op("pe", f, [k_hT, k_WA], [BK[bk]])
                p.op("act", lambda e, bk=bk, off=off, g=g, bs=bs: e.activation(
                    out=bs[:, g, :], in_=banks[bk][0:64, off:off + NT], func=AF.Copy), [BK[bk]], [k_bs])
            store(BQK_d[:, :, bpos(tok0):bpos(tok0) + NT], ["BQK_d"], bs, k_bs, sk(k_bs))
            for j in range(NT // 128):
                t0 = tok0 + j * 128

                def tm_mm(bk, col0, ncol=512, j=j, hT=hT):
                    def f(e):
                        for k in range(8):
                            r_ = e.matmul(banks[bk][:, 0:ncol], lhsT=hT[:, k, j * 128:(j + 1) * 128],
                                          rhs=WA[:, k, col0:col0 + ncol], start=(k == 0), stop=(k == 7))
                        return r_
                    return f
                bk = tm_bank()
                (vs, k_vs) = vst.next()
                p.op("pe", tm_mm(bk, 512), [k_hT, k_WA], [BK[bk]])
                p.op("act", lambda e, bk=bk, vs=vs: e.activation(out=vs, in_=banks[bk][:, :], func=AF.Copy),
                     [BK[bk]], [k_vs])
                store(VA_tm[t0:t0 + 128, :], ["VA_tm"], vs, k_vs, sk(k_vs))
                (gs, k_gs) = gst.next()
                bk = tm_bank()
                p.op("pe", tm_mm(bk, 1024), [k_hT, k_WA], [BK[bk]])
                p.op("act", lambda e, bk=bk, gs=gs: e.activation(out=gs[:, 0:512], in_=banks[bk][:, :], func=AF.Silu),
                     [BK[bk]], [k_gs])
                bk = tm_bank()
                p.op("pe", tm_mm(bk, 3584), [k_hT, k_WA], [BK[bk]])
                p.op("act", lambda e, bk=bk, gs=gs: e.activation(out=gs[:, 512:1024], in_=banks[bk][:, :],
                                                                 func=AF.Sigmoid), [BK[bk]], [k_gs])
                store(G_tm[t0:t0 + 128, :], ["G_tm"], gs, k_gs, sk(k_gs))
                bk = tm_bank()
                (vb, k_vb) = vbst.next()
                p.op("pe", tm_mm(bk, 3072), [k_hT, k_WA], [BK[bk]])
                p.op("act", lambda e, bk=bk, vb=vb: e.activation(out=vb, in_=banks[bk][:, :], func=AF.Copy),
                     [BK[bk]], [k_vb])
                store(VB_tm[t0:t0 + 128, :], ["VB_tm"], vb, k_vb, sk(k_vb))
                for d in range(2):
                    bk = tm_bank()
                    (s_, k_s) = sst.next()
                    (u_, k_u) = ust.next()
                    (lf, k_lf) = lfst.next()
                    (kt, k_kt) = ktst.next()
                    p.op("pe", tm_mm(bk, 1536 + d * 512), [k_hT, k_WA], [BK[bk]])
                    p.op("act", lambda e, bk=bk, s_=s_: e.activation(out=s_, in_=banks[bk][:, :], func=AF.Sigmoid),
                         [BK[bk]], [k_s])
                    p.op("dve", lambda e, s_=s_, u_=u_, d=d: e.tensor_tensor(out=u_, in0=s_, in1=omlb_b[:, d, :],
                                                                             op=ALU.mult), [k_s, k_omlb_b], [k_u])
                    p.op("dve", lambda e, s_=s_, u_=u_, d=d: e.tensor_tensor(out=s_, in0=u_, in1=lb_b[:, d, :],
                                                                             op=ALU.add), [k_u, k_lb_b], [k_s])
                    p.op("act", lambda e, s_=s_, lf=lf: e.activation(out=lf, in_=s_, func=AF.Ln), [k_s], [k_lf])
                    store(LF_tm[d, t0:t0 + 128, :], ["LF_tm"], lf, k_lf, sk(k_lf))
                    p.op("dve", lambda e, u_=u_, kt=kt, d=d: e.tensor_tensor(out=kt, in0=omlb_b[:, d, :], in1=u_,
                                                                             op=ALU.subtract), [k_u, k_omlb_b], [k_kt])
                    store(KA_tm[d, t0:t0 + 128, :], ["KA_tm"], kt, k_kt, sk(k_kt))
                bk = tm_bank()
                (g1_, k_g1) = gtst.next()
                (g2_, k_g2) = gt2.next()
                p.op("pe", tm_mm(bk, 4096, 16), [k_hT, k_WA], [BK[bk]])
                p.op("dve", lambda e, bk=bk, g1_=g1_: e.tensor_tensor(out=g1_, in0=banks[bk][:, 0:16], in1=gbb,
                                                                       op=ALU.add), [BK[bk], k_gbb], [k_g1])
                p.op("act", lambda e, g1_=g1_, g2_=g2_: e.activation(out=g2_, in_=g1_, func=AF.Exp, scale=-1.0),
                     [k_g1], [k_g2])
                p.op("act", lambda e, g2_=g2_: e.activation(out=g2_, in_=g2_, func=AF.Ln, bias=onec, scale=1.0),
                     [k_g2, k_one], [k_g2])
                p.op("dve", lambda e, g1_=g1_, g2_=g2_: e.tensor_scalar(
                    out=g1_.rearrange("q (a b) -> q a b", a=2)[:, :, 4:8],
                    in0=g2_.rearrange("q (a b) -> q a b", a=2)[:, :, 4:8],
                    scalar1=-1.0, scalar2=None, op0=ALU.mult), [k_g1, k_g2], [k_g1])
                store(GT_d[t0:t0 + 128, :], ["GT_d"], g1_, k_g1, sk(k_g1))
        p.barrier()


    def phase_A2():
        ar.reset()
        NT = 256
        cw2, k_cw2 = ar.alloc([4, 3], F32)
        p.dma("sp", lambda e: [e.dma_start(
            out=cw2[gg * 64:(gg + 1) * 64, :, w_],
            in_=ab_conv[w_].rearrange("(j gg q) -> gg q j", gg=2, q=64)[gg],
            allow_slow_non_contiguous=True) for w_ in range(3) for gg in range(2)],
            [], [k_cw2], "b0", ndma=6)
        xb2 = Buf(ar, 2, [4, NT + 2], F32)
        acb = Buf(ar, 2, [4, NT], F32)
        qkb2 = Buf(ar, 2, [NT // CH, 4, CH], BF16)
        for st in range(TOK // NT):
            tok0 = st * NT
            c0 = tok0 // CH
            pos0 = bpos(tok0)
            (X, k_X) = xb2.next()
            (acc, k_acc) = acb.next()
            (qo, k_qo) = qkb2.next()
            p.dma("sp", lambda e, X=X, pos0=pos0: [e.dma_start(
                out=X[gg * 64:(gg + 1) * 64, :, :],
                in_=BQK_d.rearrange("q (j gg) t -> gg q j t", gg=2)[gg, :, :, pos0 - 1:pos0 + NT + 1],
                allow_slow_non_contiguous=True) for gg in range(2)], ["BQK_d"], [k_X], sk(k_X), ndma=2)
            for j in range(4):
                p.op("dve", lambda e, X=X, acc=acc, j=j: e.tensor_scalar(
                    out=acc[:, j, :], in0=X[:, j, 0:NT], scalar1=cw2[:, j, 0:1], scalar2=None, op0=ALU.mult),
                    [k_X, k_cw2], [k_acc])
                for w in (1, 2):
                    p.op("dve", lambda e, X=X, acc=acc, j=j, w=w: e.scalar_tensor_tensor(
                        out=acc[:, j, :], in0=X[:, j, w:w + NT], scalar=cw2[:, j, w:w + 1], in1=acc[:, j, :],
                        op0=ALU.mult, op1=ALU.add), [k_X, k_cw2, k_acc], [k_acc])
            p.op("act", lambda e, acc=acc, qo=qo: e.activation(
                out=qo, in_=acc.rearrange("q j (c t) -> q c j t", t=CH), func=AF.Silu), [k_acc], [k_qo])
            p.dma("pool", lambda e, qo=qo, c0=c0: [e.dma_start(
                out=QK_c[c0:c0 + NT // CH, gg].rearrange("c q j t -> q c (j t)"),
                in_=qo[gg * 64:(gg + 1) * 64].rearrange("q c j t -> q c (j t)"),
                allow_slow_non_contiguous=True) for gg in range(2)], [k_qo], ["QK_c"], sk(k_qo), ndma=2)
        p.barrier()

    def phase_B():
        ar.reset()
        Sf = [ar.alloc([4, 128], F32) for _ in range(2)]
        Sb = [ar.alloc([4, 128], BF16) for _ in range(2)]
        Cf = [ar.alloc([4, 132], F32, parts=64) for _ in range(2)]
        Cb = [ar.alloc([4, 132], BF16, parts=64) for _ in range(2)]
        for (t, k) in Sf + Sb + Cf + Cb:
            p.op("pool", lambda e, t=t: e.memset(t, 0.0), [], [k])
        lfb = Buf(ar, 2, [512], F32, parts=64)
        qfb = Buf(ar, 2, [4, CH], BF16)
        kfb = Buf(ar, 2, [4, CH], BF16)
        ktb = Buf(ar, 2, [512], BF16, parts=64)
        vtb = Buf(ar, 2, [512], BF16, parts=64)
        E1b = Buf(ar, 2, [4, 128], F32)
        E2b = Buf(ar, 2, [4, CH], F32)
        EUb = Buf(ar, 2, [512], F32, parts=64)
        qqb = Buf(ar, 2, [4, 128], BF16)
        kkb = Buf(ar, 2, [4, CH], BF16)
        kSb = Buf(ar, 2, [512], BF16, parts=64)
        ATb = Buf(ar, 2, [4, CH], BF16, parts=64)
        osb = Buf(ar, 2, [512], BF16, parts=64)
        Vxb = Buf(ar, 2, [4, 132], BF16, parts=64)
        gtb = Buf(ar, 2, [16], F32, parts=64)
        qkb = Buf(ar, 2, [8, CH], BF16, parts=64)
        argb = Buf(ar, 2, [16], F32, parts=64)
        EXb = Buf(ar, 2, [16], F32, parts=64)
        ATmb = Buf(ar, 2, [4, CH], BF16, parts=64)
        kSmb = Buf(ar, 2, [4, CH], BF16, parts=64)
        adb = Buf(ar, 2, [8], F32, parts=64)
        omb = Buf(ar, 2, [512], BF16, parts=64)
        for (t, k) in Vxb.slots:
            p.op("pool", lambda e, t=t: e.memset(t, 1.0), [], [k])

        def qpos(h):
            return (h % 2) * 4 + h // 2

        def kpos(h):
            return (h % 2) * 4 + 2 + h // 2

        for it in range(NCH):
            for d in range(2):
                if d == 0:
                    c = it
                else:
                    c = (3 - it) if it < 4 else (NCH + 3 - it)
                tok0 = c * CH
                tl = CH - 1 if d == 0 else 0
                (lf, k_lf) = lfb.next()
                (qf, k_qf) = qfb.next()
                (kf, k_kf) = kfb.next()
                (kt, k_kt) = ktb.next()
                (vt, k_vt) = vtb.next()
                load(lf, k_lf, LF_tm[d, tok0:tok0 + CH, :], sk(k_lf), ["LF_tm"])
                load(qf, k_qf, QA_c[c], sk(k_qf), ["QA_c"])
                load(kf, k_kf, KA_c[d, c], sk(k_kf), ["KA_c"])
                load(kt, k_kt, KA_tm[d, tok0:tok0 + CH, :], sk(k_kt), ["KA_tm"])
                load(vt, k_vt, VA_tm[tok0:tok0 + CH, :], sk(k_vt), ["VA_tm"])
                (E1, k_E1) = E1b.next()
                (E2, k_E2) = E2b.next()
                (EU, k_EU) = EUb.next()
                (qq, k_qq) = qqb.next()
                (kk, k_kk) = kkb.next()
                (kS, k_kS) = kSb.next()
                (AT, k_AT) = ATb.next()
                (os_, k_os) = osb.next()

                def p1(e, lf=lf, d=d):
                    for h in range(4):
                        r_ = e.matmul(pv(0, [4, 128])[:, h, :], lhsT=lf[:, h * 128:(h + 1) * 128],
                                      rhs=cm[:, d, 0:2, :], start=True, stop=True)
                    return r_
                p.op("pe", p1, [k_lf, k_cm], [BK[0]])
                p.op("pe", lambda e, lf=lf, d=d: e.matmul(banks[1][0:64, :], lhsT=cm[:, d, 2, :], rhs=lf,
                                                          start=True, stop=True), [k_lf, k_cm], [BK[1]])
                p.op("act", lambda e, E1=E1: e.activation(out=E1, in_=pv(0, [4, 128]), func=AF.Exp), [BK[0]], [k_E1])
                p.op("act", lambda e, E2=E2: e.activation(out=E2, in_=pv(0, [4, 128])[:, :, 0:CH], func=AF.Exp,
                                                          scale=-1.0), [BK[0]], [k_E2])
                p.op("act", lambda e, EU=EU: e.activation(out=EU, in_=banks[1][0:64, :], func=AF.Exp), [BK[1]], [k_EU])
                p.op("dve", lambda e, qq=qq, E1=E1, qf=qf: e.tensor_tensor(
                    out=qq.rearrange("q h (a t) -> q h a t", a=2), in0=E1.rearrange("q h (a t) -> q h a t", a=2),
                    in1=qf.unsqueeze(2).to_broadcast([128, 4, 2, CH]), op=ALU.mult), [k_E1, k_qf], [k_qq])
                p.op("dve", lambda e, kk=kk, E2=E2, kf=kf: e.tensor_tensor(out=kk, in0=E2, in1=kf, op=ALU.mult),
                     [k_E2, k_kf], [k_kk])
                p.op("dve", lambda e, kS=kS, EU=EU, kt=kt: e.tensor_tensor(out=kS, in0=EU, in1=kt, op=ALU.mult),
                     [k_EU, k_kt], [k_kS])

                def p3(e, kk=kk, qq=qq):
                    for h in range(4):
                        r_ = e.matmul(pv(2, [4, CH], parts=64)[:, h, :], lhsT=kk[:, h, :], rhs=qq[:, h, 0:CH],
                                      start=True, stop=True)
                    return r_
                p.op("pe", p3, [k_kk, k_qq], [BK[2]])
                p.op("dve", lambda e, AT=AT, d=d: e.tensor_tensor(
                    out=AT, in0=pv(2, [4, CH], parts=64),
                    in1=cm[:, d, 3, :].unsqueeze(1).to_broadcast([64, 4, CH]), op=ALU.mult), [BK[2], k_cm], [k_AT])

                def p4(e, AT=AT, vt=vt, qq=qq, d=d):
                    for h in range(4):
                        e.matmul(banks[3][0:64, h * 128:(h + 1) * 128], lhsT=AT[:, h, :],
                                 rhs=vt[:, h * 128:(h + 1) * 128], start=True, stop=False)
                        r_ = e.matmul(banks[3][0:64, h * 128:(h + 1) * 128], lhsT=qq[:, h, CH:2 * CH],
                                      rhs=Sb[d][0][:, h, :], start=False, stop=True)
                    return r_
                p.op("pe", p4, [k_AT, k_vt, k_qq, Sb[d][1]], [BK[3]])

                def p5(e, kS=kS, vt=vt):
                    for h in range(4):
                        r_ = e.matmul(pv(4, [4, 128])[:, h, :], lhsT=kS[:, h * 128:(h + 1) * 128],
                                      rhs=vt[:, h * 128:(h + 1) * 128], start=True, stop=True)
                    return r_
                p.op("pe", p5, [k_kS, k_vt], [BK[4]])
                p.op("act", lambda e, os_=os_: e.activation(out=os_, in_=banks[3][0:64, :], func=AF.Copy),
                     [BK[3]], [k_os])
                store(O_d[d][tok0:tok0 + CH, 0:512], ["O%d" % d], os_, k_os, sk(k_os))
                for h in range(4):
                    p.op("dve", lambda e, h=h, d=d, E1=E1, tl=tl: e.scalar_tensor_tensor(
                        out=Sf[d][0][:, h, :], in0=Sf[d][0][:, h, :], scalar=E1[:, h, CH + tl:CH + tl + 1],
                        in1=pv(4, [4, 128])[:, h, :], op0=ALU.mult, op1=ALU.add),
                        [Sf[d][1], k_E1, BK[4]], [Sf[d][1]])
                p.op("act", lambda e, d=d: e.activation(out=Sb[d][0], in_=Sf[d][0], func=AF.Copy),
                     [Sf[d][1]], [Sb[d][1]])

                (Vx, k_Vx) = Vxb.next()
                (gt, k_gt) = gtb.next()
                (qk, k_qk) = qkb.next()
                (arg, k_arg) = argb.next()
                (EX, k_EX) = EXb.next()
                (ATm, k_ATm) = ATmb.next()
                (kSm, k_kSm) = kSmb.next()
                (ad, k_ad) = adb.next()
                (om, k_om) = omb.next()
                load(qk.rearrange("q (gg j) t -> q gg (j t)", gg=2), k_qk,
                     QK_c[c].rearrange("gg q j t -> q gg (j t)"), sk(k_qk), ["QK_c"])
                load(Vx[:, :, 0:128], k_Vx, VB_tm[tok0:tok0 + CH, :].rearrange("t (h e) -> t h e", h=4),
                     sk(k_Vx), ["VB_tm"])
                load(gt, k_gt, GT_d[tok0:tok0 + CH, :], sk(k_gt), ["GT_d"])

                lfc = gt[:, 8 * d + 4:8 * d + 8]
                igc = gt[:, 8 * d:8 * d + 4]

                def pg(e, lfc=lfc, d=d):
                    e.matmul(banks[6][0:64, 0:4], lhsT=cm[:, d, 1, :], rhs=lfc, start=True, stop=True)
                    return e.matmul(banks[6][0:64, 8:12], lhsT=ones64, rhs=lfc, start=True, stop=True)
                p.op("pe", pg, [k_gt, k_cm, k_ones64], [BK[6]])
                p.op("dve", lambda e, arg=arg, igc=igc: e.tensor_tensor(out=arg[:, 0:4], in0=igc,
                                                                        in1=banks[6][0:64, 0:4], op=ALU.subtract),
                     [k_gt, BK[6]], [k_arg])
                p.op("dve", lambda e, arg=arg: e.tensor_tensor(out=arg[:, 4:8], in0=banks[6][0:64, 8:12],
                                                               in1=arg[:, 0:4], op=ALU.add), [k_arg, BK[6]], [k_arg])
                p.op("dve", lambda e, arg=arg: e.tensor_scalar(out=arg[:, 8:12], in0=banks[6][0:64, 0:4],
                                                               scalar1=-1.0, scalar2=LN8, op0=ALU.mult, op1=ALU.add),
                     [BK[6]], [k_arg])
                p.op("dve", lambda e, arg=arg: e.tensor_copy(out=arg[:, 12:16], in_=banks[6][0:64, 8:12]),
                     [BK[6]], [k_arg])
                p.op("act", lambda e, arg=arg, EX=EX: e.activation(out=EX, in_=arg, func=AF.Exp), [k_arg], [k_EX])

                def pst(e, qk=qk):
                    for h in range(4):
                        e.matmul(pv(7, [4, CH], parts=64)[:, h, :], lhsT=qk[:, kpos(h), :], rhs=qk[:, qpos(h), :],
                                 start=True, stop=True)
                    for h in range(4):
                        r_ = e.transpose(out=pv(7, [4, CH], BF16, parts=64, off=256)[:, h, :], in_=qk[:, kpos(h), :],
                                         identity=identb[0:64, 0:64])
                    return r_
                p.op("pe", pst, [k_qk, k_identb], [BK[7]])
                for h in range(4):
                    p.op("dve", lambda e, h=h, ATm=ATm, EX=EX, d=d: e.scalar_tensor_tensor(
                        out=ATm[:, h, :], in0=pv(7, [4, CH], parts=64)[:, h, :], scalar=EX[:, h:h + 1],
                        in1=cm[:, d, 3, :], op0=ALU.mult, op1=ALU.mult), [BK[7], k_EX, k_cm], [k_ATm])
                p.op("dve", lambda e, kSm=kSm, EX=EX: e.tensor_tensor(
                    out=kSm, in0=pv(7, [4, CH], BF16, parts=64, off=256),
                    in1=EX[:, 4:8].unsqueeze(2).to_broadcast([64, 4, CH]), op=ALU.mult), [BK[7], k_EX], [k_kSm])

                def pn(e, ATm=ATm, Vx=Vx, qk=qk, d=d):
                    for h in range(4):
                        bk = 0 if h < 2 else 1
                        o = pv(bk, [2, 132], parts=64)[:, h % 2, 0:129]
                        e.matmul(o, lhsT=ATm[:, h, :], rhs=Vx[:, h, 0:129], start=True, stop=False)
                        r_ = e.matmul(o, lhsT=qk[:, qpos(h), :], rhs=Cb[d][0][:, h, 0:129], start=False, stop=True)
                    return r_
                p.op("pe", pn, [k_ATm, k_Vx, k_qk, Cb[d][1]], [BK[0], BK[1]])

                def pc(e, kSm=kSm, Vx=Vx):
                    for h in range(4):
                        bk = 2 if h < 2 else 4
                        r_ = e.matmul(pv(bk, [2, 132], parts=64)[:, h % 2, 0:129], lhsT=kSm[:, h, :],
                                      rhs=Vx[:, h, 0:129], start=True, stop=True)
                    return r_
                p.op("pe", pc, [k_kSm, k_Vx], [BK[2], BK[4]])
                for hb in range(2):
                    p.op("act", lambda e, hb=hb, ad=ad: e.activation(
                        out=ad[:, 2 * hb:2 * hb + 2], in_=pv(hb, [2, 132], parts=64)[:, :, 128], func=AF.Abs),
                        [BK[hb]], [k_ad])
                p.op("dve", lambda e, ad=ad, EX=EX: e.tensor_tensor(out=ad[:, 0:4], in0=ad[:, 0:4], in1=EX[:, 8:12],
                                                                    op=ALU.max), [k_ad, k_EX], [k_ad])
                p.op("dve", lambda e, ad=ad: e.reciprocal(out=ad[:, 4:8], in_=ad[:, 0:4]), [k_ad], [k_ad])
                for h in range(4):
                    p.op("act", lambda e, h=h, om=om, ad=ad: e.activation(
                        out=om[:, h * 128:(h + 1) * 128], in_=pv(h // 2, [2, 132], parts=64)[:, h % 2, 0:128],
                        func=AF.Copy, scale=ad[:, 4 + h:5 + h]), [BK[h // 2], k_ad], [k_om])
                store(O_d[d][tok0:tok0 + CH, 512:1024], ["O%d" % d], om, k_om, sk(k_om))
                for h in range(4):
                    bk = 2 if h < 2 else 4
                    p.op("dve", lambda e, h=h, d=d, EX=EX, bk=bk: e.scalar_tensor_tensor(
                        out=Cf[d][0][:, h, 0:129], in0=Cf[d][0][:, h, 0:129], scalar=EX[:, 12 + h:13 + h],
                        in1=pv(bk, [2, 132], parts=64)[:, h % 2, 0:129], op0=ALU.mult, op1=ALU.add),
                        [Cf[d][1], k_EX, BK[bk]], [Cf[d][1]])
                p.op("act", lambda e, d=d: e.activation(out=Cb[d][0], in_=Cf[d][0], func=AF.Copy),
                     [Cf[d][1]], [Cb[d][1]])
        p.barrier()

    def phase_C1():
        ar.reset()
        WO, k_WO = ar.alloc([8, D], BF16)
        load_weight_bf16(WO, k_WO, ab_w_out, 8, "wO")
        OG, k_OG = ar.alloc([D], F32)
        load(OG, k_OG, ab_out_g.unsqueeze(0).to_broadcast([128, D]), "c1og")
        ofb = Buf(ar, 2, [D], BF16)
        obb = Buf(ar, 2, [D], BF16)
        o32b = Buf(ar, 2, [D], F32)
        gb = Buf(ar, 2, [D], BF16)
        xb = Buf(ar, 2, [D], F32)
        sqb = Buf(ar, 2, [D], F32)
        s8b = Buf(ar, 2, [16], F32)
        omb = Buf(ar, 2, [D], BF16)
        oTb = Buf(ar, 2, [8, 128], BF16)
        ssb = Buf(ar, 2, [4], F32)
        xob = Buf(ar, 2, [D], F32)
        for t in range(TOK // 128):
            t0 = t * 128
            r = 1 if t0 < NCTX else 0
            (of, k_of) = ofb.next()
            (ob, k_ob) = obb.next()
            (g, k_g) = gb.next()
            (xt, k_xt) = xb.next()
            (sq, k_sq) = sqb.next()
            (s8, k_s8) = s8b.next()
            (om, k_om) = omb.next()
            (oT, k_oT) = oTb.next()
            load(of, k_of, O_d[0][t0:t0 + 128, :], sk(k_of), ["O0"])
            load(ob, k_ob, O_d[1][t0:t0 + 128, :], sk(k_ob), ["O1"])
            load(g, k_g, G_tm[t0:t0 + 128, :], sk(k_g), ["G_tm"])
            load(xt, k_xt, xs[t0:t0 + 128, :], sk(k_xt))
            (o32, k_o32) = o32b.next()
            p.op("dve", lambda e, of=of, ob=ob, o32=o32: e.tensor_tensor(out=o32, in0=of, in1=ob, op=ALU.add),
                 [k_of, k_ob], [k_o32])
            of, k_of = o32, k_o32
            p.op("act", lambda e, of=of, sq=sq: e.activation(out=sq, in_=of, func=AF.Square), [k_of], [k_sq])
            p.op("dve", lambda e, sq=sq, s8=s8: e.tensor_reduce(
                out=s8[:, 0:8], in_=sq.rearrange("q (h e) -> q h e", h=8), axis=AX.X, op=ALU.add), [k_sq], [k_s8])
            rstd_from_ssq(s8[:, 0:8], k_s8, s8[:, 8:16], k_s8, 128)
            p.op("dve", lambda e, of=of, s8=s8: e.tensor_tensor(
                out=of.rearrange("q (h e) -> q h e", h=8), in0=of.rearrange("q (h e) -> q h e", h=8),
                in1=s8[:, 8:16].unsqueeze(2).to_broadcast([128, 8, 128]), op=ALU.mult), [k_of, k_s8], [k_of])
            p.op("dve", lambda e, of=of: e.tensor_tensor(out=of, in0=of, in1=OG, op=ALU.mult), [k_of, k_OG], [k_of])
            p.op("dve", lambda e, of=of, g=g, om=om: e.tensor_tensor(out=om, in0=of, in1=g, op=ALU.mult),
                 [k_of, k_g], [k_om])

            def tr(e, om=om):
                for k in range(8):
                    r_ = e.transpose(out=pv(0, [8, 128], BF16)[:, k, :], in_=om[:, k * 128:(k + 1) * 128],
                                     identity=identb)
                return r_
            p.op("pe", tr, [k_om, k_identb], [BK[0]])
            p.op("act", lambda e, oT=oT: e.activation(out=oT, in_=pv(0, [8, 128], BF16), func=AF.Copy),
                 [BK[0]], [k_oT])
            pyb = (1 + 2 * (t % 2), 2 + 2 * (t % 2))
            for hf in range(2):
                def mm(e, hf=hf, oT=oT, bk=pyb[hf]):
                    for k in range(8):
                        r_ = e.matmul(banks[bk][:, :], lhsT=oT[:, k, :], rhs=WO[:, k, hf * 512:(hf + 1) * 512],
                                      start=(k == 0), stop=(k == 7))
                    return r_
                p.op("pe", mm, [k_oT, k_WO], [BK[pyb[hf]]])
            residual_out(pyb, xt, k_xt, GG1, k_GG1, r, XM[t0:t0 + 128, :], "XM", (sqb, ssb, xob), "sxm")
        p.barrier()

    def phase_FFN(l, src, skey, dst, dkey, ntok, ctx_tokens):
        ar.reset()
        W1, k_W1 = ar.alloc([8, 2 * DFF], BF16)
        W2, k_W2 = ar.alloc([22, D], BF16)
        load_weight_bf16(W1, k_W1, ffn_w_in[l], 8, "w1")
        load_weight_bf16(W2, k_W2, ffn_w_out[l], 22, "w2")
        NTM = 512
        xb = Buf(ar, 2, [D], F32)
        sqb = Buf(ar, 1, [D], BF16)
        ssb = Buf(ar, 4, [4], F32)
        xnb = Buf(ar, 1, [D], F32)
        hTb = Buf(ar, 1, [8, NTM], BF16)
        aTb = Buf(ar, 1, [22, NTM], BF16)
        sgb = Buf(ar, 2, [NTM], F32)
        xob = Buf(ar, 1, [D], F32)
        fmb = [2, 3, 4, 5]
        fm_i = [0]

        def fm_slot():
            i = fm_i[0]
            fm_i[0] = (i + 1) % 4
            return fmb[i]

        sts = []
        t_ = 0
        if ctx_tokens:
            sts.append((0, ctx_tokens))
            t_ = ctx_tokens
        while t_ < ntok:
            sts.append((t_, NTM))
            t_ += NTM
        assert t_ == ntok
        for (tok0, NT) in sts:
            r = 1 if tok0 < ctx_tokens else 0
            (hT, k_hT) = hTb.next()
            for j in range(NT // 128):
                t0 = tok0 + j * 128
                prep_tile(src[t0:t0 + 128, :], hT, k_hT, j * 128, gm2, k_gm2, 3, r, (xb, sqb, ssb, xnb))
            (aT, k_aT) = aTb.next()
            for cb in range(22):
                bg = fm_slot()
                bu = fm_slot()
                (sg, k_sg) = sgb.next()

                def mm(e, bk, col0, hT=hT, NT=NT):
                    for k in range(8):
                        r_ = e.matmul(banks[bk][:, 0:NT], lhsT=W1[:, k, col0:col0 + 128], rhs=hT[:, k, 0:NT],
                                      start=(k == 0), stop=(k == 7))
                    return r_
                p.op("pe", lambda e, bg=bg, cb=cb, mm=mm: mm(e, bg, cb * 128), [k_hT, k_W1], [BK[bg]])
                p.op("pe", lambda e, bu=bu, cb=cb, mm=mm: mm(e, bu, DFF + cb * 128), [k_hT, k_W1], [BK[bu]])
                p.op("act", lambda e, bg=bg, sg=sg, NT=NT: e.activation(out=sg[:, 0:NT], in_=banks[bg][:, 0:NT],
                                                                        func=AF.Silu), [BK[bg]], [k_sg])
                p.op("dve", lambda e, bu=bu, sg=sg, aT=aT, cb=cb, NT=NT: e.tensor_tensor(
                    out=aT[:, cb, 0:NT], in0=sg[:, 0:NT], in1=banks[bu][:, 0:NT], op=ALU.mult),
                    [k_sg, BK[bu]], [k_aT])
            for j in range(NT // 128):
                t0 = tok0 + j * 128
                pyb = (6, 7)
                for hf in range(2):
                    def mm2(e, hf=hf, aT=aT, j=j, bk=pyb[hf]):
                        for cb in range(22):
                            r_ = e.matmul(banks[bk][:, :], lhsT=aT[:, cb, j * 128:(j + 1) * 128],
                                          rhs=W2[:, cb, hf * 512:(hf + 1) * 512], start=(cb == 0), stop=(cb == 21))
                        return r_
                    p.op("pe", mm2, [k_aT, k_W2], [BK[pyb[hf]]])
                (xt, k_xt) = xb.next()
                load(xt, k_xt, src[t0:t0 + 128, :], sk(k_xt))
                residual_out(pyb, xt, k_xt, GG2, k_GG2, r, dst[t0:t0 + 128, :], dkey, (sqb, ssb, xob), "sff")
        p.barrier()

    state = {}

    def phase_D():
        ar.reset()
        state["hi0"] = ar.hi
        KT, k_KT = ar.alloc([2, TOK], BF16, persist=True)
        Vs, k_Vs = ar.alloc([TOK // 128, 2, 132], BF16, persist=True)
        state["KT"] = (KT, k_KT)
        state["Vs"] = (Vs, k_Vs)
        p.op("pool", lambda e: e.memset(Vs, 1.0), [], [k_Vs])
        WQ, k_WQ = ar.alloc([8, 1536], BF16)
        load_weight_bf16(WQ, k_WQ, attn_w_qkv, 8, "wq")
        QKG, k_QKG = ar.alloc([10, 128], F32)
        load(QKG[:, 0:8, :], k_QKG, attn_qk_g[0:1, :].unsqueeze(1).to_broadcast([128, 8, 128]), "d0")
        load(QKG[:, 8:10, :], k_QKG, attn_qk_g[1:2, :].unsqueeze(1).to_broadcast([128, 2, 128]), "d1")
        p.op("dve", lambda e: e.tensor_scalar(out=QKG[:, 0:8, :], in0=QKG[:, 0:8, :], scalar1=128.0 ** -0.5,
                                              scalar2=None, op0=ALU.mult), [k_QKG], [k_QKG])
        NT = 256
        xb = Buf(ar, 2, [D], F32)
        sqb = Buf(ar, 1, [D], BF16)
        ssb = Buf(ar, 2, [4], F32)
        xnb = Buf(ar, 2, [D], F32)
        hTb = Buf(ar, 2, [8, NT], BF16)
        rpb = Buf(ar, 2, [2, 128], F32)
        sq2 = Buf(ar, 2, [10, 128], F32)
        s10 = Buf(ar, 2, [20], F32)
        t1b = Buf(ar, 2, [10, 128], F32)
        t2b = Buf(ar, 2, [10, 128], F32)
        qrb = Buf(ar, 2, [10, 128], BF16)
        qsb = Buf(ar, 2, [8, 128], BF16)
        def prep_st(st):
            tok0 = st * NT
            r = 1 if tok0 < NCTX else 0
            (hT, k_hT) = hTb.next()
            for j in range(NT // 128):
                t0 = tok0 + j * 128
                prep_tile(X1[t0:t0 + 128, :], hT, k_hT, j * 128, gm1, k_gm1, 0, r, (xb, sqb, ssb, xnb))
            return hT, k_hT

        nxt = prep_st(0)
        for st in range(TOK // NT):
            tok0 = st * NT
            is_ctx = tok0 < NCTX
            r = 1 if is_ctx else 0
            (hT, k_hT) = nxt if (PIPE_PREP or st == 0) else prep_st(st)
            if PIPE_PREP and st + 1 < TOK // NT:
                nxt = prep_st(st + 1)
            for j in range(NT // 128):
                t0 = tok0 + j * 128
                tile = t0 // 128
                own = (not is_ctx) and (t0 - NCTX) < OWN
                nh = 10 if own else 2
                h0 = 0 if own else 8
                def mm(e, bk, col0, j=j, hT=hT):
                    for k in range(8):
                        r_ = e.matmul(banks[bk][:, :], lhsT=hT[:, k, j * 128:(j + 1) * 128],
                                      rhs=WQ[:, k, col0:col0 + 512], start=(k == 0), stop=(k == 7))
                    return r_
                p.op("pe", lambda e, mm=mm: mm(e, 4, 1024), [k_hT, k_WQ], [BK[4]])
                if own:
                    p.op("pe", lambda e, mm=mm: mm(e, 2, 0), [k_hT, k_WQ], [BK[2]])
                    p.op("pe", lambda e, mm=mm: mm(e, 3, 512), [k_hT, k_WQ], [BK[3]])
                p.op("act", lambda e, tile=tile: e.activation(
                    out=Vs[:, tile, :, 0:128], in_=pv(4, [4, 128])[:, 2:4, :], func=AF.Copy), [BK[4]], [k_Vs])
                (sq, k_sq) = sq2.next()
                (s1, k_s1) = s10.next()
                (t1, k_t1) = t1b.next()
                (t2, k_t2) = t2b.next()
                (qr, k_qr) = qrb.next()
                srcs = []
                if own:
                    srcs += [(2, 0, 4), (3, 4, 4)]
                srcs += [(4, 8, 2)]
                for (bk, hh, n) in srcs:
                    p.op("act", lambda e, bk=bk, hh=hh, n=n, sq=sq: e.activation(
                        out=sq[:, hh:hh + n, :], in_=pv(bk, [4, 128])[:, 0:n, :], func=AF.Square), [BK[bk]], [k_sq])
                p.op("dve", lambda e, sq=sq, s1=s1, h0=h0, nh=nh: e.tensor_reduce(
                    out=s1[:, h0:h0 + nh], in_=sq[:, h0:h0 + nh, :], axis=AX.X, op=ALU.add), [k_sq], [k_s1])
                rstd_from_ssq(s1[:, h0:h0 + nh], k_s1, s1[:, 10 + h0:10 + h0 + nh], k_s1, 128)
                for (bk, hh, n) in srcs:
                    p.op("dve", lambda e, bk=bk, hh=hh, n=n, t1=t1, s1=s1: e.tensor_tensor(
                        out=t1[:, hh:hh + n, :], in0=pv(bk, [4, 128])[:, 0:n, :],
                        in1=s1[:, 10 + hh:10 + hh + n].unsqueeze(2).to_broadcast([128, n, 128]), op=ALU.mult),
                        [BK[bk], k_s1], [k_t1])
                if is_ctx:
                    p.op("dve", lambda e, t1=t1, qr=qr: e.tensor_tensor(
                        out=qr[:, 8:10, :], in0=t1[:, 8:10, :], in1=QKG[:, 8:10, :], op=ALU.mult),
                        [k_t1, k_QKG], [k_qr])
                else:
                    (rp, k_rp) = rpb.next()
                    lt0 = t0 - NCTX
                    load(rp, k_rp, c_rope[lt0:lt0 + 128], sk(k_rp))
                    sl = slice(h0, h0 + nh)
                    p.op("dve", lambda e, t1=t1, sl=sl: e.tensor_tensor(
                        out=t1[:, sl, :], in0=t1[:, sl, :], in1=QKG[:, sl, :], op=ALU.mult), [k_t1, k_QKG], [k_t1])
                    p.op("dve", lambda e, t1=t1, t2=t2, rp=rp, sl=sl, nh=nh: e.tensor_tensor(
                        out=t2[:, sl, :], in0=t1[:, sl, :],
                        in1=rp[:, 0, :].unsqueeze(1).to_broadcast([128, nh, 128]), op=ALU.mult),
                        [k_t1, k_rp], [k_t2])

                    def v4(a, sl=sl):
                        return a[:, sl, :].rearrange("q h (a b) -> q h a b", a=2)
                    for (ho, hi) in ((0, 32), (32, 0)):
                        p.op("dve", lambda e, t1=t1, sq=sq, rp=rp, ho=ho, hi=hi, nh=nh, v4=v4: e.tensor_tensor(
                            out=v4(sq)[:, :, :, ho:ho + 32], in0=v4(t1)[:, :, :, hi:hi + 32],
                            in1=rp[:, 1, :].rearrange("q (a b) -> q a b", a=2)[:, :, ho:ho + 32]
                            .unsqueeze(1).to_broadcast([128, nh, 2, 32]), op=ALU.mult),
                            [k_t1, k_rp], [k_sq])
                    p.op("dve", lambda e, t2=t2, sq=sq, qr=qr, sl=sl: e.tensor_tensor(
                        out=qr[:, sl, :], in0=t2[:, sl, :], in1=sq[:, sl, :], op=ALU.add), [k_t2, k_sq], [k_qr])
                def trk(e, qr=qr):
                    for g in range(2):
                        r_ = e.transpose(out=pv(5, [8, 128], BF16)[:, g, :], in_=qr[:, 8 + g, :], identity=identb)
                    return r_
                p.op("pe", trk, [k_qr, k_identb], [BK[5]])
                p.op("act", lambda e, t0=t0: e.activation(out=KT[:, :, t0:t0 + 128],
                                                          in_=pv(5, [8, 128], BF16)[:, 0:2, :], func=AF.Copy),
                     [BK[5]], [k_KT])
                if own:
                    (qs, k_qs) = qsb.next()

                    def trq(e, qr=qr):
                        for h in range(8):
                            r_ = e.transpose(out=pv(6, [8, 128], BF16)[:, h, :], in_=qr[:, h, :], identity=identb)
                        return r_
                    p.op("pe", trq, [k_qr, k_identb], [BK[6]])
                    p.op("act", lambda e, qs=qs: e.activation(out=qs, in_=pv(6, [8, 128], BF16), func=AF.Copy),
                         [BK[6]], [k_qs])
                    store(QT_d[(t0 - NCTX) // 128], ["QT_d"], qs, k_qs, sk(k_qs))
        p.barrier()

    def phase_E():
        ar.reset()
        (KT, k_KT) = state["KT"]
        (Vs, k_Vs) = state["Vs"]
        WO, k_WO = ar.alloc([8, D], BF16)
        load_weight_bf16(WO, k_WO, attn_w_out, 8, "wo1")
        qTb = Buf(ar, 2, [8, 128], BF16)
        xb = Buf(ar, 2, [D], F32)
        PTb = Buf(ar, 4, [512], BF16)
        atb = Buf(ar, 2, [8, 128], BF16)
        aTb = Buf(ar, 2, [8, 128], BF16)
        rdb = Buf(ar, 2, [4], F32)
        rdnb = Buf(ar, 2, [512], F32)
        onesb, k_onesb = ar.alloc([128], BF16)
        p.op("pool", lambda e: e.memset(onesb, 1.0), [], [k_onesb])
        sqb = Buf(ar, 1, [D], BF16)
        ssb = Buf(ar, 2, [4], F32)
        xob = Buf(ar, 2, [D], F32)
        NKT = TOK // 128
        NQ = OWN // 128
        units = [(qi, g, kt) for qi in range(NQ) for g in range(2) for kt in range(NKT)]
        qbuf = {}

        def get_q(qi):
            if qi not in qbuf:
                (qT, k_qT) = qTb.next()
                load(qT, k_qT, QT_d[qi], sk(k_qT), ["QT_d"])
                qbuf[qi] = (qT, k_qT)
            return qbuf[qi]

        pts = {}

        def issue_score(u):
            (qi, g, kt) = units[u]
            (qT, k_qT) = get_q(qi)
            sb_ = u % 2
            (PT, k_PT) = PTb.next()
            pts[u] = (PT, k_PT)
            p.op("pe", lambda e, sb_=sb_, g=g, kt=kt, qT=qT: e.matmul(
                banks[sb_][:, :], lhsT=KT[:, g, kt * 128:(kt + 1) * 128],
                rhs=qT[:, 4 * g:4 * g + 4, :].rearrange("q h t -> q (h t)"), start=True, stop=True),
                [k_KT, k_qT], [BK[sb_]])
            p.op("act", lambda e, sb_=sb_, PT=PT: e.activation(out=PT, in_=banks[sb_][:, :], func=AF.Exp),
                 [BK[sb_]], [k_PT])

        for u_ in range(AHEAD):
            issue_score(u_)
        cur = {}
        for u, (qi, g, kt) in enumerate(units):
            if g == 0 and kt == 0:
                (xt, k_xt) = xb.next()
                (at, k_at) = atb.next()
                (aT, k_aT) = aTb.next()
                load(xt, k_xt, X1[NCTX + qi * 128:NCTX + (qi + 1) * 128, :], sk(k_xt), ["X1"])
                cur = dict(xt=xt, k_xt=k_xt, at=at, k_at=k_at, aT=aT, k_aT=k_aT)
            if kt == 0:
                (rd, k_rd) = rdb.next()
                cur["rd"], cur["k_rd"] = rd, k_rd
            pob = (3 + 2 * g, 4 + 2 * g)
            if AHEAD == 0:
                issue_score(u)
            (PT, k_PT) = pts.pop(u)

            def pvm(e, PT=PT, kt=kt, g=g, pob=pob):
                e.matmul(banks[pob[0]][:, :], lhsT=Vs[:, kt, g, 0:128], rhs=PT,
                         start=(kt == 0), stop=(kt == NKT - 1))
                return e.matmul(banks[pob[1]][:, :], lhsT=onesb, rhs=PT,
                                start=(kt == 0), stop=(kt == NKT - 1))
            if AHEAD > 0 and u + AHEAD < len(units):
                issue_score(u + AHEAD)
            p.op("pe", pvm, [k_PT, k_Vs, k_onesb], [BK[pob[0]], BK[pob[1]]])
            if kt == NKT - 1:
                aT, k_aT = cur["aT"], cur["k_aT"]
                (rdn, k_rdn) = rdnb.next()
                p.op("dve", lambda e, rdn=rdn, pob=pob: e.reciprocal(out=rdn, in_=banks[pob[1]][:, :]),
                     [BK[pob[1]]], [k_rdn])
                p.op("dve", lambda e, rdn=rdn, pob=pob, aT=aT, g=g: e.tensor_tensor(
                    out=aT[:, 4 * g:4 * g + 4, :].rearrange("q h t -> q (h t)"), in0=banks[pob[0]][:, :],
                    in1=rdn, op=ALU.mult), [BK[pob[0]], k_rdn], [k_aT])
                if g == 1:
                    xt, k_xt = cur["xt"], cur["k_xt"]
                    pyb = (2, 7)
                    for hf in range(2):
                        def mm(e, hf=hf, aT=aT, bk=pyb[hf]):
                            for k in range(8):
                                r_ = e.matmul(banks[bk][:, :], lhsT=aT[:, k, :], rhs=WO[:, k, hf * 512:(hf + 1) * 512],
                                              start=(k == 0), stop=(k == 7))
                            return r_
                        p.op("pe", mm, [k_aT, k_WO], [BK[pyb[hf]]])
                    residual_out(pyb, xt, k_xt, GG1, k_GG1, 0, X2[qi * 128:(qi + 1) * 128, :], "X2",
                                 (sqb, ssb, xob), "sx2")
        p.barrier()
        ar.hi = state["hi0"]

    phases = [
        ("ada0", lambda: ada_layer(0)),
        ("A", phase_A),
        ("A2", phase_A2),
        ("B", phase_B),
        ("C1", phase_C1),
        ("C2", lambda: phase_FFN(0, XM, "XM", X1, "X1", TOK, NCTX)),
        ("ada1", lambda: ada_layer(1)),
        ("D", phase_D),
        ("E", phase_E),
        ("F", lambda: phase_FFN(1, X2, "X2", y_out, "y", OWN, 0)),
    ]
    finals = []
    for name, fn in phases:
        fn()
        if stop_after == name:
            break
    else:
        finals = ["y"]
    if debug:
        finals = finals + [k for k in ("ADA_d", "QA_c", "KA_c", "QK_c", "KA_tm", "LF_tm", "VA_tm", "BQK_d", "VB_tm", "GT_d",
                                       "G_tm", "O0", "O1", "XM", "X1", "X2", "QT_d") if k in p.last_writer]
    stats = p.emit(final_wait_keys=finals)
    es.close()
    return nc, stats


def _consts():
    s = np.arange(64)[:, None]
    t = np.arange(64)[None, :]
    cm = np.zeros((64, 2, 4, 64), np.float32)
    tri_f = (s <= t).astype(np.float32)
    cm[:, 0, 0] = tri_f - (s <= 31).astype(np.float32)
    cm[:, 0, 1] = tri_f
    cm[:, 0, 2] = (s > t).astype(np.float32)
    cm[:, 0, 3] = tri_f
    tri_b = (s >= t).astype(np.float32)
    cm[:, 1, 0] = tri_b - (s >= 32).astype(np.float32)
    cm[:, 1, 1] = tri_b
    cm[:, 1, 2] = (s < t).astype(np.float32)
    cm[:, 1, 3] = tri_b
    sel = np.zeros((2, 2, 128), np.float32)
    sel[0, 0, :] = 1.0
    sel[1, 1, :] = 1.0
    pos = np.arange(NLAT)
    row = (pos // 64).astype(np.float32)
    col = (pos % 64).astype(np.float32)
    inv = np.power(np.float32(10000.0), -np.arange(0, 64, 2, dtype=np.float32) / np.float32(64)).astype(np.float32)
    ar_ = (row[:, None] * inv[None, :]).astype(np.float32)
    ac_ = (col[:, None] * inv[None, :]).astype(np.float32)
    rope = np.zeros((NLAT, 2, 128), np.float32)
    rope[:, 0, 0:32] = np.cos(ar_)
    rope[:, 0, 32:64] = np.cos(ar_)
    rope[:, 0, 64:96] = np.cos(ac_)
    rope[:, 0, 96:128] = np.cos(ac_)
    rope[:, 1, 0:32] = -np.sin(ar_)
    rope[:, 1, 32:64] = np.sin(ar_)
    rope[:, 1, 64:96] = -np.sin(ac_)
    rope[:, 1, 96:128] = np.sin(ac_)
    return {"c_ident": np.eye(128, dtype=np.float32), "c_cm": cm, "c_sel": sel}, rope


def make_in_maps(inputs, cores=range(8)):
    f = lambda a: np.ascontiguousarray(np.asarray(a, dtype=np.float32))
    x, c, ctx, c_ctx = f(inputs["x"]), f(inputs["c"]), f(inputs["ctx"]), f(inputs["c_ctx"])
    consts, rope = _consts()
    w_in = f(inputs["ab_w_in"])[0]
    conv = f(inputs["ab_conv"])[0]
    gb = f(inputs["ab_gate_b"])[0].reshape(16)
    lb = f(inputs["hgrn_lb"])
    w_in_r = w_in.copy()
    w_in_r[:, 1536:2048] = w_in[:, 2048:2560]
    w_in_r[:, 2048:2560] = w_in[:, 1536:2048]
    w_in_r[:, 4096:4104] = w_in[:, 4104:4112]
    w_in_r[:, 4104:4112] = w_in[:, 4096:4104]
    conv_r = np.ascontiguousarray(conv[::-1])
    gb_r = np.concatenate([gb[8:16], gb[0:8]])
    lb_r = np.ascontiguousarray(lb[::-1])
    rope_r = np.ascontiguousarray(rope[::-1])
    shared = {
        "ada_w": f(inputs["ada_w"]), "ada_b": f(inputs["ada_b"]), "norm_g": f(inputs["norm_g"]),
        "ffn_w_in": f(inputs["ffn_w_in"]), "ffn_w_out": f(inputs["ffn_w_out"]),
        "ab_out_g": f(inputs["ab_out_g"])[0], "ab_w_out": f(inputs["ab_w_out"])[0],
        "attn_w_qkv": f(inputs["attn_w_qkv"])[0], "attn_qk_g": f(inputs["attn_qk_g"])[0],
        "attn_w_out": f(inputs["attn_w_out"])[0],
    }
    shared.update(consts)
    maps = []
    for core in cores:
        b, half = core // 2, core % 2
        m = dict(shared)
        if half == 0:
            m["xs"] = np.ascontiguousarray(np.concatenate([ctx[b], x[b]], axis=0))
            m.update({"ab_w_in": w_in, "ab_conv": conv, "ab_gate_b": gb, "hgrn_lb": lb, "c_rope": rope})
        else:
            m["xs"] = np.ascontiguousarray(np.concatenate([ctx[b][::-1], x[b][::-1]], axis=0))
            m.update({"ab_w_in": w_in_r, "ab_conv": conv_r, "ab_gate_b": gb_r, "hgrn_lb": lb_r, "c_rope": rope_r})
        m["cvec"] = np.ascontiguousarray(np.stack([c[b], c_ctx], axis=0))
        maps.append(m)
    return maps


_NC_CACHE = {}


def kernel(**inputs):
    if "nc" not in _NC_CACHE:
        _NC_CACHE["nc"] = build_nc()[0]
    nc = _NC_CACHE["nc"]
    maps = make_in_maps(inputs)
    res = run_bass_kernel_spmd(nc, maps, core_ids=list(range(8)))
    out = np.zeros((4, NLAT, D), np.float32)
    for core in range(8):
        b, half = core // 2, core % 2
        y = np.asarray(res.results[core]["y"])
        if half == 0:
            out[b, 0:OWN] = y
        else:
            out[b, OWN:NLAT] = y[::-1]
    return out
```
